# Optimizing a Trainium2 kernel written in Bass

```python
import functools
import jax, jax.numpy as jnp
from jax import lax
import numpy as np

D_MODEL = 1024
BATCH = 32
SEQ = 256
DEPTH = 2
DEC_BATCH = 2
DEC_SEQ = 1024
PAST_LEN = 512

GRID_W = 64
HEAD_DIM = 64
N_Q_HEADS = 8
N_KV_HEADS = 2
GQA_GROUP = N_Q_HEADS // N_KV_HEADS
ATTN_WIDTH = N_Q_HEADS * HEAD_DIM
KV_WIDTH = N_KV_HEADS * HEAD_DIM
ATTN_SCALE = HEAD_DIM ** -0.5
Q_BLOCK = 128
ROPE_THETA = 10000.0
AXIS_ROT = HEAD_DIM // 2
CONV_WIDTH = 256
SGU_WIDTH = 256
SGU_HEADS = 4
SGU_HEAD_DIM = SGU_WIDTH // SGU_HEADS
CHUNK = 128
MIX_WIDTH = ATTN_WIDTH + CONV_WIDTH + SGU_WIDTH
_Q_END = ATTN_WIDTH
_K_END = _Q_END + KV_WIDTH
_V_END = _K_END + KV_WIDTH
_CI_END = _V_END + CONV_WIDTH
_CB_END = _CI_END + CONV_WIDTH
_CC_END = _CB_END + CONV_WIDTH
_SU_END = _CC_END + SGU_WIDTH
IN_WIDTH = _SU_END + SGU_WIDTH
IN_SPLITS = (_Q_END, _K_END, _V_END, _CI_END, _CB_END, _CC_END, _SU_END)
D_FF = 2816
N_EXPERTS = 8
TOP_K = 2
D_FF_EXPERT = 1408
EXPERT_BLOCK = 128
EPS = 1e-6
DEEPNORM_ALPHA = (2 * DEPTH) ** 0.25
DEEPNORM_BETA = (8 * DEPTH) ** -0.25
F32 = jnp.float32

kernel_name = "hybrid_dit_prefix_ctx_step"


def _standardize(xf):
    mu = jnp.mean(xf, -1, keepdims=True)
    d = xf - mu
    return d * lax.rsqrt(jnp.mean(jnp.square(d), -1, keepdims=True) + EPS)


def _ln_plain(x):
    return _standardize(x.astype(F32)).astype(x.dtype)


def _ln_affine(x, g, b):
    return (_standardize(x.astype(F32)) * g.astype(F32) + b.astype(F32)).astype(x.dtype)


def _rms_heads(x, g):
    xf = x.astype(F32)
    y = xf * lax.rsqrt(jnp.mean(jnp.square(xf), -1, keepdims=True) + EPS) * g.astype(F32)
    return y.astype(x.dtype)


def _axial_rope_tables(n_tokens):
    rows = n_tokens // GRID_W
    row = jnp.repeat(jnp.arange(rows, dtype=F32), GRID_W)
    col = jnp.tile(jnp.arange(GRID_W, dtype=F32), rows)
    inv_freq = ROPE_THETA ** (-jnp.arange(0, AXIS_ROT, 2, dtype=F32) / AXIS_ROT)
    ang = jnp.stack([row[:, None] * inv_freq, col[:, None] * inv_freq], axis=1)
    return jnp.cos(ang), jnp.sin(ang)


def _apply_rope(x, cos, sin):
    B, S, H, _ = x.shape
    xf = x.astype(F32).reshape(B, S, H, 2, 2, AXIS_ROT // 2)
    x1, x2 = xf[..., 0, :], xf[..., 1, :]
    c = cos[None, :, None]
    s = sin[None, :, None]
    out = jnp.stack([x1 * c - x2 * s, x2 * c + x1 * s], axis=-2)
    return out.reshape(x.shape).astype(x.dtype)


def _block_attention(q, k, v):
    B, S = q.shape[:2]
    nb = S // Q_BLOCK
    qb = q.reshape(B, nb, Q_BLOCK, N_KV_HEADS, GQA_GROUP, HEAD_DIM).transpose(1, 0, 2, 3, 4, 5)

    def one_block(qblk):
        s = jnp.einsum('bqhgd,bkhd->bhgqk', qblk, k).astype(F32) * ATTN_SCALE
        p = jax.nn.softmax(s, axis=-1).astype(v.dtype)
        return jnp.einsum('bhgqk,bkhd->bqhgd', p, v)

    o = lax.map(one_block, qb)
    return o.transpose(1, 0, 2, 3, 4, 5).reshape(B, S, ATTN_WIDTH)


def _latent_attention(q, k, v, k_ctx, v_ctx, cos, sin):
    q = _apply_rope(q, cos, sin)
    k = _apply_rope(k, cos, sin)
    k_all = jnp.concatenate([k, k_ctx.astype(k.dtype)], axis=1)
    v_all = jnp.concatenate([v, v_ctx.astype(v.dtype)], axis=1)
    return _block_attention(q, k_all, v_all)


def _short_conv(x, w, b):
    xp = jnp.pad(x, ((0, 0), (1, 1), (0, 0)))
    return xp[:, :-2] * w[0] + xp[:, 1:-1] * w[1] + xp[:, 2:] * w[2] + b


def _chunk_mlp(u, v, g, w_s, b_s):
    B, S, _ = v.shape
    n = S // CHUNK
    vh = v.reshape(B, n, CHUNK, SGU_HEADS, SGU_HEAD_DIM)
    vn = (_standardize(vh.astype(F32)) * g.reshape(SGU_HEADS, SGU_HEAD_DIM).astype(F32)).astype(v.dtype)
    s = jnp.einsum('hpq,bnqhc->bnphc', w_s, vn) + b_s.T[:, :, None]
    return u * s.reshape(B, S, SGU_WIDTH)


def _token_mixers(h, attend, p):
    B, S, _ = h.shape
    z = h @ p['w_in']
    q, k, v, c_in, c_b, c_c, s_u, s_v = jnp.split(z, IN_SPLITS, axis=-1)
    q = _rms_heads(q.reshape(B, S, N_Q_HEADS, HEAD_DIM), p['q_g'])
    k = _rms_heads(k.reshape(B, S, N_KV_HEADS, HEAD_DIM), p['k_g'])
    v = v.reshape(B, S, N_KV_HEADS, HEAD_DIM)
    a_out = attend(q, k, v)
    conv_out = c_b * _short_conv(c_c * c_in, p['conv_w'], p['conv_b'])
    sgu_out = _chunk_mlp(s_u, s_v, p['sgu_g'], p['sgu_w'], p['sgu_b'])
    mix = jnp.concatenate([a_out, conv_out, sgu_out], axis=-1) @ p['w_out']
    return mix, k, v


def _modulation(cond, w, b):
    m = (jax.nn.silu(cond) @ w + b)[:, None, :]
    return jnp.split(m, 6, axis=-1)


def _swiglu(h, w1, w3, w2):
    return (jax.nn.silu(h @ w1) * (h @ w3)) @ w2


def _moe_swiglu(h, router_w, w1, w3, w2):
    shp = h.shape
    x = h.reshape(-1, shp[-1])
    T = x.shape[0]
    logits = (x @ router_w).astype(F32)
    top_v, top_i = lax.top_k(logits, TOP_K)
    gates = jax.nn.softmax(top_v, axis=-1)
    n_assign = T * TOP_K
    e_flat = top_i.reshape(-1).astype(jnp.int32)
    tok_flat = jnp.repeat(jnp.arange(T, dtype=jnp.int32), TOP_K)
    counts = jnp.bincount(e_flat, length=N_EXPERTS)
    padded = (counts + EXPERT_BLOCK - 1) // EXPERT_BLOCK * EXPERT_BLOCK
    pad_end = jnp.cumsum(padded)
    pad_start = pad_end - padded
    cnt_start = jnp.cumsum(counts) - counts
    order = jnp.argsort(e_flat * n_assign + jnp.arange(n_assign, dtype=jnp.int32))
    e_sorted = e_flat[order]
    dest = pad_start[e_sorted] + jnp.arange(n_assign, dtype=jnp.int32) - cnt_start[e_sorted]
    n_blocks = -(-n_assign // EXPERT_BLOCK) + N_EXPERTS
    n_rows = n_blocks * EXPERT_BLOCK
    row_tok = jnp.full((n_rows,), T, jnp.int32).at[dest].set(tok_flat[order])
    row_gate = jnp.zeros((n_rows,), x.dtype).at[dest].set(gates.reshape(-1)[order].astype(x.dtype))
    block_e = jnp.minimum(
        jnp.searchsorted(pad_end, jnp.arange(n_blocks, dtype=jnp.int32) * EXPERT_BLOCK, side='right'),
        N_EXPERTS - 1)
    x_pad = jnp.concatenate([x, jnp.zeros((1, x.shape[1]), x.dtype)], axis=0)
    x_rows = x_pad[row_tok].reshape(n_blocks, EXPERT_BLOCK, x.shape[1])

    def expert_block(args):
        xb, e = args
        return _swiglu(xb, w1[e], w3[e], w2[e])

    y_rows = lax.map(expert_block, (x_rows, block_e)).reshape(n_rows, x.shape[1])
    y = jnp.zeros((T + 1, x.shape[1]), x.dtype).at[row_tok].add(y_rows * row_gate[:, None])
    return y[:T].reshape(shp)


def _layer(x, cond, attend, p, ffn):
    shift_a, scale_a, gate_a, shift_f, scale_f, gate_f = _modulation(cond, p['ada_w'], p['ada_b'])
    h = _ln_plain(x) * (1 + scale_a) + shift_a
    mix, k, v = _token_mixers(h, attend, p)
    x = _ln_affine(DEEPNORM_ALPHA * x + gate_a * mix, p['ln1_g'], p['ln1_b'])
    h = _ln_plain(x) * (1 + scale_f) + shift_f
    x = _ln_affine(DEEPNORM_ALPHA * x + gate_f * ffn(h), p['ln2_g'], p['ln2_b'])
    return x, k, v


def setup_inputs(seed: int = 0) -> dict:
    key = jax.random.key(seed)
    ks = jax.random.split(key, 28)
    n_dense = (DEPTH + 1) // 2
    n_moe = DEPTH // 2

    def nrm(k, shape, s):
        return jax.random.normal(k, shape, F32) * s

    return {
        'x_prompt': nrm(ks[0], (BATCH, SEQ, D_MODEL), 1.0),
        'x_sample': nrm(ks[1], (DEC_BATCH, DEC_SEQ, D_MODEL), 1.0),
        'cache_k': nrm(ks[2], (DEC_BATCH, DEPTH, PAST_LEN, N_KV_HEADS, HEAD_DIM), 1.0),
        'cache_v': nrm(ks[3], (DEC_BATCH, DEPTH, PAST_LEN, N_KV_HEADS, HEAD_DIM), 1.0),
        'c': nrm(ks[4], (DEC_BATCH, D_MODEL), 1.0),
        'c_ctx': nrm(ks[5], (D_MODEL,), 1.0),
        'ada_w': nrm(ks[6], (DEPTH, D_MODEL, 6 * D_MODEL), 0.5 * D_MODEL ** -0.5),
        'ada_b': nrm(ks[7], (DEPTH, 6 * D_MODEL), 0.02),
        'w_in': nrm(ks[8], (DEPTH, D_MODEL, IN_WIDTH), D_MODEL ** -0.5),
        'q_norm_g': 1.0 + nrm(ks[9], (DEPTH, HEAD_DIM), 0.02),
        'k_norm_g': 1.0 + nrm(ks[10], (DEPTH, HEAD_DIM), 0.02),
        'conv_w': nrm(ks[11], (DEPTH, 3, CONV_WIDTH), 3 ** -0.5),
        'conv_b': nrm(ks[12], (DEPTH, CONV_WIDTH), 0.02),
        'sgu_norm_g': 1.0 + nrm(ks[13], (DEPTH, SGU_WIDTH), 0.02),
        'sgu_w': nrm(ks[14], (DEPTH, SGU_HEADS, CHUNK, CHUNK), CHUNK ** -0.5),
        'sgu_b': 1.0 + nrm(ks[15], (DEPTH, SGU_HEADS, CHUNK), 0.02),
        'w_out': nrm(ks[16], (DEPTH, MIX_WIDTH, D_MODEL), MIX_WIDTH ** -0.5 * DEEPNORM_BETA),
        'ln1_g': 1.0 + nrm(ks[17], (DEPTH, D_MODEL), 0.02),
        'ln1_b': nrm(ks[18], (DEPTH, D_MODEL), 0.02),
        'ln2_g': 1.0 + nrm(ks[19], (DEPTH, D_MODEL), 0.02),
        'ln2_b': nrm(ks[20], (DEPTH, D_MODEL), 0.02),
        'ffn_w1': nrm(ks[21], (n_dense, D_MODEL, D_FF), D_MODEL ** -0.5),
        'ffn_w3': nrm(ks[22], (n_dense, D_MODEL, D_FF), D_MODEL ** -0.5),
        'ffn_w2': nrm(ks[23], (n_dense, D_FF, D_MODEL), D_FF ** -0.5 * DEEPNORM_BETA),
        'router_w': nrm(ks[24], (n_moe, D_MODEL, N_EXPERTS), D_MODEL ** -0.5),
        'moe_w1': nrm(ks[25], (n_moe, N_EXPERTS, D_MODEL, D_FF_EXPERT), D_MODEL ** -0.5),
        'moe_w3': nrm(ks[26], (n_moe, N_EXPERTS, D_MODEL, D_FF_EXPERT), D_MODEL ** -0.5),
        'moe_w2': nrm(ks[27], (n_moe, N_EXPERTS, D_FF_EXPERT, D_MODEL), D_FF_EXPERT ** -0.5 * DEEPNORM_BETA),
    }


def reference(x_prompt, x_sample, cache_k, cache_v, c, c_ctx, ada_w, ada_b, w_in, q_norm_g, k_norm_g,
              conv_w, conv_b, sgu_norm_g, sgu_w, sgu_b, w_out, ln1_g, ln1_b, ln2_g, ln2_b,
              ffn_w1, ffn_w3, ffn_w2, router_w, moe_w1, moe_w3, moe_w2):
    cos, sin = _axial_rope_tables(x_sample.shape[1])
    ctx_cond = c_ctx[None, :]
    y_p = x_prompt
    y_s = x_sample
    new_ks = []
    new_vs = []
    for l in range(DEPTH):
        p = {
            'ada_w': ada_w[l], 'ada_b': ada_b[l], 'w_in': w_in[l],
            'q_g': q_norm_g[l], 'k_g': k_norm_g[l],
            'conv_w': conv_w[l], 'conv_b': conv_b[l],
            'sgu_g': sgu_norm_g[l], 'sgu_w': sgu_w[l], 'sgu_b': sgu_b[l],
            'w_out': w_out[l],
            'ln1_g': ln1_g[l], 'ln1_b': ln1_b[l], 'ln2_g': ln2_g[l], 'ln2_b': ln2_b[l],
        }
        i = l // 2
        if l % 2 == 0:
            ffn = functools.partial(_swiglu, w1=ffn_w1[i], w3=ffn_w3[i], w2=ffn_w2[i])
        else:
            ffn = functools.partial(_moe_swiglu, router_w=router_w[i], w1=moe_w1[i], w3=moe_w3[i], w2=moe_w2[i])
        y_p, k_ctx, v_ctx = _layer(y_p, ctx_cond, _block_attention, p, ffn)
        new_ks.append(k_ctx)
        new_vs.append(v_ctx)
        lat_attend = functools.partial(_latent_attention, k_ctx=cache_k[:, l], v_ctx=cache_v[:, l], cos=cos, sin=sin)
        y_s, _, _ = _layer(y_s, c, lat_attend, p, ffn)
    new_k = jnp.stack(new_ks, axis=1)
    new_v = jnp.stack(new_vs, axis=1)
    return (y_p, y_s, new_k, new_v)
```

```python
from contextlib import ExitStack
import numpy as np
import concourse.bass as bass
import concourse.mybir as mybir
from concourse.bass_utils import run_bass_kernel_spmd

F32 = mybir.dt.float32
BF16 = mybir.dt.bfloat16
AF = mybir.ActivationFunctionType
ALU = mybir.AluOpType
AX = mybir.AxisListType

D = 1024
NT = 10
NTOK = 1280
DEPTH = 2
EPS = 1e-6
ALPHA = (2 * DEPTH) ** 0.25
SCALE = 64 ** -0.5
NEG = -30000.0
TG = [(0, 512), (512, 512), (1024, 256)]


class Res:
    __slots__ = ("name", "last_w", "readers", "par_w", "psum")

    def __init__(self, name="", psum=False):
        self.name = name
        self.psum = psum
        self.last_w = None
        self.readers = []
        self.par_w = []


class Prog:
    COMPUTE = ("pe", "act", "dve", "pool")
    NQ = 8

    def __init__(self, nc):
        self.nc = nc
        self.eng = {"pe": nc.tensor, "act": nc.scalar, "dve": nc.vector,
                    "pool": nc.gpsimd, "sp": nc.sync}
        self.count = {e: 0 for e in self.COMPUTE}
        self.ops = {e: [] for e in self.eng}
        self.seen = {e: {} for e in self.eng}
        self.semobj = {}
        self.dsem = {}
        self.dcount = {}
        self.dnext = {}
        for e in self.COMPUTE:
            self.semobj[f"prog_{e}"] = nc.alloc_semaphore(name=f"prog_{e}")
        for q in ("sp", "pool"):
            self.dsem[q] = []
            for i in range(self.NQ):
                nm = f"dma_{q}_{i}"
                self.semobj[nm] = nc.alloc_semaphore(name=nm)
                self.dsem[q].append(nm)
                self.dcount[nm] = 0
            self.dnext[q] = 0
        self.final = []
        self.extra = {e: [] for e in self.eng}

    def _need(self, waits, tok):
        if tok is None:
            return
        s, v = tok
        if waits.get(s, 0) < v:
            waits[s] = v

    def _deps(self, e, reads, writes, pwrites=()):
        waits = {}
        for s, v in self.extra[e]:
            self._need(waits, (s, v))
        self.extra[e] = []
        for r in reads:
            self._need(waits, r.last_w)
            for t in r.par_w:
                self._need(waits, t)
        for w in writes:
            self._need(waits, w.last_w)
            for t in w.par_w:
                self._need(waits, t)
            for t in w.readers:
                self._need(waits, t)
        for w in pwrites:
            self._need(waits, w.last_w)
            for t in w.readers:
                self._need(waits, t)
        out = []
        for s, v in waits.items():
            if e == "pe" and s == "prog_pe":
                continue
            if self.seen[e].get(s, 0) >= v:
                continue
            self.seen[e][s] = v
            out.append((s, v))
        return out

    def _commit(self, tok, reads, writes, pwrites=()):
        for r in reads:
            r.readers.append(tok)
        for w in writes:
            w.last_w = tok
            w.readers = []
            w.par_w = []
        for w in pwrites:
            w.par_w.append(tok)

    PAR = True

    def op(self, e, reads, writes, fn, pwrites=()):
        if not self.PAR:
            writes, pwrites = list(writes) + list(pwrites), ()
        if e != "pe":
            extra_w = [r for r in reads if r.psum and r not in writes]
            if extra_w:
                writes = list(writes) + extra_w
        waits = self._deps(e, reads, writes, pwrites)
        self.count[e] += 1
        tok = (f"prog_{e}", self.count[e])
        self.ops[e].append((waits, fn, tok))
        self._commit(tok, reads, writes, pwrites)
        return tok

    def dma(self, q, reads, writes, fn, is_output=False, pwrites=()):
        i = self.dnext[q]
        self.dnext[q] = (i + 1) % self.NQ
        nm = self.dsem[q][i]
        waits = self._deps(q, reads, writes, pwrites)
        prev = self.dcount[nm]
        if prev > 0 and self.seen[q].get(nm, 0) < prev:
            self.seen[q][nm] = prev
            waits.append((nm, prev))
        self.dcount[nm] = prev + 16
        tok = (nm, prev + 16)
        self.ops[q].append((waits, fn, tok))
        self._commit(tok, reads, writes, pwrites)
        if is_output:
            self.final.append(tok)
        return tok

    def barrier(self):
        toks = [(f"prog_{e}", self.count[e]) for e in self.COMPUTE if self.count[e] > 0]
        toks += [(nm, v) for nm, v in self.dcount.items() if v > 0]
        for e in self.eng:
            self.extra[e] = list(toks)

    def emit(self):
        nc = self.nc
        fin = {}
        for s, v in self.final:
            fin[s] = max(fin.get(s, 0), v)

        def run(e, engine):
            for waits, fn, tok in self.ops[e]:
                for s, v in waits:
                    engine.wait_ge(self.semobj[s], v)
                ins = fn(engine)
                s, v = tok
                ins.then_inc(self.semobj[s], 1 if s.startswith("prog_") else 16)
            if e == "sp":
                for s, v in fin.items():
                    engine.wait_ge(self.semobj[s], v)

        with nc.Block() as block:
            @block.tensor
            def _(eng):
                run("pe", eng)

            @block.scalar
            def _(eng):
                run("act", eng)

            @block.vector
            def _(eng):
                run("dve", eng)

            @block.gpsimd
            def _(eng):
                run("pool", eng)

            @block.sync
            def _(eng):
                run("sp", eng)


def build_program(n_layers=DEPTH):
    nc = bass.Bass("TRN2", target_bir_lowering=False)

    def din(name, shape):
        return nc.dram_tensor(name, list(shape), F32, kind="ExternalInput").ap()

    def dout(name, shape):
        return nc.dram_tensor(name, list(shape), F32, kind="ExternalOutput").ap()

    x_d = din("x", [NTOK, D])
    cond_d = din("cond", [16, 128])
    ck_d = din("ck", [DEPTH, 512, 128])
    cv_d = din("cv", [DEPTH, 512, 128])
    mq_d = din("mq", [64, NTOK])
    mk_d = din("mk", [64, 1792])
    rc_d = din("ropec", [NTOK, 64])
    rs_d = din("ropes", [NTOK, 64])
    cflag_d = din("cflag", [128, 4])
    ident_d = din("ident", [128, 128])
    ada_w_d = din("ada_w", [DEPTH, 12, 128, 4096])
    ada_b_d = din("ada_b", [DEPTH, 48, 128])
    w_in_d = din("w_in", [DEPTH, D, 2048])
    w_fm_d = din("w_in_fm", [DEPTH, 6, 128, 1024])
    qg_d = din("q_g", [DEPTH, 64])
    kg_d = din("k_g", [DEPTH, 64])
    convw_d = din("conv_w", [DEPTH, 6, 128])
    convb_d = din("conv_b", [DEPTH, 2, 128])
    sgug_d = din("sgu_g", [DEPTH, 256])
    sguw_d = din("sgu_w", [DEPTH, 4, 128, 128])
    sgub_d = din("sgu_b", [DEPTH, 4, 128])
    w_out_d = din("w_out", [DEPTH, D, D])
    ln1g_d = din("ln1_g", [DEPTH, D])
    ln1b_d = din("ln1_b", [DEPTH, D])
    ln2g_d = din("ln2_g", [DEPTH, D])
    ln2b_d = din("ln2_b", [DEPTH, D])
    fw1_d = din("ffn_w1", [2, 128, 8 * 1408])
    fw3_d = din("ffn_w3", [2, 128, 8 * 1408])
    fw2_d = din("ffn_w2", [2816, D])
    rw_d = din("router_w", [D, 8])
    mw1_d = din("moe_w1", [8, 128, 8 * 1408])
    mw3_d = din("moe_w3", [8, 128, 8 * 1408])
    mw2_d = din("moe_w2", [8, 1408, D])

    y_d = dout("y", [NTOK, D])
    nk_d = dout("nk", [DEPTH, NTOK, 128])
    nv_d = dout("nv", [DEPTH, NTOK, 128])

    P = Prog(nc)

    def I(eng, method, reads, writes, *a, pw=(), **kw):
        return P.op(eng, reads, writes, lambda e: getattr(e, method)(*a, **kw), pwrites=pw)

    def MM(reads, writes, lst):
        return P.op("pe", reads, writes, lambda e: [e.matmul(**kw) for kw in lst][-1])

    def TR(reads, writes, lst, pw=()):
        return P.op("pe", reads, writes, lambda e: [e.transpose(*a) for a in lst][-1], pwrites=pw)

    def DMA(q, reads, writes, out, in_, is_output=False, pw=()):
        return P.dma(q, reads, writes, lambda e: e.dma_start(out=out, in_=in_), is_output=is_output, pwrites=pw)

    with ExitStack() as top:
        uid = [0]

        def S(es, name, shape, dt=F32):
            uid[0] += 1
            return es.enter_context(nc.sbuf_tensor(f"s{uid[0]}_{name}", list(shape), dt))

        banks = [top.enter_context(nc.psum_tensor(f"bank{i}", [128, 512], F32)) for i in range(8)]
        rbank = [Res(f"bank{i}", psum=True) for i in range(8)]
        pool_ptr = {"a": 0, "b": 0}

        def bank(pool):
            i = pool_ptr[pool]
            pool_ptr[pool] = (i + 1) % 4
            j = i if pool == "a" else 4 + i
            return banks[j], rbank[j]

        x_sb = S(top, "x_sb", [128, NT, D])
        r_x = [Res(f"x{t}") for t in range(NT)]
        hT = S(top, "hT", [128, 8, NTOK], BF16)
        r_h = [Res(f"hT{t}") for t in range(NT)]
        mx67 = S(top, "mx67", [128, 2, NTOK], BF16)
        mx45 = S(top, "mx45", [128, 2, NTOK], BF16)
        gate4 = S(top, "gate4", [128, 4, D])
        r_gate = [Res(f"gate{i}") for i in range(4)]
        lnp = S(top, "lnp", [128, 4, D])
        r_lnp = [Res(f"lnp{i}") for i in range(4)]
        modT = S(top, "modT", [128, 48, 2])
        r_modT = Res("modT")
        fm = S(top, "fm", [128, 72])
        r_fm = Res("fm")
        identf = S(top, "identf", [128, 128])
        r_id = Res("ident")
        cflag = S(top, "cflag", [128, 4])
        r_cflag = Res("cflag")
        condT = S(top, "condT", [128, 16])
        scTb = S(top, "scTb", [128, 16], BF16)
        scAB = S(top, "scAB", [128, 8, 2], BF16)
        r_sc = Res("sc")
        G = S(top, "G", [128, NT, 8])
        r_G = [Res(f"G{t}") for t in range(NT)]
        epsc = S(top, "epsc", [128, 1])
        r_eps = Res("eps")

        def mixT(kc):
            if kc < 4:
                return hT[:, kc, :]
            return mx45[:, kc - 4, :] if kc < 6 else mx67[:, kc - 6, :]

        DMA("sp", [], [r_id], identf[:], ident_d)
        DMA("sp", [], [r_cflag], cflag[:], cflag_d)
        I("dve", "memset", [], [r_eps], epsc[:], EPS)

        def rstd_from_var(var_ap, out_ap, r_in, r_out):
            I("act", "activation", [r_in, r_eps], [r_out], out=out_ap, in_=var_ap, func=AF.Sqrt,
              bias=epsc[:, 0:1], scale=1.0)
            I("dve", "reciprocal", [r_out], [r_out], out=out_ap, in_=out_ap)

        with ExitStack() as es0:
            stg = S(es0, "stg0", [16, 128])
            r_stg = Res("stg0")
            DMA("sp", [], [r_stg], stg[:], cond_d)
            I("act", "activation", [r_stg], [r_stg], out=stg[:], in_=stg[:], func=AF.Silu)
            pb, rpb = bank("a")
            TR([r_stg, r_id], [rpb], [(pb[:, 0:16], stg[:], identf[0:16, 0:16])])
            I("dve", "tensor_copy", [rpb], [r_sc], out=condT[:], in_=pb[:, 0:16])
            I("dve", "tensor_copy", [r_sc], [r_sc], out=scTb[:], in_=condT[:])
            I("dve", "tensor_copy", [r_sc], [r_sc], out=scAB[:].rearrange("p k c -> p c k"),
              in_=condT[:].rearrange("p (c k) -> p c k", k=8))
            for t in range(NT):
                DMA("sp", [], [r_x[t]], x_sb[:, t, :], x_d[t * 128:(t + 1) * 128, :])
        P.barrier()

        for l in range(n_layers):
            moe = (l % 2 == 1)
            last = (l == n_layers - 1)

            with ExitStack() as es:
                awb = [S(es, f"awb{i}", [128, 8, 512], BF16) for i in range(3)]
                r_awb = [Res(f"awb{i}") for i in range(3)]
                rowsb = [S(es, f"rowsb{i}", [2, 512]) for i in range(2)]
                r_rows = [Res(f"rows{i}") for i in range(2)]
                gbias = [S(es, f"gbias{i}", [128, 512]) for i in range(2)]
                r_gb = [Res(f"gb{i}") for i in range(2)]
                scRep = S(es, "scRep", [128, 16, 128], BF16)
                r_scRep = Res("scRep")
                stg = S(es, "stgl", [64, 128])
                r_stg = Res("stgl")
                I("dve", "tensor_copy", [r_sc], [r_scRep], out=scRep[:],
                  in_=scTb[:].unsqueeze(2).to_broadcast([128, 16, 128]))
                DMA("sp", [], [], stg[0:48, :], ada_b_d[l], pw=[r_stg])
                DMA("sp", [], [], stg[48:54, :], convw_d[l], pw=[r_stg])
                DMA("sp", [], [], stg[54:56, :], convb_d[l], pw=[r_stg])
                DMA("sp", [], [], stg[56:60, :], sgub_d[l], pw=[r_stg])
                pb, rpb = bank("a")
                TR([r_stg, r_id], [rpb], [(pb[:, 0:60], stg[0:60, :], identf[0:60, 0:60])])
                I("dve", "tensor_copy", [rpb], [r_fm], out=fm[:, 0:60], in_=pb[:, 0:60])
                I("dve", "tensor_scalar", [r_fm], [r_fm], out=fm[:, 60:66], in0=fm[:, 48:54],
                  scalar1=-1.0, scalar2=None, op0=ALU.mult)
                for i, dd in enumerate((ln1g_d, ln1b_d, ln2g_d, ln2b_d)):
                    DMA("sp", [], [r_lnp[i]], lnp[:, i, :], dd[l:l + 1, :].partition_broadcast(128))

                pfm, rpfm = bank("b")
                gi = 0
                for cc in range(12):
                    slot, half = cc // 2, cc % 2
                    bi = cc % 3
                    DMA("pool", [], [r_awb[bi]], awb[bi][:],
                        ada_w_d[l, cc].rearrange("p (kc n) -> p kc n", kc=8))
                    prow, rprow = bank("a")
                    MM([r_awb[bi], r_sc], [rprow], [dict(out=prow[0:2, :], lhsT=scAB[:, kc, :], rhs=awb[bi][:, kc, :],
                                                        start=(kc == 0), stop=(kc == 7)) for kc in range(8)])
                    I("act", "copy", [rprow], [r_rows[cc % 2]], out=rowsb[cc % 2][0:2, :], in_=prow[0:2, :])
                    TR([r_rows[cc % 2], r_id], [], [(pfm[:, 2 * (cc * 4 + oc):2 * (cc * 4 + oc) + 2],
                                                    rowsb[cc % 2][0:2, oc * 128:(oc + 1) * 128], identf[0:2, 0:2]) for oc in range(4)], pw=[rpfm])
                    if slot in (2, 5):
                        gslot = 0 if slot == 2 else 2
                        DMA("sp", [], [r_gb[gi % 2]], gbias[gi % 2][:],
                            ada_b_d[l, cc * 4:(cc + 1) * 4, :].rearrange("(o a) b -> o (a b)", o=1).partition_broadcast(128))
                        for cnd in range(2):
                            pr_, rpr = bank("a")
                            MM([r_awb[bi], r_scRep], [rpr],
                               [dict(out=pr_[:], lhsT=scRep[:, cnd * 8 + kc, :], rhs=awb[bi][:, kc, :],
                                     start=(kc == 0), stop=(kc == 7)) for kc in range(8)])
                            I("dve", "tensor_tensor", [rpr, r_gb[gi % 2]], [r_gate[gslot + cnd]],
                              out=gate4[:, gslot + cnd, half * 512:(half + 1) * 512], in0=pr_[:],
                              in1=gbias[gi % 2][:], op=ALU.add)
                        gi += 1
                I("dve", "tensor_tensor", [rpfm, r_fm], [r_modT], out=modT[:],
                  in0=pfm[:, 0:96].rearrange("p (c k) -> p c k", k=2),
                  in1=fm[:, 0:48].unsqueeze(2).to_broadcast([128, 48, 2]), op=ALU.add)
                for s_ in (1, 4):
                    I("dve", "tensor_scalar", [r_modT], [r_modT], out=modT[:, s_ * 8:(s_ + 1) * 8, :],
                      in0=modT[:, s_ * 8:(s_ + 1) * 8, :], scalar1=1.0, scalar2=None, op0=ALU.add)
            P.barrier()

            with ExitStack() as es:
                ropec = S(es, "ropec", [128, NT, 64])
                ropes = S(es, "ropes", [128, NT, 64])
                r_rope = Res("rope")
                qTall = S(es, "qTall", [128, 8, NTOK], BF16)
                r_qT = [Res(f"qT{t}") for t in range(NT)]
                kTall = S(es, "kTall", [128, 2, 1792], BF16)
                r_kT = [Res(f"kT{t}") for t in range(14)]
                vaug = S(es, "vaug", [128, 14, 2, 128], BF16)
                r_va = [Res(f"va{t}") for t in range(14)]
                wbuf = S(es, "wbuf", [128, 8, 1280], BF16)
                r_wbuf = Res("wbuf")
                wfb = S(es, "wfb", [128, 8, 256], BF16)
                r_wfb = Res("wfb")
                u_sb = S(es, "u_sb", [128, NTOK])
                y_sb = S(es, "y_sb", [128, NTOK])
                r_u, r_y = Res("u"), Res("y")
                xn0 = S(es, "xn", [128, D])
                rdbuf = S(es, "rdbuf", [128, D])
                bufA0 = S(es, "bufA", [128, 640])
                bufB0 = S(es, "bufB", [128, 640])
                bufC0 = S(es, "bufC", [128, 640])
                kvs0 = S(es, "kvs", [128, 640])
                Eb = [S(es, f"E{i}", [128, 512], BF16) for i in range(5)]
                r_E = [Res(f"E{i}") for i in range(5)]
                rd = [rdbuf[:, 0:512], rdbuf[:, 512:1024]]
                r_rd = [Res(f"rd{i}") for i in range(2)]
                g64 = S(es, "g64", [128, 2, 64])
                sgug = S(es, "sgug", [128, 256])
                r_gv = Res("gv")
                wsT = S(es, "wsT", [128, 4, 128], BF16)
                r_wsT = Res("wsT")
                rwf = S(es, "rwf", [128, 8, 8])
                r_rw = Res("rw")
                smc = S(es, "smc", [128, 8])
                r_smc = Res("smc")

                class TS:
                    pass
                sets = []
                for i in range(2):
                    ts = TS()
                    if i == 0:
                        ts.xn, ts.bufA, ts.bufB, ts.bufC, ts.kvs = xn0[:], bufA0[:], bufB0[:], bufC0[:], kvs0[:]
                    else:
                        ts.xn, ts.bufA, ts.bufB = rdbuf[:], u_sb[:, 0:640], u_sb[:, 640:1280]
                        ts.bufC, ts.kvs = y_sb[:, 0:640], y_sb[:, 640:1280]
                    ts.sg = S(es, f"sg{i}", [128, 256])
                    ts.vn = S(es, f"vn{i}", [128, 256], BF16)
                    ts.st = S(es, f"st{i}", [128, 6, 6])
                    ts.mv = S(es, f"mv{i}", [128, 5, 2])
                    ts.sm = S(es, f"sm{i}", [128, 48])
                    for nm in ("xn", "bA", "bB", "bC", "kvs", "sg", "vn", "stl", "mvl", "sml", "smq", "sts", "mvs", "sms", "rt"):
                        setattr(ts, "r_" + nm, Res(f"{nm}{i}"))
                    sets.append(ts)
                s0 = sets[0]
                wsf = s0.xn[:, 0:512].rearrange("p (h q) -> p h q", q=128)
                r_wsf = s0.r_xn
                ckf = s0.bufB[:, 0:512].rearrange("p (c d) -> p c d", d=128)
                cvf = s0.bufC[:, 0:512].rearrange("p (c d) -> p c d", d=128)
                r_ckf, r_cvf = s0.r_bB, s0.r_bC
                h2f = u_sb[:, 0:1024].rearrange("p (kc t) -> p kc t", t=128)
                r_h2f = r_u

                DMA("sp", [], [], ropec[:], rc_d.rearrange("(t p) d -> p t d", p=128), pw=[r_rope])
                DMA("sp", [], [], ropes[:], rs_d.rearrange("(t p) d -> p t d", p=128), pw=[r_rope])
                DMA("pool", [], [], wbuf[:, :, 0:768],
                    w_in_d[l, :, 0:768].rearrange("(kc p) n -> p kc n", p=128), pw=[r_wbuf])
                DMA("pool", [], [], wbuf[:, :, 768:1280],
                    w_in_d[l, :, 1536:2048].rearrange("(kc p) n -> p kc n", p=128), pw=[r_wbuf])
                for h in range(8):
                    DMA("pool", [], [], qTall[64:128, h, :], mq_d, pw=r_qT)
                for g in range(2):
                    DMA("pool", [], [], kTall[64:128, g, :], mk_d, pw=r_kT)
                I("dve", "memset", [], r_va, vaug[:, :, :, 64:128], 1.0)
                DMA("sp", [], [], g64[:, 0, :], qg_d[l:l + 1, :].partition_broadcast(128), pw=[r_gv])
                DMA("sp", [], [], g64[:, 1, :], kg_d[l:l + 1, :].partition_broadcast(128), pw=[r_gv])
                DMA("sp", [], [], sgug[:], sgug_d[l:l + 1, :].partition_broadcast(128), pw=[r_gv])
                DMA("sp", [], [r_wsf], wsf, sguw_d[l].rearrange("h p q -> p h q"))
                if moe:
                    DMA("sp", [], [r_rw], rwf[:], rw_d.rearrange("(kc p) e -> p kc e", p=128))

                pb, rpb = bank("a")
                TR([r_wsf, r_id], [rpb], [(pb[:, h * 128:(h + 1) * 128], wsf[:, h, :], identf[:]) for h in range(4)])
                I("dve", "tensor_copy", [rpb], [r_wsT], out=wsT[:].rearrange("p h q -> p (h q)"), in_=pb[:])

                def ln_stats(ts, src_ap, r_src):
                    st, mv, sm = ts.st, ts.mv, ts.sm
                    P.op("dve", [r_src], [ts.r_stl], lambda e, st=st, src_ap=src_ap: [e.bn_stats(out=st[:, 0, :], in_=src_ap[:, 0:512]),
                                                                                      e.bn_stats(out=st[:, 1, :], in_=src_ap[:, 512:1024])][-1])
                    I("dve", "bn_aggr", [ts.r_stl], [ts.r_mvl], out=mv[:, 0, :], in_=st[:, 0:2, :].rearrange("p a b -> p (a b)"))
                    rstd_from_var(mv[:, 0, 1:2], sm[:, 0:1], ts.r_mvl, ts.r_sml)
                    I("dve", "scalar_tensor_tensor", [ts.r_mvl, ts.r_sml], [ts.r_sml], out=sm[:, 1:2], in0=mv[:, 0, 0:1],
                      scalar=-1.0, in1=sm[:, 0:1], op0=ALU.mult, op1=ALU.mult)

                def modulate_transpose(ts, t, slot_shift, slot_scale, src_ap, r_src, want_f32):
                    cnd = 0 if t < 8 else 1
                    xn, r_xn, sm = ts.xn, ts.r_xn, ts.sm
                    ln_stats(ts, src_ap, r_src)
                    I("act", "activation", [r_src, ts.r_sml], [r_xn], out=xn, in_=src_ap, func=AF.Identity,
                      bias=sm[:, 1:2], scale=sm[:, 0:1])
                    pa, rpa = bank("a")
                    pb2, rpb2 = bank("a")
                    TR([r_xn, r_id], [rpa, rpb2],
                       [((pa if kc < 4 else pb2)[:, (kc % 4) * 128:(kc % 4 + 1) * 128], xn[:, kc * 128:(kc + 1) * 128], identf[:])
                        for kc in range(8)])
                    for kc in range(8):
                        src = (pa if kc < 4 else pb2)[:, (kc % 4) * 128:(kc % 4 + 1) * 128]
                        rsrc = rpa if kc < 4 else rpb2
                        sc_ap = modT[:, slot_scale * 8 + kc, cnd:cnd + 1]
                        sh_ap = modT[:, slot_shift * 8 + kc, cnd:cnd + 1]
                        if want_f32:
                            dst, rdst = h2f[:, kc, :], r_h2f
                        else:
                            dst, rdst = hT[:, kc, t * 128:(t + 1) * 128], r_h[t]
                        if kc < 4:
                            I("act", "activation", [rsrc, r_modT], [], out=dst, in_=src, func=AF.Identity,
                              bias=sh_ap, scale=sc_ap, pw=[rdst])
                        else:
                            I("dve", "tensor_scalar", [rsrc, r_modT], [], out=dst, in0=src, scalar1=sc_ap,
                              scalar2=sh_ap, op0=ALU.mult, op1=ALU.add, pw=[rdst])
                    if want_f32:
                        I("dve", "tensor_copy", [r_h2f], [r_h[t]], out=hT[:, :, t * 128:(t + 1) * 128], in_=h2f)

                def stage_env(t):
                    ts = sets[t % 2]
                    return ts, slice(t * 128, (t + 1) * 128)

                def stA(t):
                    ts, tc_ = stage_env(t)
                    modulate_transpose(ts, t, 0, 1, x_sb[:, t, :], r_x[t], False)

                def stB(t):
                    ts, tc_ = stage_env(t)
                    bufA, bufB, bufC, kvs, sg, vn = ts.bufA, ts.bufB, ts.bufC, ts.kvs, ts.sg, ts.vn
                    r_bA, r_bB, r_bC, r_kvs, r_sg, r_vn = ts.r_bA, ts.r_bB, ts.r_bC, ts.r_kvs, ts.r_sg, ts.r_vn
                    st, mv, sm = ts.st, ts.mv, ts.sm
                    pq, rpq = bank("b")
                    MM([r_h[t], r_wbuf], [rpq], [dict(out=pq[:], lhsT=hT[:, kc, tc_], rhs=wbuf[:, kc, 0:512],
                                                      start=(kc == 0), stop=(kc == 7)) for kc in range(8)])
                    pk, rpk = bank("b")
                    MM([r_h[t], r_wbuf], [rpk], [dict(out=pk[:, 0:256], lhsT=hT[:, kc, tc_], rhs=wbuf[:, kc, 512:768],
                                                      start=(kc == 0), stop=(kc == 7)) for kc in range(8)])
                    ps_, rps = bank("b")
                    MM([r_h[t], r_wbuf], [rps], [dict(out=ps_[:], lhsT=hT[:, kc, tc_], rhs=wbuf[:, kc, 768:1280],
                                                      start=(kc == 0), stop=(kc == 7)) for kc in range(8)])
                    I("act", "copy", [rpq], [], out=bufA[:, 0:512], in_=pq[:], pw=[r_bA])
                    I("act", "copy", [rpk], [], out=bufA[:, 512:640], in_=pk[:, 0:128], pw=[r_bA])
                    I("act", "copy", [rpk], [], out=kvs[:, 0:128], in_=pk[:, 128:256], pw=[r_kvs])
                    I("act", "copy", [rps], [], out=kvs[:, 128:640], in_=ps_[:], pw=[r_kvs])
                    DMA("sp", [r_kvs], [], nv_d[l, tc_, :], kvs[:, 0:128], is_output=True)
                    I("dve", "tensor_copy", [r_kvs], [r_va[t]], out=vaug[:, t, :, 0:64],
                      in_=kvs[:, 0:128].rearrange("p (g d) -> p g d", g=2))

                def stC(t):
                    ts, tc_ = stage_env(t)
                    bufA, bufB, bufC, kvs, sg, vn = ts.bufA, ts.bufB, ts.bufC, ts.kvs, ts.sg, ts.vn
                    r_bA, r_bB, r_bC, r_kvs, r_sg, r_vn = ts.r_bA, ts.r_bB, ts.r_bC, ts.r_kvs, ts.r_sg, ts.r_vn
                    st, mv, sm = ts.st, ts.mv, ts.sm
                    smq = sm[:, 8:18]
                    I("act", "activation", [r_bA], [r_bB], out=bufB, in_=bufA, func=AF.Square)
                    I("dve", "reduce_sum", [r_bB], [ts.r_smq], out=smq,
                      in_=bufB.rearrange("p (h d) -> p h d", d=64), axis=AX.X)
                    I("dve", "tensor_scalar", [ts.r_smq], [ts.r_smq], out=smq, in0=smq,
                      scalar1=1.0 / 64, scalar2=None, op0=ALU.mult)
                    rstd_from_var(smq, smq, ts.r_smq, ts.r_smq)
                    A3 = bufA.rearrange("p (h d) -> p h d", d=64)
                    I("dve", "tensor_tensor", [r_bA, ts.r_smq], [r_bA], out=A3, in0=A3,
                      in1=smq.unsqueeze(2).to_broadcast([128, 10, 64]), op=ALU.mult)
                    I("dve", "tensor_tensor", [r_bA, r_gv], [r_bA], out=A3[:, 0:8, :], in0=A3[:, 0:8, :],
                      in1=g64[:, 0:1, :].to_broadcast([128, 8, 64]), op=ALU.mult)
                    I("dve", "tensor_tensor", [r_bA, r_gv], [r_bA], out=A3[:, 8:10, :], in0=A3[:, 8:10, :],
                      in1=g64[:, 1:2, :].to_broadcast([128, 2, 64]), op=ALU.mult)
                    B3 = bufB.rearrange("p (h d) -> p h d", d=64)
                    I("dve", "tensor_tensor", [r_bA, r_rope], [r_bB], out=B3, in0=A3,
                      in1=ropec[:, t:t + 1, :].to_broadcast([128, 10, 64]), op=ALU.mult)
                    A5 = bufA.rearrange("p (h a j f) -> p h a j f", a=2, j=2, f=16)
                    C5 = bufC.rearrange("p (h a j f) -> p h a j f", a=2, j=2, f=16)
                    S5 = ropes[:, t, :].rearrange("p (a j f) -> p a j f", a=2, j=2, f=16)
                    for a in range(2):
                        for j in range(2):
                            I("pool", "tensor_tensor", [r_bA, r_rope], [], out=C5[:, :, a, j, :], in0=A5[:, :, a, 1 - j, :],
                              in1=S5[:, a:a + 1, j, :].to_broadcast([128, 10, 16]), op=ALU.mult, pw=[r_bC])
                    I("dve", "tensor_tensor", [r_bB, r_bC], [r_bA], out=bufA, in0=bufB, in1=bufC, op=ALU.add)
                    DMA("sp", [r_bA], [], nk_d[l, tc_, :], bufA[:, 512:640], is_output=True)
                    sv3 = kvs[:, 384:640].rearrange("p (h d) -> p h d", d=64)
                    sms = sm[:, 20:24]
                    P.op("dve", [r_kvs], [ts.r_sts], lambda e, sv3=sv3, st=st: [e.bn_stats(out=st[:, 2 + h, :], in_=sv3[:, h, :]) for h in range(4)][-1])
                    P.op("dve", [ts.r_sts], [ts.r_mvs], lambda e, mv=mv, st=st: [e.bn_aggr(out=mv[:, 1 + h, :], in_=st[:, 2 + h, :]) for h in range(4)][-1])
                    rstd_from_var(mv[:, 1:5, 1], sms, ts.r_mvs, ts.r_sms)
                    sg3 = sg[:].rearrange("p (h d) -> p h d", d=64)
                    I("dve", "tensor_tensor", [r_kvs, ts.r_mvs], [r_sg], out=sg3, in0=sv3,
                      in1=mv[:, 1:5, 0:1].to_broadcast([128, 4, 64]), op=ALU.subtract)
                    I("dve", "tensor_tensor", [r_sg, ts.r_sms], [r_sg], out=sg3, in0=sg3,
                      in1=sms.unsqueeze(2).to_broadcast([128, 4, 64]), op=ALU.mult)
                    I("dve", "tensor_tensor", [r_sg, r_gv], [r_vn], out=vn[:], in0=sg[:], in1=sgug[:], op=ALU.mult)

                def stD(t):
                    ts, tc_ = stage_env(t)
                    bufA, bufB, bufC, kvs, sg, vn = ts.bufA, ts.bufB, ts.bufC, ts.kvs, ts.sg, ts.vn
                    r_bA, r_bB, r_bC, r_kvs, r_sg, r_vn = ts.r_bA, ts.r_bB, ts.r_bC, ts.r_kvs, ts.r_sg, ts.r_vn
                    pa, rpa = bank("a")
                    pb2, rpb2 = bank("a")
                    TR([r_bA, r_id], [rpa, rpb2],
                       [(pa[:, c * 128:(c + 1) * 128], bufA[:, c * 128:(c + 1) * 128], identf[:]) for c in range(4)]
                       + [(pb2[:, 0:128], bufA[:, 512:640], identf[:])])
                    pa3 = pa[:].rearrange("p (c t) -> p c t", t=128)
                    I("act", "copy", [rpa], [], out=qTall[0:64, 0:8:2, tc_], in_=pa3[0:64, :, :], pw=[r_qT[t]])
                    I("act", "copy", [rpa], [], out=qTall[0:64, 1:8:2, tc_], in_=pa3[64:128, :, :], pw=[r_qT[t]])
                    I("dve", "tensor_copy", [rpb2], [], out=kTall[0:64, 0, tc_], in_=pb2[0:64, 0:128], pw=[r_kT[t]])
                    I("dve", "tensor_copy", [rpb2], [], out=kTall[0:64, 1, tc_], in_=pb2[64:128, 0:128], pw=[r_kT[t]])
                    psg, rpsg = bank("b")
                    MM([r_vn, r_wsT], [rpsg], [dict(out=psg[:, h * 64:(h + 1) * 64], lhsT=wsT[:, h, :], rhs=vn[:, h * 64:(h + 1) * 64],
                                                    start=True, stop=True) for h in range(4)])
                    for h in range(4):
                        I("dve", "scalar_tensor_tensor", [rpsg, r_fm, r_kvs], [r_sg], out=sg[:, h * 64:(h + 1) * 64],
                          in0=psg[:, h * 64:(h + 1) * 64], scalar=fm[:, 56 + h:57 + h],
                          in1=kvs[:, 128 + h * 64:128 + (h + 1) * 64], op0=ALU.add, op1=ALU.mult)
                    pa, rpa = bank("a")
                    TR([r_sg, r_id], [rpa], [(pa[:, c * 128:(c + 1) * 128], sg[:, c * 128:(c + 1) * 128], identf[:]) for c in range(2)])
                    I("act", "copy", [rpa], [], out=mx67[:, :, tc_], in_=pa[:, 0:256].rearrange("p (c t) -> p c t", t=128), pw=[r_h[t]])

                for s_ in range(NT + 3):
                    if 0 <= s_ - 3 < NT:
                        stD(s_ - 3)
                    if 0 <= s_ - 2 < NT:
                        stC(s_ - 2)
                    if 0 <= s_ - 1 < NT:
                        stB(s_ - 1)
                    if s_ < NT:
                        stA(s_)
                DMA("sp", [], [r_ckf], ckf, ck_d[l].rearrange("(c p) d -> p c d", p=128))
                DMA("sp", [], [r_cvf], cvf, cv_d[l].rearrange("(c p) d -> p c d", p=128))
                pb, rpb = bank("a")
                TR([r_ckf, r_id], [rpb], [(pb[:, c * 128:(c + 1) * 128], ckf[:, c, :], identf[:]) for c in range(4)])
                for g in range(2):
                    I("dve", "tensor_copy", [rpb], r_kT[10:14], out=kTall[0:64, g, 1280:1792],
                      in_=pb[g * 64:(g + 1) * 64, :])
                I("dve", "tensor_copy", [r_cvf], r_va[10:14], out=vaug[:, 10:14, :, 0:64],
                  in_=cvf.rearrange("p c (g d) -> p c g d", g=2))
                P.barrier()

                for ch in range(2):
                    cols = {"cin": 768 + ch * 128, "cb": 1024 + ch * 128, "cc": 1280 + ch * 128}
                    DMA("pool", [], [r_wfb], wfb[:, :, 0:128],
                        w_fm_d[l, 0 + ch].rearrange("p (kc n) -> p kc n", kc=8))
                    DMA("pool", [], [r_wfb], wfb[:, :, 128:256],
                        w_fm_d[l, 4 + ch].rearrange("p (kc n) -> p kc n", kc=8))
                    for (g0, gn) in TG:
                        p1, rp1 = bank("b")
                        MM(r_h + [r_wfb], [rp1], [dict(out=p1[:, 0:gn], lhsT=wfb[:, kc, 0:128], rhs=hT[:, kc, g0:g0 + gn],
                                                       start=(kc == 0), stop=(kc == 7)) for kc in range(8)])
                        p2, rp2 = bank("b")
                        MM(r_h + [r_wfb], [rp2], [dict(out=p2[:, 0:gn], lhsT=wfb[:, kc, 128:256], rhs=hT[:, kc, g0:g0 + gn],
                                                       start=(kc == 0), stop=(kc == 7)) for kc in range(8)])
                        I("act", "copy", [rp1], [r_u], out=u_sb[:, g0:g0 + gn], in_=p1[:, 0:gn])
                        I("dve", "tensor_tensor", [rp2, r_u], [r_u], out=u_sb[:, g0:g0 + gn], in0=p2[:, 0:gn],
                          in1=u_sb[:, g0:g0 + gn], op=ALU.mult)
                    w0, w1, w2 = fm[:, 48 + ch:49 + ch], fm[:, 50 + ch:51 + ch], fm[:, 52 + ch:53 + ch]
                    nw0, nw2 = fm[:, 60 + ch:61 + ch], fm[:, 64 + ch:65 + ch]
                    cbias = fm[:, 54 + ch:55 + ch]
                    I("act", "activation", [r_u, r_fm], [r_y], out=y_sb[:], in_=u_sb[:], func=AF.Identity, bias=cbias, scale=w1)
                    I("dve", "scalar_tensor_tensor", [r_u, r_y, r_fm], [r_y], out=y_sb[:, 1:NTOK], in0=u_sb[:, 0:NTOK - 1],
                      scalar=w0, in1=y_sb[:, 1:NTOK], op0=ALU.mult, op1=ALU.add)
                    I("dve", "scalar_tensor_tensor", [r_u, r_y, r_fm], [r_y], out=y_sb[:, 0:NTOK - 1], in0=u_sb[:, 1:NTOK],
                      scalar=w2, in1=y_sb[:, 0:NTOK - 1], op0=ALU.mult, op1=ALU.add)
                    uv = u_sb[:].rearrange("p (a b) -> p a b", b=256)
                    yv = y_sb[:].rearrange("p (a b) -> p a b", b=256)
                    I("dve", "tensor_tensor", [r_u, r_cflag], [r_smc], out=smc[:, 0:4], in0=uv[:, 0:4, 255], in1=cflag[:], op=ALU.mult)
                    I("dve", "scalar_tensor_tensor", [r_smc, r_y, r_fm], [r_y], out=yv[:, 1:5, 0], in0=smc[:, 0:4], scalar=nw0,
                      in1=yv[:, 1:5, 0], op0=ALU.mult, op1=ALU.add)
                    I("dve", "tensor_tensor", [r_u, r_cflag], [r_smc], out=smc[:, 4:8], in0=uv[:, 1:5, 0], in1=cflag[:], op=ALU.mult)
                    I("dve", "scalar_tensor_tensor", [r_smc, r_y, r_fm], [r_y], out=yv[:, 0:4, 255], in0=smc[:, 4:8], scalar=nw2,
                      in1=yv[:, 0:4, 255], op0=ALU.mult, op1=ALU.add)
                    DMA("pool", [], [r_wfb], wfb[:, :, 0:128],
                        w_fm_d[l, 2 + ch].rearrange("p (kc n) -> p kc n", kc=8))
                    pcb = []
                    for (g0, gn) in TG:
                        p1, rp1 = bank("b")
                        MM(r_h + [r_wfb], [rp1], [dict(out=p1[:, 0:gn], lhsT=wfb[:, kc, 0:128], rhs=hT[:, kc, g0:g0 + gn],
                                                       start=(kc == 0), stop=(kc == 7)) for kc in range(8)])
                        pcb.append((p1, rp1, g0, gn))
                    for (p1, rp1, g0, gn) in pcb:
                        I("dve", "tensor_tensor", [rp1, r_y], [], out=mx45[:, ch, g0:g0 + gn], in0=p1[:, 0:gn],
                          in1=y_sb[:, g0:g0 + gn], op=ALU.mult, pw=[r_h[t_] for t_ in range(g0 // 128, (g0 + gn) // 128)])

                DMA("pool", [], [r_wbuf], wbuf[:, :, 0:1024], w_out_d[l].rearrange("(kc p) n -> p kc n", p=128))
                ecnt = [0]
                rcnt = [0]
                LOOK = 3

                def attention(q0, qn, kts):
                    r_q = [r_qT[t] for t in range(q0 // 128, (q0 + qn) // 128)]
                    r_out = [r_h[t] for t in range(q0 // 128, (q0 + qn) // 128)]
                    for h in range(8):
                        g = h // 4
                        po, rpo = bank("b")
                        pend = []
                        n = len(kts)

                        def flush(last):
                            pi, pe_i, pkt = pend.pop(0)
                            MM([r_E[pe_i], r_va[pkt]], [rpo], [dict(out=po[:, 0:qn], lhsT=vaug[:, pkt, g, :], rhs=Eb[pe_i][:, 0:qn],
                                                                    start=(pi == 0), stop=last)])
                        for i, kt in enumerate(kts):
                            psx, rps_ = bank("a")
                            MM(r_q + [r_kT[kt]], [rps_], [dict(out=psx[:, 0:qn], lhsT=kTall[:, g, kt * 128:(kt + 1) * 128],
                                                               rhs=qTall[:, h, q0:q0 + qn], start=True, stop=True)])
                            ei = ecnt[0] % 5
                            ecnt[0] += 1
                            I("act", "activation", [rps_], [r_E[ei]], out=Eb[ei][:, 0:qn], in_=psx[:, 0:qn], func=AF.Exp, scale=SCALE)
                            pend.append((i, ei, kt))
                            if len(pend) > LOOK:
                                flush(False)
                        while pend:
                            flush(len(pend) == 1)
                        ri = rcnt[0] % 2
                        rcnt[0] += 1
                        I("dve", "reciprocal", [rpo], [r_rd[ri]], out=rd[ri][64:128, 0:qn], in_=po[64:128, 0:qn])
                        ph = (h % 2) * 64
                        I("dve", "tensor_tensor", [rpo, r_rd[ri]], [],
                          out=hT[ph:ph + 64, h // 2, q0:q0 + qn], in0=po[0:64, 0:qn], in1=rd[ri][64:128, 0:qn], op=ALU.mult, pw=r_out)

                ktsA = list(range(8)) + [10, 11, 12, 13]
                attention(0, 512, ktsA)
                attention(512, 512, ktsA)
                attention(1024, 256, [8, 9])
                P.barrier()

                e1banks = {}

                def stE1a(t):
                    tc_ = slice(t * 128, (t + 1) * 128)
                    e1banks[t] = []
                    for half in range(2):
                        pw, rpw = bank("b")
                        MM([r_h[t], r_wbuf], [rpw], [dict(out=pw[:], lhsT=mixT(kc)[:, tc_], rhs=wbuf[:, kc, half * 512:(half + 1) * 512],
                                                          start=(kc == 0), stop=(kc == 7)) for kc in range(8)])
                        e1banks[t].append((pw, rpw))

                def stE1(t):
                    ts = sets[t % 2]
                    xn, r_xn, sm = ts.xn, ts.r_xn, ts.sm
                    tc_ = slice(t * 128, (t + 1) * 128)
                    cnd = 0 if t < 8 else 1
                    for half in range(2):
                        pw, rpw = e1banks[t][half]
                        hs = slice(half * 512, (half + 1) * 512)
                        I("dve", "tensor_tensor", [rpw, r_gate[cnd]], [], out=xn[:, hs], in0=pw[:], in1=gate4[:, cnd, hs], op=ALU.mult, pw=[r_xn])
                    I("dve", "scalar_tensor_tensor", [r_x[t], r_xn], [r_x[t]], out=x_sb[:, t, :], in0=x_sb[:, t, :], scalar=ALPHA,
                      in1=xn, op0=ALU.mult, op1=ALU.add)
                    ln_stats(ts, x_sb[:, t, :], r_x[t])
                    I("act", "activation", [r_x[t], ts.r_sml], [r_xn], out=xn, in_=x_sb[:, t, :], func=AF.Identity,
                      bias=sm[:, 1:2], scale=sm[:, 0:1])
                    I("dve", "tensor_tensor", [r_xn, r_lnp[0]], [r_xn], out=xn, in0=xn, in1=lnp[:, 0, :], op=ALU.mult)
                    I("pool", "tensor_tensor", [r_xn, r_lnp[1]], [r_x[t]], out=x_sb[:, t, :], in0=xn, in1=lnp[:, 1, :], op=ALU.add)

                def stE2(t):
                    ts = sets[t % 2]
                    xn, r_xn, sm = ts.xn, ts.r_xn, ts.sm
                    tc_ = slice(t * 128, (t + 1) * 128)
                    cnd = 0 if t < 8 else 1
                    modulate_transpose(ts, t, 3, 4, x_sb[:, t, :], r_x[t], moe)
                    I("act", "mul", [r_x[t]], [r_x[t]], out=x_sb[:, t, :], in_=x_sb[:, t, :], mul=ALPHA)
                    if moe:
                        pr_, rpr = bank("a")
                        MM([r_h2f, r_rw], [rpr], [dict(out=pr_[:, 0:8], lhsT=h2f[:, kc, :], rhs=rwf[:, kc, :],
                                                       start=(kc == 0), stop=(kc == 7)) for kc in range(8)])
                        r_rt = ts.r_rt
                        lg = sm[:, 24:32]
                        m1, m2, dd, e1 = sm[:, 2:3], sm[:, 3:4], sm[:, 4:5], sm[:, 5:6]
                        eq1, eq2, l2 = sm[:, 32:40], sm[:, 40:48], ts.sg[:, 0:8]
                        I("dve", "tensor_copy", [rpr], [r_rt], out=lg, in_=pr_[:, 0:8])
                        I("dve", "reduce_max", [r_rt], [r_rt], out=m1, in_=lg, axis=AX.X)
                        I("dve", "tensor_scalar", [r_rt], [r_rt], out=eq1, in0=lg, scalar1=m1, scalar2=None, op0=ALU.is_equal)
                        I("dve", "scalar_tensor_tensor", [r_rt], [r_rt, ts.r_sg], out=l2, in0=eq1, scalar=-1e30, in1=lg,
                          op0=ALU.mult, op1=ALU.add)
                        I("dve", "reduce_max", [r_rt, ts.r_sg], [r_rt], out=m2, in_=l2, axis=AX.X)
                        I("dve", "tensor_scalar", [r_rt, ts.r_sg], [r_rt], out=eq2, in0=l2, scalar1=m2, scalar2=None, op0=ALU.is_equal)
                        I("dve", "tensor_tensor", [r_rt], [r_rt], out=dd, in0=m2, in1=m1, op=ALU.subtract)
                        I("act", "activation", [r_rt], [r_rt], out=e1, in_=dd, func=AF.Exp)
                        I("dve", "tensor_scalar", [r_rt], [r_rt], out=dd, in0=e1, scalar1=1.0, scalar2=None, op0=ALU.add)
                        I("dve", "reciprocal", [r_rt], [r_rt], out=dd, in_=dd)
                        I("dve", "tensor_tensor", [r_rt], [r_rt], out=e1, in0=e1, in1=dd, op=ALU.mult)
                        I("dve", "tensor_scalar", [r_rt], [r_G[t]], out=G[:, t, :], in0=eq1, scalar1=dd, scalar2=None, op0=ALU.mult)
                        I("dve", "scalar_tensor_tensor", [r_rt, r_G[t]], [r_G[t]], out=G[:, t, :], in0=eq2, scalar=e1,
                          in1=G[:, t, :], op0=ALU.mult, op1=ALU.add)

                for s_ in range(NT + 1):
                    if s_ < NT:
                        stE1a(s_)
                    if 0 <= s_ - 1 < NT:
                        stE2(s_ - 1)
                    if s_ < NT:
                        stE1(s_)
            P.barrier()

            with ExitStack() as es:
                w1c = [S(es, f"w1c{i}", [128, 8, 384], BF16) for i in range(3)]
                w3c = [S(es, f"w3c{i}", [128, 8, 384], BF16) for i in range(3)]
                r_w1c = [Res(f"w1c{i}") for i in range(3)]
                r_w3c = [Res(f"w3c{i}") for i in range(3)]
                w2b = S(es, "w2b", [128, 11, D], BF16)
                r_w2b = Res("w2b")
                gT = S(es, "gT", [128, 11, NTOK], BF16)
                r_gT = [Res(f"gT{g}") for g in range(3)]
                sa = [S(es, f"sa{i}", [128, 512]) for i in range(2)]
                r_sa = [Res(f"sa{i}") for i in range(2)]
                tmp = [S(es, f"tmp{i}", [128, 512]) for i in range(2)]
                r_tmp = [Res(f"tmp{i}") for i in range(2)]
                xn = S(es, "xn2", [128, D])
                r_xn = Res("xn2")
                st = S(es, "st2", [128, 2, 6])
                mv = S(es, "mv2", [128, 2])
                sm = S(es, "sm2", [128, 4])
                r_st, r_mv, r_sm = Res("st2"), Res("mv2"), Res("sm2")

                if moe:
                    experts = [(mw1_d[e], mw3_d[e], mw2_d[e], e) for e in range(8)]
                else:
                    experts = [(fw1_d[e], fw3_d[e], fw2_d[e * 1408:(e + 1) * 1408, :], None) for e in range(2)]
                chunks = [(0, 384), (384, 384), (768, 384), (1152, 256)]
                cidx = 0
                sidx = 0
                tidx = 0
                for (w1_ap, w3_ap, w2_ap, ge) in experts:
                    DMA("pool", [], [r_w2b], w2b[:], w2_ap.rearrange("(kc p) n -> p kc n", p=128))
                    for (c0, cw) in chunks:
                        bi = cidx % 3
                        cidx += 1
                        DMA("pool", [], [r_w1c[bi]], w1c[bi][:, :, 0:cw], w1_ap[:, 8 * c0:8 * (c0 + cw)].rearrange("p (kc n) -> p kc n", kc=8))
                        DMA("pool", [], [r_w3c[bi]], w3c[bi][:, :, 0:cw], w3_ap[:, 8 * c0:8 * (c0 + cw)].rearrange("p (kc n) -> p kc n", kc=8))
                        for sub in range(cw // 128):
                            fc = c0 // 128 + sub
                            for gi_, (g0, gn) in enumerate(TG):
                                rh = [r_h[t] for t in range(g0 // 128, (g0 + gn) // 128)]
                                pa_, rpa_ = bank("a")
                                MM(rh + [r_w1c[bi]], [rpa_], [dict(out=pa_[:, 0:gn], lhsT=w1c[bi][:, kc, sub * 128:(sub + 1) * 128],
                                                                   rhs=hT[:, kc, g0:g0 + gn], start=(kc == 0), stop=(kc == 7)) for kc in range(8)])
                                pb_, rpb_ = bank("b")
                                MM(rh + [r_w3c[bi]], [rpb_], [dict(out=pb_[:, 0:gn], lhsT=w3c[bi][:, kc, sub * 128:(sub + 1) * 128],
                                                                   rhs=hT[:, kc, g0:g0 + gn], start=(kc == 0), stop=(kc == 7)) for kc in range(8)])
                                si = sidx % 2
                                sidx += 1
                                I("act", "activation", [rpa_], [r_sa[si]], out=sa[si][:, 0:gn], in_=pa_[:, 0:gn], func=AF.Silu)
                                I("dve", "tensor_tensor", [r_sa[si], rpb_], [], out=gT[:, fc, g0:g0 + gn], in0=sa[si][:, 0:gn],
                                  in1=pb_[:, 0:gn], op=ALU.mult, pw=[r_gT[gi_]])
                    for t in range(NT):
                        cnd = 0 if t < 8 else 1
                        tc_ = slice(t * 128, (t + 1) * 128)
                        for half in range(2):
                            hs = slice(half * 512, (half + 1) * 512)
                            pd, rpd = bank("a" if (t * 2 + half) % 2 == 0 else "b")
                            MM([r_gT[min(t // 4, 2)], r_w2b], [rpd], [dict(out=pd[:], lhsT=gT[:, kc, tc_], rhs=w2b[:, kc, hs],
                                                                          start=(kc == 0), stop=(kc == 10)) for kc in range(11)])
                            ti = tidx % 2
                            tidx += 1
                            if ge is None:
                                I("dve", "tensor_tensor", [rpd, r_gate[2 + cnd]], [r_tmp[ti]], out=tmp[ti][:], in0=pd[:],
                                  in1=gate4[:, 2 + cnd, hs], op=ALU.mult)
                            else:
                                I("act", "activation", [rpd, r_G[t]], [r_tmp[ti]], out=tmp[ti][:], in_=pd[:], func=AF.Identity,
                                  scale=G[:, t, ge:ge + 1])
                                I("dve", "tensor_tensor", [r_tmp[ti], r_gate[2 + cnd]], [r_tmp[ti]], out=tmp[ti][:], in0=tmp[ti][:],
                                  in1=gate4[:, 2 + cnd, hs], op=ALU.mult)
                            I("dve", "tensor_tensor", [r_tmp[ti], r_x[t]], [r_x[t]], out=x_sb[:, t, hs], in0=x_sb[:, t, hs],
                              in1=tmp[ti][:], op=ALU.add)
                for t in range(NT):
                    src = x_sb[:, t, :]
                    P.op("dve", [r_x[t]], [r_st], lambda e, src=src, st=st: [e.bn_stats(out=st[:, 0, :], in_=src[:, 0:512]),
                                                                       e.bn_stats(out=st[:, 1, :], in_=src[:, 512:1024])][-1])
                    I("dve", "bn_aggr", [r_st], [r_mv], out=mv[:], in_=st[:].rearrange("p a b -> p (a b)"))
                    rstd_from_var(mv[:, 1:2], sm[:, 0:1], r_mv, r_sm)
                    I("dve", "scalar_tensor_tensor", [r_mv, r_sm], [r_sm], out=sm[:, 1:2], in0=mv[:, 0:1],
                      scalar=-1.0, in1=sm[:, 0:1], op0=ALU.mult, op1=ALU.mult)
                    I("act", "activation", [r_x[t], r_sm], [r_xn], out=xn[:], in_=src, func=AF.Identity, bias=sm[:, 1:2], scale=sm[:, 0:1])
                    I("dve", "tensor_tensor", [r_xn, r_lnp[2]], [r_xn], out=xn[:], in0=xn[:], in1=lnp[:, 2, :], op=ALU.mult)
                    I("pool", "tensor_tensor", [r_xn, r_lnp[3]], [r_x[t]], out=src, in0=xn[:], in1=lnp[:, 3, :], op=ALU.add)
                    if last:
                        DMA("sp", [r_x[t]], [], y_d[t * 128:(t + 1) * 128, :], src, is_output=True)
            P.barrier()

        P.emit()
    return nc


def _rope_tables():
    n = 1024
    rows = n // 64
    row = np.repeat(np.arange(rows, dtype=np.float32), 64)
    col = np.tile(np.arange(64, dtype=np.float32), rows)
    inv_freq = (np.float32(10000.0) ** (-np.arange(0, 32, 2, dtype=np.float32) / np.float32(32))).astype(np.float32)
    ang = np.stack([row[:, None] * inv_freq, col[:, None] * inv_freq], axis=1).astype(np.float32)
    cos, sin = np.cos(ang).astype(np.float32), np.sin(ang).astype(np.float32)
    cosE = np.stack([cos, cos], axis=2).reshape(n, 64)
    sinE = np.stack([-sin, sin], axis=2).reshape(n, 64)
    return cosE.astype(np.float32), sinE.astype(np.float32)


def _core_inputs(c, inp, shared):
    xs, xp = inp["x_sample"], inp["x_prompt"]
    cosE, sinE = shared["rope"]
    rc = np.ones((NTOK, 64), np.float32)
    rs = np.zeros((NTOK, 64), np.float32)
    ids_q = np.zeros(NTOK, np.int64)
    ids_k = np.zeros(1792, np.int64)
    if c < 2:
        x = np.concatenate([xs[c], xp[c]], axis=0)
        cond = np.stack([inp["c"][c], inp["c_ctx"]])
        ck = inp["cache_k"][c].reshape(DEPTH, 512, 128)
        cv = inp["cache_v"][c].reshape(DEPTH, 512, 128)
        rc[:1024], rs[:1024] = cosE, sinE
        ids_q[1024:] = 1
        ids_k[:NTOK] = ids_q
        ids_k[NTOK:] = 0
        cflag = np.tile(np.array([0, 0, 0, 1], np.float32), (128, 1))
    else:
        p0 = 2 + 5 * (c - 2)
        x = xp[p0:p0 + 5].reshape(NTOK, D)
        cond = np.stack([inp["c_ctx"], inp["c_ctx"]])
        ck = np.zeros((DEPTH, 512, 128), np.float32)
        cv = np.zeros((DEPTH, 512, 128), np.float32)
        ids_q = np.arange(NTOK) // 256
        ids_k[:NTOK] = ids_q
        ids_k[NTOK:] = 63
        cflag = np.ones((128, 4), np.float32)
    mk = (np.arange(64)[:, None] == ids_k[None, :]).astype(np.float32)
    mq = np.where(np.arange(64)[:, None] == ids_q[None, :], 0.0, NEG).astype(np.float32)
    m = dict(shared["weights"])
    m.update({
        "x": np.ascontiguousarray(x, dtype=np.float32),
        "cond": np.ascontiguousarray(cond.reshape(16, 128), dtype=np.float32),
        "ck": np.ascontiguousarray(ck, dtype=np.float32), "cv": np.ascontiguousarray(cv, dtype=np.float32),
        "mq": mq, "mk": mk, "ropec": rc, "ropes": rs, "cflag": cflag,
        "ident": np.eye(128, dtype=np.float32),
    })
    return m


_NC_CACHE = {}


def kernel(**inp):
    inp = {k: np.asarray(v) for k, v in inp.items()}
    f = lambda a: np.ascontiguousarray(a, dtype=np.float32)
    def tile_cols(w):
        E = w.shape[0]
        parts = []
        for c0, cw in ((0, 384), (384, 384), (768, 384), (1152, 256)):
            blk = w[:, :, c0:c0 + cw].reshape(E, 8, 128, cw).transpose(0, 2, 1, 3).reshape(E, 128, 8 * cw)
            parts.append(blk)
        return f(np.concatenate(parts, axis=2))

    ada_t = inp["ada_w"].reshape(DEPTH, 8, 128, 12, 512).transpose(0, 3, 2, 1, 4).reshape(DEPTH, 12, 128, 4096)
    wfm_t = inp["w_in"][:, :, 768:1536].reshape(DEPTH, 8, 128, 6, 128).transpose(0, 3, 2, 1, 4).reshape(DEPTH, 6, 128, 1024)
    fw1 = inp["ffn_w1"][0].reshape(D, 2, 1408).transpose(1, 0, 2)
    fw3 = inp["ffn_w3"][0].reshape(D, 2, 1408).transpose(1, 0, 2)
    weights = {
        "ada_w": f(ada_t), "ada_b": f(inp["ada_b"].reshape(DEPTH, 48, 128)),
        "w_in": f(inp["w_in"]), "w_in_fm": f(wfm_t), "q_g": f(inp["q_norm_g"]), "k_g": f(inp["k_norm_g"]),
        "conv_w": f(inp["conv_w"].reshape(DEPTH, 6, 128)), "conv_b": f(inp["conv_b"].reshape(DEPTH, 2, 128)),
        "sgu_g": f(inp["sgu_norm_g"]), "sgu_w": f(inp["sgu_w"]), "sgu_b": f(inp["sgu_b"]),
        "w_out": f(inp["w_out"]), "ln1_g": f(inp["ln1_g"]), "ln1_b": f(inp["ln1_b"]),
        "ln2_g": f(inp["ln2_g"]), "ln2_b": f(inp["ln2_b"]),
        "ffn_w1": tile_cols(fw1), "ffn_w3": tile_cols(fw3), "ffn_w2": f(inp["ffn_w2"][0]),
        "router_w": f(inp["router_w"][0]), "moe_w1": tile_cols(inp["moe_w1"][0]), "moe_w3": tile_cols(inp["moe_w3"][0]),
        "moe_w2": f(inp["moe_w2"][0]),
    }
    shared = {"weights": weights, "rope": _rope_tables()}
    in_maps = [_core_inputs(c, inp, shared) for c in range(8)]
    if "nc" not in _NC_CACHE:
        _NC_CACHE["nc"] = build_program()
    res = run_bass_kernel_spmd(_NC_CACHE["nc"], in_maps, core_ids=list(range(8)))
    R = res.results
    y_p = np.zeros((32, 256, D), np.float32)
    y_s = np.zeros((2, 1024, D), np.float32)
    new_k = np.zeros((32, DEPTH, 256, 2, 64), np.float32)
    new_v = np.zeros((32, DEPTH, 256, 2, 64), np.float32)
    for c in range(8):
        y, nk, nv = R[c]["y"], R[c]["nk"], R[c]["nv"]
        if c < 2:
            y_s[c] = y[:1024]
            y_p[c] = y[1024:]
            new_k[c] = nk[:, 1024:].reshape(DEPTH, 256, 2, 64)
            new_v[c] = nv[:, 1024:].reshape(DEPTH, 256, 2, 64)
        else:
            p0 = 2 + 5 * (c - 2)
            y_p[p0:p0 + 5] = y.reshape(5, 256, D)
            new_k[p0:p0 + 5] = nk.reshape(DEPTH, 5, 256, 2, 64).transpose(1, 0, 2, 3, 4)
            new_v[p0:p0 + 5] = nv.reshape(DEPTH, 5, 256, 2, 64).transpose(1, 0, 2, 3, 4)
    return (y_p, y_s, new_k, new_v)
```

```python
from contextlib import ExitStack
import numpy as np
import concourse.bass as bass
import concourse.mybir as mybir
from concourse.bass_utils import run_bass_kernel_spmd

F32 = mybir.dt.float32
BF16 = mybir.dt.bfloat16
AF = mybir.ActivationFunctionType
ALU = mybir.AluOpType
AX = mybir.AxisListType

D = 1024
NT = 10
NTOK = 1280
DEPTH = 2
EPS = 1e-6
ALPHA = (2 * DEPTH) ** 0.25
SCALE = 64 ** -0.5
NEG = -30000.0
TG = [(0, 512), (512, 512), (1024, 256)]


class Res:
    __slots__ = ("name", "last_w", "readers", "par_w", "psum")

    def __init__(self, name="", psum=False):
        self.name = name
        self.psum = psum
        self.last_w = None
        self.readers = []
        self.par_w = []


class Prog:
    COMPUTE = ("pe", "act", "dve", "pool")
    NQ = 8

    def __init__(self, nc):
        self.nc = nc
        self.eng = {"pe": nc.tensor, "act": nc.scalar, "dve": nc.vector,
                    "pool": nc.gpsimd, "sp": nc.sync}
        self.count = {e: 0 for e in self.COMPUTE}
        self.ops = {e: [] for e in self.eng}
        self.seen = {e: {} for e in self.eng}
        self.semobj = {}
        self.dsem = {}
        self.dcount = {}
        self.dnext = {}
        for e in self.COMPUTE:
            self.semobj[f"prog_{e}"] = nc.alloc_semaphore(name=f"prog_{e}")
        for q in ("sp", "pool"):
            self.dsem[q] = []
            for i in range(self.NQ):
                nm = f"dma_{q}_{i}"
                self.semobj[nm] = nc.alloc_semaphore(name=nm)
                self.dsem[q].append(nm)
                self.dcount[nm] = 0
            self.dnext[q] = 0
        self.final = []
        self.extra = {e: [] for e in self.eng}

    def _need(self, waits, tok):
        if tok is None:
            return
        s, v = tok
        if waits.get(s, 0) < v:
            waits[s] = v

    def _deps(self, e, reads, writes, pwrites=()):
        waits = {}
        for s, v in self.extra[e]:
            self._need(waits, (s, v))
        self.extra[e] = []
        for r in reads:
            self._need(waits, r.last_w)
            for t in r.par_w:
                self._need(waits, t)
        for w in writes:
            self._need(waits, w.last_w)
            for t in w.par_w:
                self._need(waits, t)
            for t in w.readers:
                self._need(waits, t)
        for w in pwrites:
            self._need(waits, w.last_w)
            for t in w.readers:
                self._need(waits, t)
        out = []
        for s, v in waits.items():
            if e == "pe" and s == "prog_pe":
                continue
            if self.seen[e].get(s, 0) >= v:
                continue
            self.seen[e][s] = v
            out.append((s, v))
        return out

    def _commit(self, tok, reads, writes, pwrites=()):
        for r in reads:
            r.readers.append(tok)
        for w in writes:
            w.last_w = tok
            w.readers = []
            w.par_w = []
        for w in pwrites:
            w.par_w.append(tok)

    PAR = True

    def op(self, e, reads, writes, fn, pwrites=()):
        if not self.PAR:
            writes, pwrites = list(writes) + list(pwrites), ()
        if e != "pe":
            extra_w = [r for r in reads if r.psum and r not in writes]
            if extra_w:
                writes = list(writes) + extra_w
        waits = self._deps(e, reads, writes, pwrites)
        self.count[e] += 1
        tok = (f"prog_{e}", self.count[e])
        self.ops[e].append((waits, fn, tok))
        self._commit(tok, reads, writes, pwrites)
        return tok

    def dma(self, q, reads, writes, fn, is_output=False, pwrites=()):
        i = self.dnext[q]
        self.dnext[q] = (i + 1) % self.NQ
        nm = self.dsem[q][i]
        waits = self._deps(q, reads, writes, pwrites)
        prev = self.dcount[nm]
        if prev > 0 and self.seen[q].get(nm, 0) < prev:
            self.seen[q][nm] = prev
            waits.append((nm, prev))
        self.dcount[nm] = prev + 16
        tok = (nm, prev + 16)
        self.ops[q].append((waits, fn, tok))
        self._commit(tok, reads, writes, pwrites)
        if is_output:
            self.final.append(tok)
        return tok

    def barrier(self):
        toks = [(f"prog_{e}", self.count[e]) for e in self.COMPUTE if self.count[e] > 0]
        toks += [(nm, v) for nm, v in self.dcount.items() if v > 0]
        for e in self.eng:
            self.extra[e] = list(toks)

    def emit(self):
        nc = self.nc
        fin = {}
        for s, v in self.final:
            fin[s] = max(fin.get(s, 0), v)

        def run(e, engine):
            for waits, fn, tok in self.ops[e]:
                for s, v in waits:
                    engine.wait_ge(self.semobj[s], v)
                ins = fn(engine)
                s, v = tok
                ins.then_inc(self.semobj[s], 1 if s.startswith("prog_") else 16)
            if e == "sp":
                for s, v in fin.items():
                    engine.wait_ge(self.semobj[s], v)

        with nc.Block() as block:
            @block.tensor
            def _(eng):
                run("pe", eng)

            @block.scalar
            def _(eng):
                run("act", eng)

            @block.vector
            def _(eng):
                run("dve", eng)

            @block.gpsimd
            def _(eng):
                run("pool", eng)

            @block.sync
            def _(eng):
                run("sp", eng)


def build_program(n_layers=DEPTH):
    nc = bass.Bass("TRN2", target_bir_lowering=False)

    def din(name, shape):
        return nc.dram_tensor(name, list(shape), F32, kind="ExternalInput").ap()

    def dout(name, shape):
        return nc.dram_tensor(name, list(shape), F32, kind="ExternalOutput").ap()

    x_d = din("x", [NTOK, D])
    cond_d = din("cond", [16, 128])
    ck_d = din("ck", [DEPTH, 512, 128])
    cv_d = din("cv", [DEPTH, 512, 128])
    mq_d = din("mq", [64, NTOK])
    mk_d = din("mk", [64, 1792])
    rc_d = din("ropec", [NTOK, 64])
    rs_d = din("ropes", [NTOK, 64])
    cflag_d = din("cflag", [128, 4])
    ident_d = din("ident", [128, 128])
    ada_w_d = din("ada_w", [DEPTH, 12, 128, 4096])
    ada_b_d = din("ada_b", [DEPTH, 48, 128])
    w_in_d = din("w_in", [DEPTH, D, 2048])
    w_fm_d = din("w_in_fm", [DEPTH, 6, 128, 1024])
    qg_d = din("q_g", [DEPTH, 64])
    kg_d = din("k_g", [DEPTH, 64])
    convw_d = din("conv_w", [DEPTH, 6, 128])
    convb_d = din("conv_b", [DEPTH, 2, 128])
    sgug_d = din("sgu_g", [DEPTH, 256])
    sguw_d = din("sgu_w", [DEPTH, 4, 128, 128])
    sgub_d = din("sgu_b", [DEPTH, 4, 128])
    w_out_d = din("w_out", [DEPTH, D, D])
    ln1g_d = din("ln1_g", [DEPTH, D])
    ln1b_d = din("ln1_b", [DEPTH, D])
    ln2g_d = din("ln2_g", [DEPTH, D])
    ln2b_d = din("ln2_b", [DEPTH, D])
    fw1_d = din("ffn_w1", [2, 128, 8 * 1408])
    fw3_d = din("ffn_w3", [2, 128, 8 * 1408])
    fw2_d = din("ffn_w2", [2816, D])
    rw_d = din("router_w", [D, 8])
    mw1_d = din("moe_w1", [8, 128, 8 * 1408])
    mw3_d = din("moe_w3", [8, 128, 8 * 1408])
    mw2_d = din("moe_w2", [8, 1408, D])

    y_d = dout("y", [NTOK, D])
    nk_d = dout("nk", [DEPTH, NTOK, 128])
    nv_d = dout("nv", [DEPTH, NTOK, 128])

    P = Prog(nc)

    def I(eng, method, reads, writes, *a, pw=(), **kw):
        return P.op(eng, reads, writes, lambda e: getattr(e, method)(*a, **kw), pwrites=pw)

    def MM(reads, writes, lst):
        return P.op("pe", reads, writes, lambda e: [e.matmul(**kw) for kw in lst][-1])

    def TR(reads, writes, lst, pw=()):
        return P.op("pe", reads, writes, lambda e: [e.transpose(*a) for a in lst][-1], pwrites=pw)

    def DMA(q, reads, writes, out, in_, is_output=False, pw=()):
        return P.dma(q, reads, writes, lambda e: e.dma_start(out=out, in_=in_), is_output=is_output, pwrites=pw)

    with ExitStack() as top:
        uid = [0]

        def S(es, name, shape, dt=F32):
            uid[0] += 1
            return es.enter_context(nc.sbuf_tensor(f"s{uid[0]}_{name}", list(shape), dt))

        banks = [top.enter_context(nc.psum_tensor(f"bank{i}", [128, 512], F32)) for i in range(8)]
        rbank = [Res(f"bank{i}", psum=True) for i in range(8)]
        pool_ptr = {"a": 0, "b": 0}

        def bank(pool):
            i = pool_ptr[pool]
            pool_ptr[pool] = (i + 1) % 4
            j = i if pool == "a" else 4 + i
            return banks[j], rbank[j]

        x_sb = S(top, "x_sb", [128, NT, D])
        r_x = [Res(f"x{t}") for t in range(NT)]
        hT = S(top, "hT", [128, 8, NTOK], BF16)
        r_h = [Res(f"hT{t}") for t in range(NT)]
        mx67 = S(top, "mx67", [128, 2, NTOK], BF16)
        mx45 = S(top, "mx45", [128, 2, NTOK], BF16)
        gate4 = S(top, "gate4", [128, 4, D])
        r_gate = [Res(f"gate{i}") for i in range(4)]
        lnp = S(top, "lnp", [128, 4, D])
        r_lnp = [Res(f"lnp{i}") for i in range(4)]
        modT = S(top, "modT", [128, 48, 2])
        r_modT = Res("modT")
        fm = S(top, "fm", [128, 72])
        r_fm = Res("fm")
        identf = S(top, "identf", [128, 128])
        r_id = Res("ident")
        cflag = S(top, "cflag", [128, 4])
        r_cflag = Res("cflag")
        condT = S(top, "condT", [128, 16])
        scTb = S(top, "scTb", [128, 16], BF16)
        scAB = S(top, "scAB", [128, 8, 2], BF16)
        r_sc = Res("sc")
        G = S(top, "G", [128, NT, 8])
        r_G = [Res(f"G{t}") for t in range(NT)]
        GQ2 = S(top, "GQ2", [128, NT, 8])
        DDt = S(top, "DDt", [128, 4, NT])
        r_DD = Res("DDt")
        epsc = S(top, "epsc", [128, 1])
        r_eps = Res("eps")

        def mixT(kc):
            if kc < 4:
                return hT[:, kc, :]
            return mx45[:, kc - 4, :] if kc < 6 else mx67[:, kc - 6, :]

        DMA("sp", [], [r_id], identf[:], ident_d)
        DMA("sp", [], [r_cflag], cflag[:], cflag_d)
        I("dve", "memset", [], [r_eps], epsc[:], EPS)

        def rstd_from_var(var_ap, out_ap, r_in, r_out):
            I("act", "activation", [r_in, r_eps], [r_out], out=out_ap, in_=var_ap, func=AF.Sqrt,
              bias=epsc[:, 0:1], scale=1.0)
            I("dve", "reciprocal", [r_out], [r_out], out=out_ap, in_=out_ap)

        with ExitStack() as es0:
            stg = S(es0, "stg0", [16, 128])
            r_stg = Res("stg0")
            DMA("sp", [], [r_stg], stg[:], cond_d)
            I("act", "activation", [r_stg], [r_stg], out=stg[:], in_=stg[:], func=AF.Silu)
            pb, rpb = bank("a")
            TR([r_stg, r_id], [rpb], [(pb[:, 0:16], stg[:], identf[0:16, 0:16])])
            I("dve", "tensor_copy", [rpb], [r_sc], out=condT[:], in_=pb[:, 0:16])
            I("dve", "tensor_copy", [r_sc], [r_sc], out=scTb[:], in_=condT[:])
            I("dve", "tensor_copy", [r_sc], [r_sc], out=scAB[:].rearrange("p k c -> p c k"),
              in_=condT[:].rearrange("p (c k) -> p c k", k=8))
            for t in range(NT):
                DMA("sp", [], [r_x[t]], x_sb[:, t, :], x_d[t * 128:(t + 1) * 128, :])
        P.barrier()

        for l in range(n_layers):
            moe = (l % 2 == 1)
            last = (l == n_layers - 1)

            with ExitStack() as es:
                awb = [S(es, f"awb{i}", [128, 8, 512], BF16) for i in range(3)]
                r_awb = [Res(f"awb{i}") for i in range(3)]
                rowsb = [S(es, f"rowsb{i}", [2, 512]) for i in range(2)]
                r_rows = [Res(f"rows{i}") for i in range(2)]
                gbias = [S(es, f"gbias{i}", [128, 512]) for i in range(2)]
                r_gb = [Res(f"gb{i}") for i in range(2)]
                scRep = S(es, "scRep", [128, 16, 128], BF16)
                r_scRep = Res("scRep")
                stg = S(es, "stgl", [64, 128])
                r_stg = Res("stgl")
                I("dve", "tensor_copy", [r_sc], [r_scRep], out=scRep[:],
                  in_=scTb[:].unsqueeze(2).to_broadcast([128, 16, 128]))
                DMA("sp", [], [], stg[0:48, :], ada_b_d[l], pw=[r_stg])
                DMA("sp", [], [], stg[48:54, :], convw_d[l], pw=[r_stg])
                DMA("sp", [], [], stg[54:56, :], convb_d[l], pw=[r_stg])
                DMA("sp", [], [], stg[56:60, :], sgub_d[l], pw=[r_stg])
                pb, rpb = bank("a")
                TR([r_stg, r_id], [rpb], [(pb[:, 0:60], stg[0:60, :], identf[0:60, 0:60])])
                I("dve", "tensor_copy", [rpb], [r_fm], out=fm[:, 0:60], in_=pb[:, 0:60])
                I("dve", "tensor_scalar", [r_fm], [r_fm], out=fm[:, 60:66], in0=fm[:, 48:54],
                  scalar1=-1.0, scalar2=None, op0=ALU.mult)
                for i, dd in enumerate((ln1g_d, ln1b_d, ln2g_d, ln2b_d)):
                    DMA("sp", [], [r_lnp[i]], lnp[:, i, :], dd[l:l + 1, :].partition_broadcast(128))

                pfm, rpfm = bank("b")
                gi = 0
                for cc in range(12):
                    slot, half = cc // 2, cc % 2
                    bi = cc % 3
                    DMA("pool", [], [r_awb[bi]], awb[bi][:],
                        ada_w_d[l, cc].rearrange("p (kc n) -> p kc n", kc=8))
                    prow, rprow = bank("a")
                    MM([r_awb[bi], r_sc], [rprow], [dict(out=prow[0:2, :], lhsT=scAB[:, kc, :], rhs=awb[bi][:, kc, :],
                                                        start=(kc == 0), stop=(kc == 7)) for kc in range(8)])
                    I("act", "copy", [rprow], [r_rows[cc % 2]], out=rowsb[cc % 2][0:2, :], in_=prow[0:2, :])
                    TR([r_rows[cc % 2], r_id], [], [(pfm[:, 2 * (cc * 4 + oc):2 * (cc * 4 + oc) + 2],
                                                    rowsb[cc % 2][0:2, oc * 128:(oc + 1) * 128], identf[0:2, 0:2]) for oc in range(4)], pw=[rpfm])
                    if slot in (2, 5):
                        gslot = 0 if slot == 2 else 2
                        DMA("sp", [], [r_gb[gi % 2]], gbias[gi % 2][:],
                            ada_b_d[l, cc * 4:(cc + 1) * 4, :].rearrange("(o a) b -> o (a b)", o=1).partition_broadcast(128))
                        for cnd in range(2):
                            pr_, rpr = bank("a")
                            MM([r_awb[bi], r_scRep], [rpr],
                               [dict(out=pr_[:], lhsT=scRep[:, cnd * 8 + kc, :], rhs=awb[bi][:, kc, :],
                                     start=(kc == 0), stop=(kc == 7)) for kc in range(8)])
                            I("dve", "tensor_tensor", [rpr, r_gb[gi % 2]], [r_gate[gslot + cnd]],
                              out=gate4[:, gslot + cnd, half * 512:(half + 1) * 512], in0=pr_[:],
                              in1=gbias[gi % 2][:], op=ALU.add)
                        gi += 1
                I("dve", "tensor_tensor", [rpfm, r_fm], [r_modT], out=modT[:],
                  in0=pfm[:, 0:96].rearrange("p (c k) -> p c k", k=2),
                  in1=fm[:, 0:48].unsqueeze(2).to_broadcast([128, 48, 2]), op=ALU.add)
                for s_ in (1, 4):
                    I("dve", "tensor_scalar", [r_modT], [r_modT], out=modT[:, s_ * 8:(s_ + 1) * 8, :],
                      in0=modT[:, s_ * 8:(s_ + 1) * 8, :], scalar1=1.0, scalar2=None, op0=ALU.add)
            P.barrier()

            with ExitStack() as es:
                ropec = S(es, "ropec", [128, NT, 64])
                ropes = S(es, "ropes", [128, NT, 64])
                r_rope = Res("rope")
                qTall = S(es, "qTall", [128, 8, NTOK], BF16)
                r_qT = [Res(f"qT{t}") for t in range(NT)]
                kTall = S(es, "kTall", [128, 2, 1792], BF16)
                r_kT = [Res(f"kT{t}") for t in range(14)]
                vaug = S(es, "vaug", [128, 14, 2, 128], BF16)
                r_va = [Res(f"va{t}") for t in range(14)]
                wbuf = S(es, "wbuf", [128, 8, 1280], BF16)
                r_wbuf = Res("wbuf")
                wfb = S(es, "wfb", [128, 8, 256], BF16)
                r_wfb = Res("wfb")
                u_sb = S(es, "u_sb", [128, NTOK])
                y_sb = S(es, "y_sb", [128, NTOK])
                r_u, r_y = Res("u"), Res("y")
                xn0 = S(es, "xn", [128, D])
                rdbuf = S(es, "rdbuf", [128, D])
                bufA0 = S(es, "bufA", [128, 640])
                bufB0 = S(es, "bufB", [128, 640])
                bufC0 = S(es, "bufC", [128, 640])
                kvs0 = S(es, "kvs", [128, 640])
                Eb = [S(es, f"E{i}", [128, 512], BF16) for i in range(5)]
                r_E = [Res(f"E{i}") for i in range(5)]
                rd = [rdbuf[:, 0:512], rdbuf[:, 512:1024]]
                r_rd = [Res(f"rd{i}") for i in range(2)]
                g64 = S(es, "g64", [128, 2, 64])
                sgug = S(es, "sgug", [128, 256])
                r_gv = Res("gv")
                wsT = S(es, "wsT", [128, 4, 128], BF16)
                r_wsT = Res("wsT")
                rwf = S(es, "rwf", [128, 8, 8])
                r_rw = Res("rw")
                smc = S(es, "smc", [128, 8])
                r_smc = Res("smc")

                class TS:
                    pass
                sets = []
                for i in range(2):
                    ts = TS()
                    if i == 0:
                        ts.xn, ts.bufA, ts.bufB, ts.bufC, ts.kvs = xn0[:], bufA0[:], bufB0[:], bufC0[:], kvs0[:]
                    else:
                        ts.xn, ts.bufA, ts.bufB = rdbuf[:], u_sb[:, 0:640], u_sb[:, 640:1280]
                        ts.bufC, ts.kvs = y_sb[:, 0:640], y_sb[:, 640:1280]
                    ts.sg = S(es, f"sg{i}", [128, 256])
                    ts.vn = S(es, f"vn{i}", [128, 256], BF16)
                    ts.st = S(es, f"st{i}", [128, 6, 6])
                    ts.mv = S(es, f"mv{i}", [128, 5, 2])
                    ts.sm = S(es, f"sm{i}", [128, 48])
                    for nm in ("xn", "bA", "bB", "bC", "kvs", "sg", "vn", "stl", "mvl", "sml", "smq", "sts", "mvs", "sms", "rt"):
                        setattr(ts, "r_" + nm, Res(f"{nm}{i}"))
                    sets.append(ts)
                s0 = sets[0]
                wsf = s0.xn[:, 0:512].rearrange("p (h q) -> p h q", q=128)
                r_wsf = s0.r_xn
                ckf = s0.bufB[:, 0:512].rearrange("p (c d) -> p c d", d=128)
                cvf = s0.bufC[:, 0:512].rearrange("p (c d) -> p c d", d=128)
                r_ckf, r_cvf = s0.r_bB, s0.r_bC
                h2f = u_sb[:, 0:1024].rearrange("p (kc t) -> p kc t", t=128)
                r_h2f = r_u

                DMA("sp", [], [], ropec[:], rc_d.rearrange("(t p) d -> p t d", p=128), pw=[r_rope])
                DMA("sp", [], [], ropes[:], rs_d.rearrange("(t p) d -> p t d", p=128), pw=[r_rope])
                DMA("pool", [], [], wbuf[:, :, 0:768],
                    w_in_d[l, :, 0:768].rearrange("(kc p) n -> p kc n", p=128), pw=[r_wbuf])
                DMA("pool", [], [], wbuf[:, :, 768:1280],
                    w_in_d[l, :, 1536:2048].rearrange("(kc p) n -> p kc n", p=128), pw=[r_wbuf])
                for h in range(8):
                    DMA("pool", [], [], qTall[64:128, h, :], mq_d, pw=r_qT)
                for g in range(2):
                    DMA("pool", [], [], kTall[64:128, g, :], mk_d, pw=r_kT)
                I("dve", "memset", [], r_va, vaug[:, :, :, 64:128], 1.0)
                DMA("sp", [], [], g64[:, 0, :], qg_d[l:l + 1, :].partition_broadcast(128), pw=[r_gv])
                DMA("sp", [], [], g64[:, 1, :], kg_d[l:l + 1, :].partition_broadcast(128), pw=[r_gv])
                DMA("sp", [], [], sgug[:], sgug_d[l:l + 1, :].partition_broadcast(128), pw=[r_gv])
                DMA("sp", [], [r_wsf], wsf, sguw_d[l].rearrange("h p q -> p h q"))
                if moe:
                    DMA("sp", [], [r_rw], rwf[:], rw_d.rearrange("(kc p) e -> p kc e", p=128))

                pb, rpb = bank("a")
                TR([r_wsf, r_id], [rpb], [(pb[:, h * 128:(h + 1) * 128], wsf[:, h, :], identf[:]) for h in range(4)])
                I("dve", "tensor_copy", [rpb], [r_wsT], out=wsT[:].rearrange("p h q -> p (h q)"), in_=pb[:])

                def ln_stats(ts, src_ap, r_src):
                    st, mv, sm = ts.st, ts.mv, ts.sm
                    P.op("dve", [r_src], [ts.r_stl], lambda e, st=st, src_ap=src_ap: [e.bn_stats(out=st[:, 0, :], in_=src_ap[:, 0:512]),
                                                                                      e.bn_stats(out=st[:, 1, :], in_=src_ap[:, 512:1024])][-1])
                    I("dve", "bn_aggr", [ts.r_stl], [ts.r_mvl], out=mv[:, 0, :], in_=st[:, 0:2, :].rearrange("p a b -> p (a b)"))
                    rstd_from_var(mv[:, 0, 1:2], sm[:, 0:1], ts.r_mvl, ts.r_sml)
                    I("dve", "scalar_tensor_tensor", [ts.r_mvl, ts.r_sml], [ts.r_sml], out=sm[:, 1:2], in0=mv[:, 0, 0:1],
                      scalar=-1.0, in1=sm[:, 0:1], op0=ALU.mult, op1=ALU.mult)

                def modulate_transpose(ts, t, slot_shift, slot_scale, src_ap, r_src, want_f32):
                    cnd = 0 if t < 8 else 1
                    xn, r_xn, sm = ts.xn, ts.r_xn, ts.sm
                    ln_stats(ts, src_ap, r_src)
                    I("act", "activation", [r_src, ts.r_sml], [r_xn], out=xn, in_=src_ap, func=AF.Identity,
                      bias=sm[:, 1:2], scale=sm[:, 0:1])
                    pa, rpa = bank("a")
                    pb2, rpb2 = bank("a")
                    TR([r_xn, r_id], [rpa, rpb2],
                       [((pa if kc < 4 else pb2)[:, (kc % 4) * 128:(kc % 4 + 1) * 128], xn[:, kc * 128:(kc + 1) * 128], identf[:])
                        for kc in range(8)])
                    for kc in range(8):
                        src = (pa if kc < 4 else pb2)[:, (kc % 4) * 128:(kc % 4 + 1) * 128]
                        rsrc = rpa if kc < 4 else rpb2
                        sc_ap = modT[:, slot_scale * 8 + kc, cnd:cnd + 1]
                        sh_ap = modT[:, slot_shift * 8 + kc, cnd:cnd + 1]
                        if want_f32:
                            dst, rdst = h2f[:, kc, :], r_h2f
                        else:
                            dst, rdst = hT[:, kc, t * 128:(t + 1) * 128], r_h[t]
                        if kc < 4:
                            I("act", "activation", [rsrc, r_modT], [], out=dst, in_=src, func=AF.Identity,
                              bias=sh_ap, scale=sc_ap, pw=[rdst])
                        else:
                            I("dve", "tensor_scalar", [rsrc, r_modT], [], out=dst, in0=src, scalar1=sc_ap,
                              scalar2=sh_ap, op0=ALU.mult, op1=ALU.add, pw=[rdst])
                    if want_f32:
                        I("dve", "tensor_copy", [r_h2f], [r_h[t]], out=hT[:, :, t * 128:(t + 1) * 128], in_=h2f)

                def stage_env(t):
                    ts = sets[t % 2]
                    return ts, slice(t * 128, (t + 1) * 128)

                def stA(t):
                    ts, tc_ = stage_env(t)
                    modulate_transpose(ts, t, 0, 1, x_sb[:, t, :], r_x[t], False)

                def stB(t):
                    ts, tc_ = stage_env(t)
                    bufA, bufB, bufC, kvs, sg, vn = ts.bufA, ts.bufB, ts.bufC, ts.kvs, ts.sg, ts.vn
                    r_bA, r_bB, r_bC, r_kvs, r_sg, r_vn = ts.r_bA, ts.r_bB, ts.r_bC, ts.r_kvs, ts.r_sg, ts.r_vn
                    st, mv, sm = ts.st, ts.mv, ts.sm
                    pq, rpq = bank("b")
                    MM([r_h[t], r_wbuf], [rpq], [dict(out=pq[:], lhsT=hT[:, kc, tc_], rhs=wbuf[:, kc, 0:512],
                                                      start=(kc == 0), stop=(kc == 7)) for kc in range(8)])
                    pk, rpk = bank("b")
                    MM([r_h[t], r_wbuf], [rpk], [dict(out=pk[:, 0:256], lhsT=hT[:, kc, tc_], rhs=wbuf[:, kc, 512:768],
                                                      start=(kc == 0), stop=(kc == 7)) for kc in range(8)])
                    ps_, rps = bank("b")
                    MM([r_h[t], r_wbuf], [rps], [dict(out=ps_[:], lhsT=hT[:, kc, tc_], rhs=wbuf[:, kc, 768:1280],
                                                      start=(kc == 0), stop=(kc == 7)) for kc in range(8)])
                    I("act", "copy", [rpq], [], out=bufA[:, 0:512], in_=pq[:], pw=[r_bA])
                    I("act", "copy", [rpk], [], out=bufA[:, 512:640], in_=pk[:, 0:128], pw=[r_bA])
                    I("act", "copy", [rpk], [], out=kvs[:, 0:128], in_=pk[:, 128:256], pw=[r_kvs])
                    I("act", "copy", [rps], [], out=kvs[:, 128:640], in_=ps_[:], pw=[r_kvs])
                    DMA("sp", [r_kvs], [], nv_d[l, tc_, :], kvs[:, 0:128], is_output=True)
                    I("dve", "tensor_copy", [r_kvs], [r_va[t]], out=vaug[:, t, :, 0:64],
                      in_=kvs[:, 0:128].rearrange("p (g d) -> p g d", g=2))

                def stC(t):
                    ts, tc_ = stage_env(t)
                    bufA, bufB, bufC, kvs, sg, vn = ts.bufA, ts.bufB, ts.bufC, ts.kvs, ts.sg, ts.vn
                    r_bA, r_bB, r_bC, r_kvs, r_sg, r_vn = ts.r_bA, ts.r_bB, ts.r_bC, ts.r_kvs, ts.r_sg, ts.r_vn
                    st, mv, sm = ts.st, ts.mv, ts.sm
                    smq = sm[:, 8:18]
                    I("act", "activation", [r_bA], [r_bB], out=bufB, in_=bufA, func=AF.Square)
                    I("dve", "reduce_sum", [r_bB], [ts.r_smq], out=smq,
                      in_=bufB.rearrange("p (h d) -> p h d", d=64), axis=AX.X)
                    I("dve", "tensor_scalar", [ts.r_smq], [ts.r_smq], out=smq, in0=smq,
                      scalar1=1.0 / 64, scalar2=None, op0=ALU.mult)
                    rstd_from_var(smq, smq, ts.r_smq, ts.r_smq)
                    A3 = bufA.rearrange("p (h d) -> p h d", d=64)
                    I("dve", "tensor_tensor", [r_bA, ts.r_smq], [r_bA], out=A3, in0=A3,
                      in1=smq.unsqueeze(2).to_broadcast([128, 10, 64]), op=ALU.mult)
                    I("dve", "tensor_tensor", [r_bA, r_gv], [r_bA], out=A3[:, 0:8, :], in0=A3[:, 0:8, :],
                      in1=g64[:, 0:1, :].to_broadcast([128, 8, 64]), op=ALU.mult)
                    I("dve", "tensor_tensor", [r_bA, r_gv], [r_bA], out=A3[:, 8:10, :], in0=A3[:, 8:10, :],
                      in1=g64[:, 1:2, :].to_broadcast([128, 2, 64]), op=ALU.mult)
                    B3 = bufB.rearrange("p (h d) -> p h d", d=64)
                    I("dve", "tensor_tensor", [r_bA, r_rope], [r_bB], out=B3, in0=A3,
                      in1=ropec[:, t:t + 1, :].to_broadcast([128, 10, 64]), op=ALU.mult)
                    A5 = bufA.rearrange("p (h a j f) -> p h a j f", a=2, j=2, f=16)
                    C5 = bufC.rearrange("p (h a j f) -> p h a j f", a=2, j=2, f=16)
                    S5 = ropes[:, t, :].rearrange("p (a j f) -> p a j f", a=2, j=2, f=16)
                    for a in range(2):
                        for j in range(2):
                            I("pool", "tensor_tensor", [r_bA, r_rope], [], out=C5[:, :, a, j, :], in0=A5[:, :, a, 1 - j, :],
                              in1=S5[:, a:a + 1, j, :].to_broadcast([128, 10, 16]), op=ALU.mult, pw=[r_bC])
                    I("dve", "tensor_tensor", [r_bB, r_bC], [r_bA], out=bufA, in0=bufB, in1=bufC, op=ALU.add)
                    DMA("sp", [r_bA], [], nk_d[l, tc_, :], bufA[:, 512:640], is_output=True)
                    sv3 = kvs[:, 384:640].rearrange("p (h d) -> p h d", d=64)
                    sms = sm[:, 20:24]
                    P.op("dve", [r_kvs], [ts.r_sts], lambda e, sv3=sv3, st=st: [e.bn_stats(out=st[:, 2 + h, :], in_=sv3[:, h, :]) for h in range(4)][-1])
                    P.op("dve", [ts.r_sts], [ts.r_mvs], lambda e, mv=mv, st=st: [e.bn_aggr(out=mv[:, 1 + h, :], in_=st[:, 2 + h, :]) for h in range(4)][-1])
                    rstd_from_var(mv[:, 1:5, 1], sms, ts.r_mvs, ts.r_sms)
                    sg3 = sg[:].rearrange("p (h d) -> p h d", d=64)
                    I("dve", "tensor_tensor", [r_kvs, ts.r_mvs], [r_sg], out=sg3, in0=sv3,
                      in1=mv[:, 1:5, 0:1].to_broadcast([128, 4, 64]), op=ALU.subtract)
                    I("dve", "tensor_tensor", [r_sg, ts.r_sms], [r_sg], out=sg3, in0=sg3,
                      in1=sms.unsqueeze(2).to_broadcast([128, 4, 64]), op=ALU.mult)
                    I("dve", "tensor_tensor", [r_sg, r_gv], [r_vn], out=vn[:], in0=sg[:], in1=sgug[:], op=ALU.mult)

                def stD(t):
                    ts, tc_ = stage_env(t)
                    bufA, bufB, bufC, kvs, sg, vn = ts.bufA, ts.bufB, ts.bufC, ts.kvs, ts.sg, ts.vn
                    r_bA, r_bB, r_bC, r_kvs, r_sg, r_vn = ts.r_bA, ts.r_bB, ts.r_bC, ts.r_kvs, ts.r_sg, ts.r_vn
                    pa, rpa = bank("a")
                    pb2, rpb2 = bank("a")
                    TR([r_bA, r_id], [rpa, rpb2],
                       [(pa[:, c * 128:(c + 1) * 128], bufA[:, c * 128:(c + 1) * 128], identf[:]) for c in range(4)]
                       + [(pb2[:, 0:128], bufA[:, 512:640], identf[:])])
                    pa3 = pa[:].rearrange("p (c t) -> p c t", t=128)
                    I("act", "copy", [rpa], [], out=qTall[0:64, 0:8:2, tc_], in_=pa3[0:64, :, :], pw=[r_qT[t]])
                    I("act", "copy", [rpa], [], out=qTall[0:64, 1:8:2, tc_], in_=pa3[64:128, :, :], pw=[r_qT[t]])
                    I("dve", "tensor_copy", [rpb2], [], out=kTall[0:64, 0, tc_], in_=pb2[0:64, 0:128], pw=[r_kT[t]])
                    I("dve", "tensor_copy", [rpb2], [], out=kTall[0:64, 1, tc_], in_=pb2[64:128, 0:128], pw=[r_kT[t]])
                    psg, rpsg = bank("b")
                    MM([r_vn, r_wsT], [rpsg], [dict(out=psg[:, h * 64:(h + 1) * 64], lhsT=wsT[:, h, :], rhs=vn[:, h * 64:(h + 1) * 64],
                                                    start=True, stop=True) for h in range(4)])
                    for h in range(4):
                        I("dve", "scalar_tensor_tensor", [rpsg, r_fm, r_kvs], [r_sg], out=sg[:, h * 64:(h + 1) * 64],
                          in0=psg[:, h * 64:(h + 1) * 64], scalar=fm[:, 56 + h:57 + h],
                          in1=kvs[:, 128 + h * 64:128 + (h + 1) * 64], op0=ALU.add, op1=ALU.mult)
                    pa, rpa = bank("a")
                    TR([r_sg, r_id], [rpa], [(pa[:, c * 128:(c + 1) * 128], sg[:, c * 128:(c + 1) * 128], identf[:]) for c in range(2)])
                    I("act", "copy", [rpa], [], out=mx67[:, :, tc_], in_=pa[:, 0:256].rearrange("p (c t) -> p c t", t=128), pw=[r_h[t]])

                for s_ in range(NT + 3):
                    if 0 <= s_ - 3 < NT:
                        stD(s_ - 3)
                    if 0 <= s_ - 2 < NT:
                        stC(s_ - 2)
                    if 0 <= s_ - 1 < NT:
                        stB(s_ - 1)
                    if s_ < NT:
                        stA(s_)
                DMA("sp", [], [r_ckf], ckf, ck_d[l].rearrange("(c p) d -> p c d", p=128))
                DMA("sp", [], [r_cvf], cvf, cv_d[l].rearrange("(c p) d -> p c d", p=128))
                pb, rpb = bank("a")
                TR([r_ckf, r_id], [rpb], [(pb[:, c * 128:(c + 1) * 128], ckf[:, c, :], identf[:]) for c in range(4)])
                for g in range(2):
                    I("dve", "tensor_copy", [rpb], r_kT[10:14], out=kTall[0:64, g, 1280:1792],
                      in_=pb[g * 64:(g + 1) * 64, :])
                I("dve", "tensor_copy", [r_cvf], r_va[10:14], out=vaug[:, 10:14, :, 0:64],
                  in_=cvf.rearrange("p c (g d) -> p c g d", g=2))
                P.barrier()

                for ch in range(2):
                    cols = {"cin": 768 + ch * 128, "cb": 1024 + ch * 128, "cc": 1280 + ch * 128}
                    DMA("pool", [], [r_wfb], wfb[:, :, 0:128],
                        w_fm_d[l, 0 + ch].rearrange("p (kc n) -> p kc n", kc=8))
                    DMA("pool", [], [r_wfb], wfb[:, :, 128:256],
                        w_fm_d[l, 4 + ch].rearrange("p (kc n) -> p kc n", kc=8))
                    for (g0, gn) in TG:
                        p1, rp1 = bank("b")
                        MM(r_h + [r_wfb], [rp1], [dict(out=p1[:, 0:gn], lhsT=wfb[:, kc, 0:128], rhs=hT[:, kc, g0:g0 + gn],
                                                       start=(kc == 0), stop=(kc == 7)) for kc in range(8)])
                        p2, rp2 = bank("b")
                        MM(r_h + [r_wfb], [rp2], [dict(out=p2[:, 0:gn], lhsT=wfb[:, kc, 128:256], rhs=hT[:, kc, g0:g0 + gn],
                                                       start=(kc == 0), stop=(kc == 7)) for kc in range(8)])
                        I("act", "copy", [rp1], [r_u], out=u_sb[:, g0:g0 + gn], in_=p1[:, 0:gn])
                        I("dve", "tensor_tensor", [rp2, r_u], [r_u], out=u_sb[:, g0:g0 + gn], in0=p2[:, 0:gn],
                          in1=u_sb[:, g0:g0 + gn], op=ALU.mult)
                    w0, w1, w2 = fm[:, 48 + ch:49 + ch], fm[:, 50 + ch:51 + ch], fm[:, 52 + ch:53 + ch]
                    nw0, nw2 = fm[:, 60 + ch:61 + ch], fm[:, 64 + ch:65 + ch]
                    cbias = fm[:, 54 + ch:55 + ch]
                    I("act", "activation", [r_u, r_fm], [r_y], out=y_sb[:], in_=u_sb[:], func=AF.Identity, bias=cbias, scale=w1)
                    I("dve", "scalar_tensor_tensor", [r_u, r_y, r_fm], [r_y], out=y_sb[:, 1:NTOK], in0=u_sb[:, 0:NTOK - 1],
                      scalar=w0, in1=y_sb[:, 1:NTOK], op0=ALU.mult, op1=ALU.add)
                    I("dve", "scalar_tensor_tensor", [r_u, r_y, r_fm], [r_y], out=y_sb[:, 0:NTOK - 1], in0=u_sb[:, 1:NTOK],
                      scalar=w2, in1=y_sb[:, 0:NTOK - 1], op0=ALU.mult, op1=ALU.add)
                    uv = u_sb[:].rearrange("p (a b) -> p a b", b=256)
                    yv = y_sb[:].rearrange("p (a b) -> p a b", b=256)
                    I("dve", "tensor_tensor", [r_u, r_cflag], [r_smc], out=smc[:, 0:4], in0=uv[:, 0:4, 255], in1=cflag[:], op=ALU.mult)
                    I("dve", "scalar_tensor_tensor", [r_smc, r_y, r_fm], [r_y], out=yv[:, 1:5, 0], in0=smc[:, 0:4], scalar=nw0,
                      in1=yv[:, 1:5, 0], op0=ALU.mult, op1=ALU.add)
                    I("dve", "tensor_tensor", [r_u, r_cflag], [r_smc], out=smc[:, 4:8], in0=uv[:, 1:5, 0], in1=cflag[:], op=ALU.mult)
                    I("dve", "scalar_tensor_tensor", [r_smc, r_y, r_fm], [r_y], out=yv[:, 0:4, 255], in0=smc[:, 4:8], scalar=nw2,
                      in1=yv[:, 0:4, 255], op0=ALU.mult, op1=ALU.add)
                    DMA("pool", [], [r_wfb], wfb[:, :, 0:128],
                        w_fm_d[l, 2 + ch].rearrange("p (kc n) -> p kc n", kc=8))
                    pcb = []
                    for (g0, gn) in TG:
                        p1, rp1 = bank("b")
                        MM(r_h + [r_wfb], [rp1], [dict(out=p1[:, 0:gn], lhsT=wfb[:, kc, 0:128], rhs=hT[:, kc, g0:g0 + gn],
                                                       start=(kc == 0), stop=(kc == 7)) for kc in range(8)])
                        pcb.append((p1, rp1, g0, gn))
                    for (p1, rp1, g0, gn) in pcb:
                        I("dve", "tensor_tensor", [rp1, r_y], [], out=mx45[:, ch, g0:g0 + gn], in0=p1[:, 0:gn],
                          in1=y_sb[:, g0:g0 + gn], op=ALU.mult, pw=[r_h[t_] for t_ in range(g0 // 128, (g0 + gn) // 128)])

                DMA("pool", [], [r_wbuf], wbuf[:, :, 0:1024], w_out_d[l].rearrange("(kc p) n -> p kc n", p=128))
                ecnt = [0]
                rcnt = [0]
                LOOK = 3

                def attention(q0, qn, kts):
                    r_q = [r_qT[t] for t in range(q0 // 128, (q0 + qn) // 128)]
                    r_out = [r_h[t] for t in range(q0 // 128, (q0 + qn) // 128)]
                    for h in range(8):
                        g = h // 4
                        po, rpo = bank("b")
                        pend = []
                        n = len(kts)

                        def flush(last):
                            pi, pe_i, pkt = pend.pop(0)
                            MM([r_E[pe_i], r_va[pkt]], [rpo], [dict(out=po[:, 0:qn], lhsT=vaug[:, pkt, g, :], rhs=Eb[pe_i][:, 0:qn],
                                                                    start=(pi == 0), stop=last)])
                        for i, kt in enumerate(kts):
                            psx, rps_ = bank("a")
                            MM(r_q + [r_kT[kt]], [rps_], [dict(out=psx[:, 0:qn], lhsT=kTall[:, g, kt * 128:(kt + 1) * 128],
                                                               rhs=qTall[:, h, q0:q0 + qn], start=True, stop=True)])
                            ei = ecnt[0] % 5
                            ecnt[0] += 1
                            I("act", "activation", [rps_], [r_E[ei]], out=Eb[ei][:, 0:qn], in_=psx[:, 0:qn], func=AF.Exp, scale=SCALE)
                            pend.append((i, ei, kt))
                            if len(pend) > LOOK:
                                flush(False)
                        while pend:
                            flush(len(pend) == 1)
                        ri = rcnt[0] % 2
                        rcnt[0] += 1
                        I("dve", "reciprocal", [rpo], [r_rd[ri]], out=rd[ri][64:128, 0:qn], in_=po[64:128, 0:qn])
                        ph = (h % 2) * 64
                        I("dve", "tensor_tensor", [rpo, r_rd[ri]], [],
                          out=hT[ph:ph + 64, h // 2, q0:q0 + qn], in0=po[0:64, 0:qn], in1=rd[ri][64:128, 0:qn], op=ALU.mult, pw=r_out)

                ktsA = list(range(8)) + [10, 11, 12, 13]
                attention(0, 512, ktsA)
                attention(512, 512, ktsA)
                attention(1024, 256, [8, 9])
                P.barrier()

                e1banks = {}

                def stE1a(t):
                    tc_ = slice(t * 128, (t + 1) * 128)
                    e1banks[t] = []
                    for half in range(2):
                        pw, rpw = bank("b")
                        MM([r_h[t], r_wbuf], [rpw], [dict(out=pw[:], lhsT=mixT(kc)[:, tc_], rhs=wbuf[:, kc, half * 512:(half + 1) * 512],
                                                          start=(kc == 0), stop=(kc == 7)) for kc in range(8)])
                        e1banks[t].append((pw, rpw))

                def stE1(t):
                    ts = sets[t % 2]
                    xn, r_xn, sm = ts.xn, ts.r_xn, ts.sm
                    tc_ = slice(t * 128, (t + 1) * 128)
                    cnd = 0 if t < 8 else 1
                    for half in range(2):
                        pw, rpw = e1banks[t][half]
                        hs = slice(half * 512, (half + 1) * 512)
                        I("dve", "tensor_tensor", [rpw, r_gate[cnd]], [], out=xn[:, hs], in0=pw[:], in1=gate4[:, cnd, hs], op=ALU.mult, pw=[r_xn])
                    I("dve", "scalar_tensor_tensor", [r_x[t], r_xn], [r_x[t]], out=x_sb[:, t, :], in0=x_sb[:, t, :], scalar=ALPHA,
                      in1=xn, op0=ALU.mult, op1=ALU.add)
                    ln_stats(ts, x_sb[:, t, :], r_x[t])
                    I("act", "activation", [r_x[t], ts.r_sml], [r_xn], out=xn, in_=x_sb[:, t, :], func=AF.Identity,
                      bias=sm[:, 1:2], scale=sm[:, 0:1])
                    I("dve", "tensor_tensor", [r_xn, r_lnp[0]], [r_xn], out=xn, in0=xn, in1=lnp[:, 0, :], op=ALU.mult)
                    I("pool", "tensor_tensor", [r_xn, r_lnp[1]], [r_x[t]], out=x_sb[:, t, :], in0=xn, in1=lnp[:, 1, :], op=ALU.add)

                def stE2(t):
                    ts = sets[t % 2]
                    xn, r_xn, sm = ts.xn, ts.r_xn, ts.sm
                    tc_ = slice(t * 128, (t + 1) * 128)
                    cnd = 0 if t < 8 else 1
                    modulate_transpose(ts, t, 3, 4, x_sb[:, t, :], r_x[t], moe)
                    I("act", "mul", [r_x[t]], [r_x[t]], out=x_sb[:, t, :], in_=x_sb[:, t, :], mul=ALPHA)
                    if moe:
                        pr_, rpr = bank("a")
                        MM([r_h2f, r_rw], [rpr], [dict(out=pr_[:, 0:8], lhsT=h2f[:, kc, :], rhs=rwf[:, kc, :],
                                                       start=(kc == 0), stop=(kc == 7)) for kc in range(8)])
                        r_rt = ts.r_rt
                        lg = sm[:, 24:32]
                        m1, m2, dd, e1 = sm[:, 2:3], sm[:, 3:4], sm[:, 4:5], sm[:, 5:6]
                        eq1, eq2, l2 = sm[:, 32:40], sm[:, 40:48], ts.sg[:, 0:8]
                        I("dve", "tensor_copy", [rpr], [r_rt], out=lg, in_=pr_[:, 0:8])
                        I("dve", "reduce_max", [r_rt], [r_rt], out=m1, in_=lg, axis=AX.X)
                        I("dve", "tensor_scalar", [r_rt], [r_G[t]], out=G[:, t, :], in0=lg, scalar1=m1, scalar2=None, op0=ALU.is_equal)
                        I("dve", "scalar_tensor_tensor", [r_rt, r_G[t]], [ts.r_sg], out=l2, in0=G[:, t, :], scalar=-1e30, in1=lg,
                          op0=ALU.mult, op1=ALU.add)
                        I("dve", "reduce_max", [ts.r_sg], [r_rt], out=m2, in_=l2, axis=AX.X)
                        I("dve", "tensor_scalar", [r_rt, ts.r_sg], [r_G[t]], out=GQ2[:, t, :], in0=l2, scalar1=m2, scalar2=None, op0=ALU.is_equal)
                        I("dve", "tensor_tensor", [r_rt], [], out=DDt[:, 0, t:t + 1], in0=m2, in1=m1, op=ALU.subtract, pw=[r_DD])

                for s_ in range(NT + 1):
                    if s_ < NT:
                        stE1a(s_)
                    if 0 <= s_ - 1 < NT:
                        stE2(s_ - 1)
                    if s_ < NT:
                        stE1(s_)
                if moe:
                    I("act", "activation", [r_DD], [r_DD], out=DDt[:, 1, :], in_=DDt[:, 0, :], func=AF.Exp)
                    I("dve", "tensor_scalar", [r_DD], [r_DD], out=DDt[:, 2, :], in0=DDt[:, 1, :], scalar1=1.0, scalar2=None, op0=ALU.add)
                    I("dve", "reciprocal", [r_DD], [r_DD], out=DDt[:, 2, :], in_=DDt[:, 2, :])
                    I("dve", "tensor_tensor", [r_DD], [r_DD], out=DDt[:, 3, :], in0=DDt[:, 1, :], in1=DDt[:, 2, :], op=ALU.mult)
                    I("dve", "tensor_tensor", r_G + [r_DD], r_G, out=G[:], in0=G[:],
                      in1=DDt[:, 2, :].unsqueeze(2).to_broadcast([128, NT, 8]), op=ALU.mult)
                    I("dve", "tensor_tensor", r_G + [r_DD], r_G, out=GQ2[:], in0=GQ2[:],
                      in1=DDt[:, 3, :].unsqueeze(2).to_broadcast([128, NT, 8]), op=ALU.mult)
                    I("dve", "tensor_tensor", r_G, r_G, out=G[:], in0=G[:], in1=GQ2[:], op=ALU.add)
            P.barrier()

            with ExitStack() as es:
                w1c = [S(es, f"w1c{i}", [128, 8, 384], BF16) for i in range(3)]
                w3c = [S(es, f"w3c{i}", [128, 8, 384], BF16) for i in range(3)]
                r_w1c = [Res(f"w1c{i}") for i in range(3)]
                r_w3c = [Res(f"w3c{i}") for i in range(3)]
                w2b = S(es, "w2b", [128, 11, D], BF16)
                r_w2b = Res("w2b")
                gT = S(es, "gT", [128, 11, NTOK], BF16)
                r_gT = [Res(f"gT{g}") for g in range(3)]
                sa = [S(es, f"sa{i}", [128, 512]) for i in range(2)]
                r_sa = [Res(f"sa{i}") for i in range(2)]
                tmp = [S(es, f"tmp{i}", [128, 512]) for i in range(2)]
                r_tmp = [Res(f"tmp{i}") for i in range(2)]
                xn = S(es, "xn2", [128, D])
                r_xn = Res("xn2")
                st = S(es, "st2", [128, 2, 6])
                mv = S(es, "mv2", [128, 2])
                sm = S(es, "sm2", [128, 4])
                r_st, r_mv, r_sm = Res("st2"), Res("mv2"), Res("sm2")

                if moe:
                    experts = [(mw1_d[e], mw3_d[e], mw2_d[e], e) for e in range(8)]
                else:
                    experts = [(fw1_d[e], fw3_d[e], fw2_d[e * 1408:(e + 1) * 1408, :], None) for e in range(2)]
                chunks = [(0, 384), (384, 384), (768, 384), (1152, 256)]
                cidx = 0
                sidx = 0
                tidx = 0
                for (w1_ap, w3_ap, w2_ap, ge) in experts:
                    DMA("pool", [], [r_w2b], w2b[:], w2_ap.rearrange("(kc p) n -> p kc n", p=128))
                    for (c0, cw) in chunks:
                        bi = cidx % 3
                        cidx += 1
                        DMA("pool", [], [r_w1c[bi]], w1c[bi][:, :, 0:cw], w1_ap[:, 8 * c0:8 * (c0 + cw)].rearrange("p (kc n) -> p kc n", kc=8))
                        DMA("pool", [], [r_w3c[bi]], w3c[bi][:, :, 0:cw], w3_ap[:, 8 * c0:8 * (c0 + cw)].rearrange("p (kc n) -> p kc n", kc=8))
                        for sub in range(cw // 128):
                            fc = c0 // 128 + sub
                            for gi_, (g0, gn) in enumerate(TG):
                                rh = [r_h[t] for t in range(g0 // 128, (g0 + gn) // 128)]
                                pa_, rpa_ = bank("a")
                                MM(rh + [r_w1c[bi]], [rpa_], [dict(out=pa_[:, 0:gn], lhsT=w1c[bi][:, kc, sub * 128:(sub + 1) * 128],
                                                                   rhs=hT[:, kc, g0:g0 + gn], start=(kc == 0), stop=(kc == 7)) for kc in range(8)])
                                pb_, rpb_ = bank("b")
                                MM(rh + [r_w3c[bi]], [rpb_], [dict(out=pb_[:, 0:gn], lhsT=w3c[bi][:, kc, sub * 128:(sub + 1) * 128],
                                                                   rhs=hT[:, kc, g0:g0 + gn], start=(kc == 0), stop=(kc == 7)) for kc in range(8)])
                                si = sidx % 2
                                sidx += 1
                                I("act", "activation", [rpa_], [r_sa[si]], out=sa[si][:, 0:gn], in_=pa_[:, 0:gn], func=AF.Silu)
                                I("dve", "tensor_tensor", [r_sa[si], rpb_], [], out=gT[:, fc, g0:g0 + gn], in0=sa[si][:, 0:gn],
                                  in1=pb_[:, 0:gn], op=ALU.mult, pw=[r_gT[gi_]])
                    for t in range(NT):
                        cnd = 0 if t < 8 else 1
                        tc_ = slice(t * 128, (t + 1) * 128)
                        for half in range(2):
                            hs = slice(half * 512, (half + 1) * 512)
                            pd, rpd = bank("a" if (t * 2 + half) % 2 == 0 else "b")
                            MM([r_gT[min(t // 4, 2)], r_w2b], [rpd], [dict(out=pd[:], lhsT=gT[:, kc, tc_], rhs=w2b[:, kc, hs],
                                                                          start=(kc == 0), stop=(kc == 10)) for kc in range(11)])
                            ti = tidx % 2
                            tidx += 1
                            if ge is None:
                                I("dve", "tensor_tensor", [rpd, r_gate[2 + cnd]], [r_tmp[ti]], out=tmp[ti][:], in0=pd[:],
                                  in1=gate4[:, 2 + cnd, hs], op=ALU.mult)
                            else:
                                I("act", "activation", [rpd, r_G[t]], [r_tmp[ti]], out=tmp[ti][:], in_=pd[:], func=AF.Identity,
                                  scale=G[:, t, ge:ge + 1])
                                I("dve", "tensor_tensor", [r_tmp[ti], r_gate[2 + cnd]], [r_tmp[ti]], out=tmp[ti][:], in0=tmp[ti][:],
                                  in1=gate4[:, 2 + cnd, hs], op=ALU.mult)
                            I("dve", "tensor_tensor", [r_tmp[ti], r_x[t]], [r_x[t]], out=x_sb[:, t, hs], in0=x_sb[:, t, hs],
                              in1=tmp[ti][:], op=ALU.add)
                for t in range(NT):
                    src = x_sb[:, t, :]
                    P.op("dve", [r_x[t]], [r_st], lambda e, src=src, st=st: [e.bn_stats(out=st[:, 0, :], in_=src[:, 0:512]),
                                                                       e.bn_stats(out=st[:, 1, :], in_=src[:, 512:1024])][-1])
                    I("dve", "bn_aggr", [r_st], [r_mv], out=mv[:], in_=st[:].rearrange("p a b -> p (a b)"))
                    rstd_from_var(mv[:, 1:2], sm[:, 0:1], r_mv, r_sm)
                    I("dve", "scalar_tensor_tensor", [r_mv, r_sm], [r_sm], out=sm[:, 1:2], in0=mv[:, 0:1],
                      scalar=-1.0, in1=sm[:, 0:1], op0=ALU.mult, op1=ALU.mult)
                    I("act", "activation", [r_x[t], r_sm], [r_xn], out=xn[:], in_=src, func=AF.Identity, bias=sm[:, 1:2], scale=sm[:, 0:1])
                    I("dve", "tensor_tensor", [r_xn, r_lnp[2]], [r_xn], out=xn[:], in0=xn[:], in1=lnp[:, 2, :], op=ALU.mult)
                    I("pool", "tensor_tensor", [r_xn, r_lnp[3]], [r_x[t]], out=src, in0=xn[:], in1=lnp[:, 3, :], op=ALU.add)
                    if last:
                        DMA("sp", [r_x[t]], [], y_d[t * 128:(t + 1) * 128, :], src, is_output=True)
            P.barrier()

        P.emit()
    return nc


def _rope_tables():
    n = 1024
    rows = n // 64
    row = np.repeat(np.arange(rows, dtype=np.float32), 64)
    col = np.tile(np.arange(64, dtype=np.float32), rows)
    inv_freq = (np.float32(10000.0) ** (-np.arange(0, 32, 2, dtype=np.float32) / np.float32(32))).astype(np.float32)
    ang = np.stack([row[:, None] * inv_freq, col[:, None] * inv_freq], axis=1).astype(np.float32)
    cos, sin = np.cos(ang).astype(np.float32), np.sin(ang).astype(np.float32)
    cosE = np.stack([cos, cos], axis=2).reshape(n, 64)
    sinE = np.stack([-sin, sin], axis=2).reshape(n, 64)
    return cosE.astype(np.float32), sinE.astype(np.float32)


def _core_inputs(c, inp, shared):
    xs, xp = inp["x_sample"], inp["x_prompt"]
    cosE, sinE = shared["rope"]
    rc = np.ones((NTOK, 64), np.float32)
    rs = np.zeros((NTOK, 64), np.float32)
    ids_q = np.zeros(NTOK, np.int64)
    ids_k = np.zeros(1792, np.int64)
    if c < 2:
        x = np.concatenate([xs[c], xp[c]], axis=0)
        cond = np.stack([inp["c"][c], inp["c_ctx"]])
        ck = inp["cache_k"][c].reshape(DEPTH, 512, 128)
        cv = inp["cache_v"][c].reshape(DEPTH, 512, 128)
        rc[:1024], rs[:1024] = cosE, sinE
        ids_q[1024:] = 1
        ids_k[:NTOK] = ids_q
        ids_k[NTOK:] = 0
        cflag = np.tile(np.array([0, 0, 0, 1], np.float32), (128, 1))
    else:
        p0 = 2 + 5 * (c - 2)
        x = xp[p0:p0 + 5].reshape(NTOK, D)
        cond = np.stack([inp["c_ctx"], inp["c_ctx"]])
        ck = np.zeros((DEPTH, 512, 128), np.float32)
        cv = np.zeros((DEPTH, 512, 128), np.float32)
        ids_q = np.arange(NTOK) // 256
        ids_k[:NTOK] = ids_q
        ids_k[NTOK:] = 63
        cflag = np.ones((128, 4), np.float32)
    mk = (np.arange(64)[:, None] == ids_k[None, :]).astype(np.float32)
    mq = np.where(np.arange(64)[:, None] == ids_q[None, :], 0.0, NEG).astype(np.float32)
    m = dict(shared["weights"])
    m.update({
        "x": np.ascontiguousarray(x, dtype=np.float32),
        "cond": np.ascontiguousarray(cond.reshape(16, 128), dtype=np.float32),
        "ck": np.ascontiguousarray(ck, dtype=np.float32), "cv": np.ascontiguousarray(cv, dtype=np.float32),
        "mq": mq, "mk": mk, "ropec": rc, "ropes": rs, "cflag": cflag,
        "ident": np.eye(128, dtype=np.float32),
    })
    return m


_NC_CACHE = {}


def kernel(**inp):
    inp = {k: np.asarray(v) for k, v in inp.items()}
    f = lambda a: np.ascontiguousarray(a, dtype=np.float32)
    def tile_cols(w):
        E = w.shape[0]
        parts = []
        for c0, cw in ((0, 384), (384, 384), (768, 384), (1152, 256)):
            blk = w[:, :, c0:c0 + cw].reshape(E, 8, 128, cw).transpose(0, 2, 1, 3).reshape(E, 128, 8 * cw)
            parts.append(blk)
        return f(np.concatenate(parts, axis=2))

    ada_t = inp["ada_w"].reshape(DEPTH, 8, 128, 12, 512).transpose(0, 3, 2, 1, 4).reshape(DEPTH, 12, 128, 4096)
    wfm_t = inp["w_in"][:, :, 768:1536].reshape(DEPTH, 8, 128, 6, 128).transpose(0, 3, 2, 1, 4).reshape(DEPTH, 6, 128, 1024)
    fw1 = inp["ffn_w1"][0].reshape(D, 2, 1408).transpose(1, 0, 2)
    fw3 = inp["ffn_w3"][0].reshape(D, 2, 1408).transpose(1, 0, 2)
    weights = {
        "ada_w": f(ada_t), "ada_b": f(inp["ada_b"].reshape(DEPTH, 48, 128)),
        "w_in": f(inp["w_in"]), "w_in_fm": f(wfm_t), "q_g": f(inp["q_norm_g"]), "k_g": f(inp["k_norm_g"]),
        "conv_w": f(inp["conv_w"].reshape(DEPTH, 6, 128)), "conv_b": f(inp["conv_b"].reshape(DEPTH, 2, 128)),
        "sgu_g": f(inp["sgu_norm_g"]), "sgu_w": f(inp["sgu_w"]), "sgu_b": f(inp["sgu_b"]),
        "w_out": f(inp["w_out"]), "ln1_g": f(inp["ln1_g"]), "ln1_b": f(inp["ln1_b"]),
        "ln2_g": f(inp["ln2_g"]), "ln2_b": f(inp["ln2_b"]),
        "ffn_w1": tile_cols(fw1), "ffn_w3": tile_cols(fw3), "ffn_w2": f(inp["ffn_w2"][0]),
        "router_w": f(inp["router_w"][0]), "moe_w1": tile_cols(inp["moe_w1"][0]), "moe_w3": tile_cols(inp["moe_w3"][0]),
        "moe_w2": f(inp["moe_w2"][0]),
    }
    shared = {"weights": weights, "rope": _rope_tables()}
    in_maps = [_core_inputs(c, inp, shared) for c in range(8)]
    if "nc" not in _NC_CACHE:
        _NC_CACHE["nc"] = build_program()
    res = run_bass_kernel_spmd(_NC_CACHE["nc"], in_maps, core_ids=list(range(8)))
    R = res.results
    y_p = np.zeros((32, 256, D), np.float32)
    y_s = np.zeros((2, 1024, D), np.float32)
    new_k = np.zeros((32, DEPTH, 256, 2, 64), np.float32)
    new_v = np.zeros((32, DEPTH, 256, 2, 64), np.float32)
    for c in range(8):
        y, nk, nv = R[c]["y"], R[c]["nk"], R[c]["nv"]
        if c < 2:
            y_s[c] = y[:1024]
            y_p[c] = y[1024:]
            new_k[c] = nk[:, 1024:].reshape(DEPTH, 256, 2, 64)
            new_v[c] = nv[:, 1024:].reshape(DEPTH, 256, 2, 64)
        else:
            p0 = 2 + 5 * (c - 2)
            y_p[p0:p0 + 5] = y.reshape(5, 256, D)
            new_k[p0:p0 + 5] = nk.reshape(DEPTH, 5, 256, 2, 64).transpose(1, 0, 2, 3, 4)
            new_v[p0:p0 + 5] = nv.reshape(DEPTH, 5, 256, 2, 64).transpose(1, 0, 2, 3, 4)
    return (y_p, y_s, new_k, new_v)
```

```python
from contextlib import ExitStack
import numpy as np
import concourse.bass as bass
import concourse.mybir as mybir
from concourse.bass_utils import run_bass_kernel_spmd

F32 = mybir.dt.float32
BF16 = mybir.dt.bfloat16
AF = mybir.ActivationFunctionType
ALU = mybir.AluOpType
AX = mybir.AxisListType

D = 1024
NT = 10
NTOK = 1280
DEPTH = 2
EPS = 1e-6
ALPHA = (2 * DEPTH) ** 0.25
SCALE = 64 ** -0.5
NEG = -30000.0
TG = [(0, 512), (512, 512), (1024, 256)]


class Res:
    __slots__ = ("name", "last_w", "readers", "par_w", "psum")

    def __init__(self, name="", psum=False):
        self.name = name
        self.psum = psum
        self.last_w = None
        self.readers = []
        self.par_w = []


class Prog:
    COMPUTE = ("pe", "act", "dve", "pool")
    NQ = 8

    def __init__(self, nc):
        self.nc = nc
        self.eng = {"pe": nc.tensor, "act": nc.scalar, "dve": nc.vector,
                    "pool": nc.gpsimd, "sp": nc.sync}
        self.count = {e: 0 for e in self.COMPUTE}
        self.ops = {e: [] for e in self.eng}
        self.seen = {e: {} for e in self.eng}
        self.semobj = {}
        self.dsem = {}
        self.dcount = {}
        self.dnext = {}
        for e in self.COMPUTE:
            self.semobj[f"prog_{e}"] = nc.alloc_semaphore(name=f"prog_{e}")
        for q in ("sp", "pool"):
            self.dsem[q] = []
            for i in range(self.NQ):
                nm = f"dma_{q}_{i}"
                self.semobj[nm] = nc.alloc_semaphore(name=nm)
                self.dsem[q].append(nm)
                self.dcount[nm] = 0
            self.dnext[q] = 0
        self.final = []
        self.extra = {e: [] for e in self.eng}

    def _need(self, waits, tok):
        if tok is None:
            return
        s, v = tok
        if waits.get(s, 0) < v:
            waits[s] = v

    def _deps(self, e, reads, writes, pwrites=()):
        waits = {}
        for s, v in self.extra[e]:
            self._need(waits, (s, v))
        self.extra[e] = []
        for r in reads:
            self._need(waits, r.last_w)
            for t in r.par_w:
                self._need(waits, t)
        for w in writes:
            self._need(waits, w.last_w)
            for t in w.par_w:
                self._need(waits, t)
            for t in w.readers:
                self._need(waits, t)
        for w in pwrites:
            self._need(waits, w.last_w)
            for t in w.readers:
                self._need(waits, t)
        out = []
        for s, v in waits.items():
            if e == "pe" and s == "prog_pe":
                continue
            if self.seen[e].get(s, 0) >= v:
                continue
            self.seen[e][s] = v
            out.append((s, v))
        return out

    def _commit(self, tok, reads, writes, pwrites=()):
        for r in reads:
            r.readers.append(tok)
        for w in writes:
            w.last_w = tok
            w.readers = []
            w.par_w = []
        for w in pwrites:
            w.par_w.append(tok)

    PAR = True

    def op(self, e, reads, writes, fn, pwrites=()):
        if not self.PAR:
            writes, pwrites = list(writes) + list(pwrites), ()
        if e != "pe":
            extra_w = [r for r in reads if r.psum and r not in writes]
            if extra_w:
                writes = list(writes) + extra_w
        waits = self._deps(e, reads, writes, pwrites)
        self.count[e] += 1
        tok = (f"prog_{e}", self.count[e])
        self.ops[e].append((waits, fn, tok))
        self._commit(tok, reads, writes, pwrites)
        return tok

    def dma(self, q, reads, writes, fn, is_output=False, pwrites=()):
        i = self.dnext[q]
        self.dnext[q] = (i + 1) % self.NQ
        nm = self.dsem[q][i]
        waits = self._deps(q, reads, writes, pwrites)
        prev = self.dcount[nm]
        if prev > 0 and self.seen[q].get(nm, 0) < prev:
            self.seen[q][nm] = prev
            waits.append((nm, prev))
        self.dcount[nm] = prev + 16
        tok = (nm, prev + 16)
        self.ops[q].append((waits, fn, tok))
        self._commit(tok, reads, writes, pwrites)
        if is_output:
            self.final.append(tok)
        return tok

    def barrier(self):
        toks = [(f"prog_{e}", self.count[e]) for e in self.COMPUTE if self.count[e] > 0]
        toks += [(nm, v) for nm, v in self.dcount.items() if v > 0]
        for e in self.eng:
            self.extra[e] = list(toks)

    def emit(self):
        nc = self.nc
        fin = {}
        for s, v in self.final:
            fin[s] = max(fin.get(s, 0), v)

        def run(e, engine):
            for waits, fn, tok in self.ops[e]:
                for s, v in waits:
                    engine.wait_ge(self.semobj[s], v)
                ins = fn(engine)
                s, v = tok
                ins.then_inc(self.semobj[s], 1 if s.startswith("prog_") else 16)
            if e == "sp":
                for s, v in fin.items():
                    engine.wait_ge(self.semobj[s], v)

        with nc.Block() as block:
            @block.tensor
            def _(eng):
                run("pe", eng)

            @block.scalar
            def _(eng):
                run("act", eng)

            @block.vector
            def _(eng):
                run("dve", eng)

            @block.gpsimd
            def _(eng):
                run("pool", eng)

            @block.sync
            def _(eng):
                run("sp", eng)


def build_program(n_layers=DEPTH):
    nc = bass.Bass("TRN2", target_bir_lowering=False)

    def din(name, shape):
        return nc.dram_tensor(name, list(shape), F32, kind="ExternalInput").ap()

    def dout(name, shape):
        return nc.dram_tensor(name, list(shape), F32, kind="ExternalOutput").ap()

    x_d = din("x", [NTOK, D])
    cond_d = din("cond", [16, 128])
    ck_d = din("ck", [DEPTH, 512, 128])
    cv_d = din("cv", [DEPTH, 512, 128])
    mq_d = din("mq", [64, NTOK])
    mk_d = din("mk", [64, 1792])
    rc_d = din("ropec", [NTOK, 64])
    rs_d = din("ropes", [NTOK, 64])
    cflag_d = din("cflag", [128, 4])
    ident_d = din("ident", [128, 128])
    ada_w_d = din("ada_w", [DEPTH, 12, 128, 4096])
    ada_b_d = din("ada_b", [DEPTH, 48, 128])
    w_in_d = din("w_in", [DEPTH, D, 2048])
    w_fm_d = din("w_in_fm", [DEPTH, 6, 128, 1024])
    qg_d = din("q_g", [DEPTH, 64])
    kg_d = din("k_g", [DEPTH, 64])
    convw_d = din("conv_w", [DEPTH, 6, 128])
    convb_d = din("conv_b", [DEPTH, 2, 128])
    sgug_d = din("sgu_g", [DEPTH, 256])
    sguw_d = din("sgu_w", [DEPTH, 4, 128, 128])
    sgub_d = din("sgu_b", [DEPTH, 4, 128])
    w_out_d = din("w_out", [DEPTH, D, D])
    ln1g_d = din("ln1_g", [DEPTH, D])
    ln1b_d = din("ln1_b", [DEPTH, D])
    ln2g_d = din("ln2_g", [DEPTH, D])
    ln2b_d = din("ln2_b", [DEPTH, D])
    fw1_d = din("ffn_w1", [2, 128, 8 * 1408])
    fw3_d = din("ffn_w3", [2, 128, 8 * 1408])
    fw2_d = din("ffn_w2", [2816, D])
    rw_d = din("router_w", [D, 8])
    mw1_d = din("moe_w1", [8, 128, 8 * 1408])
    mw3_d = din("moe_w3", [8, 128, 8 * 1408])
    mw2_d = din("moe_w2", [8, 1408, D])

    y_d = dout("y", [NTOK, D])
    nk_d = dout("nk", [DEPTH, NTOK, 128])
    nv_d = dout("nv", [DEPTH, NTOK, 128])

    P = Prog(nc)

    def I(eng, method, reads, writes, *a, pw=(), **kw):
        return P.op(eng, reads, writes, lambda e: getattr(e, method)(*a, **kw), pwrites=pw)

    def MM(reads, writes, lst):
        return P.op("pe", reads, writes, lambda e: [e.matmul(**kw) for kw in lst][-1])

    def TR(reads, writes, lst, pw=()):
        return P.op("pe", reads, writes, lambda e: [e.transpose(*a) for a in lst][-1], pwrites=pw)

    def DMA(q, reads, writes, out, in_, is_output=False, pw=()):
        return P.dma(q, reads, writes, lambda e: e.dma_start(out=out, in_=in_), is_output=is_output, pwrites=pw)

    with ExitStack() as top:
        uid = [0]

        def S(es, name, shape, dt=F32):
            uid[0] += 1
            return es.enter_context(nc.sbuf_tensor(f"s{uid[0]}_{name}", list(shape), dt))

        banks = [top.enter_context(nc.psum_tensor(f"bank{i}", [128, 512], F32)) for i in range(8)]
        rbank = [Res(f"bank{i}", psum=True) for i in range(8)]
        pool_ptr = {"a": 0, "b": 0}

        def bank(pool):
            i = pool_ptr[pool]
            pool_ptr[pool] = (i + 1) % 4
            j = i if pool == "a" else 4 + i
            return banks[j], rbank[j]

        x_sb = S(top, "x_sb", [128, NT, D])
        r_x = [Res(f"x{t}") for t in range(NT)]
        hT = S(top, "hT", [128, 8, NTOK], BF16)
        r_h = [Res(f"hT{t}") for t in range(NT)]
        mx67 = S(top, "mx67", [128, 2, NTOK], BF16)
        mx45 = S(top, "mx45", [128, 2, NTOK], BF16)
        gate4 = S(top, "gate4", [128, 4, D])
        r_gate = [Res(f"gate{i}") for i in range(4)]
        lnp = S(top, "lnp", [128, 4, D])
        r_lnp = [Res(f"lnp{i}") for i in range(4)]
        modT = S(top, "modT", [128, 48, 2])
        r_modT = Res("modT")
        fm = S(top, "fm", [128, 72])
        r_fm = Res("fm")
        identf = S(top, "identf", [128, 128])
        r_id = Res("ident")
        cflag = S(top, "cflag", [128, 4])
        r_cflag = Res("cflag")
        condT = S(top, "condT", [128, 16])
        scTb = S(top, "scTb", [128, 16], BF16)
        scAB = S(top, "scAB", [128, 8, 2], BF16)
        r_sc = Res("sc")
        G = S(top, "G", [128, NT, 8])
        r_G = [Res(f"G{t}") for t in range(NT)]
        GQ2 = S(top, "GQ2", [128, NT, 8])
        DDt = S(top, "DDt", [128, 4, NT])
        r_DD = Res("DDt")
        epsc = S(top, "epsc", [128, 1])
        r_eps = Res("eps")

        def mixT(kc):
            if kc < 4:
                return hT[:, kc, :]
            return mx45[:, kc - 4, :] if kc < 6 else mx67[:, kc - 6, :]

        DMA("sp", [], [r_id], identf[:], ident_d)
        DMA("sp", [], [r_cflag], cflag[:], cflag_d)
        I("dve", "memset", [], [r_eps], epsc[:], EPS)

        def rstd_from_var(var_ap, out_ap, r_in, r_out):
            I("act", "activation", [r_in, r_eps], [r_out], out=out_ap, in_=var_ap, func=AF.Sqrt,
              bias=epsc[:, 0:1], scale=1.0)
            I("dve", "reciprocal", [r_out], [r_out], out=out_ap, in_=out_ap)

        with ExitStack() as es0:
            stg = S(es0, "stg0", [16, 128])
            r_stg = Res("stg0")
            DMA("sp", [], [r_stg], stg[:], cond_d)
            I("act", "activation", [r_stg], [r_stg], out=stg[:], in_=stg[:], func=AF.Silu)
            pb, rpb = bank("a")
            TR([r_stg, r_id], [rpb], [(pb[:, 0:16], stg[:], identf[0:16, 0:16])])
            I("dve", "tensor_copy", [rpb], [r_sc], out=condT[:], in_=pb[:, 0:16])
            I("dve", "tensor_copy", [r_sc], [r_sc], out=scTb[:], in_=condT[:])
            I("dve", "tensor_copy", [r_sc], [r_sc], out=scAB[:].rearrange("p k c -> p c k"),
              in_=condT[:].rearrange("p (c k) -> p c k", k=8))
            for t in range(NT):
                DMA("sp", [], [r_x[t]], x_sb[:, t, :], x_d[t * 128:(t + 1) * 128, :])
        P.barrier()

        for l in range(n_layers):
            moe = (l % 2 == 1)
            last = (l == n_layers - 1)

            with ExitStack() as es:
                awb = [S(es, f"awb{i}", [128, 8, 512], BF16) for i in range(3)]
                r_awb = [Res(f"awb{i}") for i in range(3)]
                rowsb = [S(es, f"rowsb{i}", [2, 512]) for i in range(2)]
                r_rows = [Res(f"rows{i}") for i in range(2)]
                gbias = [S(es, f"gbias{i}", [128, 512]) for i in range(2)]
                r_gb = [Res(f"gb{i}") for i in range(2)]
                scRep = S(es, "scRep", [128, 16, 128], BF16)
                r_scRep = Res("scRep")
                stg = S(es, "stgl", [64, 128])
                r_stg = Res("stgl")
                I("dve", "tensor_copy", [r_sc], [r_scRep], out=scRep[:],
                  in_=scTb[:].unsqueeze(2).to_broadcast([128, 16, 128]))
                DMA("sp", [], [], stg[0:48, :], ada_b_d[l], pw=[r_stg])
                DMA("sp", [], [], stg[48:54, :], convw_d[l], pw=[r_stg])
                DMA("sp", [], [], stg[54:56, :], convb_d[l], pw=[r_stg])
                DMA("sp", [], [], stg[56:60, :], sgub_d[l], pw=[r_stg])
                pb, rpb = bank("a")
                TR([r_stg, r_id], [rpb], [(pb[:, 0:60], stg[0:60, :], identf[0:60, 0:60])])
                I("dve", "tensor_copy", [rpb], [r_fm], out=fm[:, 0:60], in_=pb[:, 0:60])
                I("dve", "tensor_scalar", [r_fm], [r_fm], out=fm[:, 60:66], in0=fm[:, 48:54],
                  scalar1=-1.0, scalar2=None, op0=ALU.mult)
                for i, dd in enumerate((ln1g_d, ln1b_d, ln2g_d, ln2b_d)):
                    DMA("sp", [], [r_lnp[i]], lnp[:, i, :], dd[l:l + 1, :].partition_broadcast(128))

                pfm, rpfm = bank("b")
                gi = 0
                for cc in range(12):
                    slot, half = cc // 2, cc % 2
                    bi = cc % 3
                    DMA("pool", [], [r_awb[bi]], awb[bi][:],
                        ada_w_d[l, cc].rearrange("p (kc n) -> p kc n", kc=8))
                    prow, rprow = bank("a")
                    MM([r_awb[bi], r_sc], [rprow], [dict(out=prow[0:2, :], lhsT=scAB[:, kc, :], rhs=awb[bi][:, kc, :],
                                                        start=(kc == 0), stop=(kc == 7)) for kc in range(8)])
                    I("act", "copy", [rprow], [r_rows[cc % 2]], out=rowsb[cc % 2][0:2, :], in_=prow[0:2, :])
                    TR([r_rows[cc % 2], r_id], [], [(pfm[:, 2 * (cc * 4 + oc):2 * (cc * 4 + oc) + 2],
                                                    rowsb[cc % 2][0:2, oc * 128:(oc + 1) * 128], identf[0:2, 0:2]) for oc in range(4)], pw=[rpfm])
                    if slot in (2, 5):
                        gslot = 0 if slot == 2 else 2
                        DMA("sp", [], [r_gb[gi % 2]], gbias[gi % 2][:],
                            ada_b_d[l, cc * 4:(cc + 1) * 4, :].rearrange("(o a) b -> o (a b)", o=1).partition_broadcast(128))
                        for cnd in range(2):
                            pr_, rpr = bank("a")
                            MM([r_awb[bi], r_scRep], [rpr],
                               [dict(out=pr_[:], lhsT=scRep[:, cnd * 8 + kc, :], rhs=awb[bi][:, kc, :],
                                     start=(kc == 0), stop=(kc == 7)) for kc in range(8)])
                            I("dve", "tensor_tensor", [rpr, r_gb[gi % 2]], [r_gate[gslot + cnd]],
                              out=gate4[:, gslot + cnd, half * 512:(half + 1) * 512], in0=pr_[:],
                              in1=gbias[gi % 2][:], op=ALU.add)
                        gi += 1
                I("dve", "tensor_tensor", [rpfm, r_fm], [r_modT], out=modT[:],
                  in0=pfm[:, 0:96].rearrange("p (c k) -> p c k", k=2),
                  in1=fm[:, 0:48].unsqueeze(2).to_broadcast([128, 48, 2]), op=ALU.add)
                for s_ in (1, 4):
                    I("dve", "tensor_scalar", [r_modT], [r_modT], out=modT[:, s_ * 8:(s_ + 1) * 8, :],
                      in0=modT[:, s_ * 8:(s_ + 1) * 8, :], scalar1=1.0, scalar2=None, op0=ALU.add)
            P.barrier()

            with ExitStack() as es:
                ropec = S(es, "ropec", [128, NT, 64])
                ropes = S(es, "ropes", [128, NT, 64])
                r_rope = Res("rope")
                qTall = S(es, "qTall", [128, 8, NTOK], BF16)
                r_qT = [Res(f"qT{t}") for t in range(NT)]
                kTall = S(es, "kTall", [128, 2, 1792], BF16)
                r_kT = [Res(f"kT{t}") for t in range(14)]
                vaug = S(es, "vaug", [128, 14, 2, 128], BF16)
                r_va = [Res(f"va{t}") for t in range(14)]
                wbuf = S(es, "wbuf", [128, 8, 1280], BF16)
                r_wbuf = Res("wbuf")
                wfb = S(es, "wfb", [128, 8, 256], BF16)
                r_wfb = Res("wfb")
                u_sb = S(es, "u_sb", [128, NTOK])
                y_sb = S(es, "y_sb", [128, NTOK])
                r_u, r_y = Res("u"), Res("y")
                xn0 = S(es, "xn", [128, D])
                rdbuf = S(es, "rdbuf", [128, D])
                bufA0 = S(es, "bufA", [128, 640])
                bufB0 = S(es, "bufB", [128, 640])
                bufC0 = S(es, "bufC", [128, 640])
                kvs0 = S(es, "kvs", [128, 640])
                Eb = [S(es, f"E{i}", [128, 512], BF16) for i in range(5)]
                r_E = [Res(f"E{i}") for i in range(5)]
                rd = [rdbuf[:, 0:512], rdbuf[:, 512:1024]]
                r_rd = [Res(f"rd{i}") for i in range(2)]
                g64 = S(es, "g64", [128, 2, 64])
                sgug = S(es, "sgug", [128, 256])
                r_gv = Res("gv")
                wsT = S(es, "wsT", [128, 4, 128], BF16)
                r_wsT = Res("wsT")
                rwf = S(es, "rwf", [128, 8, 8])
                r_rw = Res("rw")
                smc = S(es, "smc", [128, 8])
                r_smc = Res("smc")

                class TS:
                    pass
                sets = []
                for i in range(2):
                    ts = TS()
                    if i == 0:
                        ts.xn, ts.bufA, ts.bufB, ts.bufC, ts.kvs = xn0[:], bufA0[:], bufB0[:], bufC0[:], kvs0[:]
                    else:
                        ts.xn, ts.bufA, ts.bufB = rdbuf[:], u_sb[:, 0:640], u_sb[:, 640:1280]
                        ts.bufC, ts.kvs = y_sb[:, 0:640], y_sb[:, 640:1280]
                    ts.sg = S(es, f"sg{i}", [128, 256])
                    ts.vn = S(es, f"vn{i}", [128, 256], BF16)
                    ts.st = S(es, f"st{i}", [128, 6, 6])
                    ts.mv = S(es, f"mv{i}", [128, 5, 2])
                    ts.sm = S(es, f"sm{i}", [128, 48])
                    for nm in ("xn", "bA", "bB", "bC", "kvs", "sg", "vn", "stl", "mvl", "sml", "smq", "sts", "mvs", "sms", "rt"):
                        setattr(ts, "r_" + nm, Res(f"{nm}{i}"))
                    sets.append(ts)
                s0 = sets[0]
                wsf = s0.xn[:, 0:512].rearrange("p (h q) -> p h q", q=128)
                r_wsf = s0.r_xn
                ckf = s0.bufB[:, 0:512].rearrange("p (c d) -> p c d", d=128)
                cvf = s0.bufC[:, 0:512].rearrange("p (c d) -> p c d", d=128)
                r_ckf, r_cvf = s0.r_bB, s0.r_bC
                h2f_bufs = [u_sb[:, 0:1024].rearrange("p (kc t) -> p kc t", t=128),
                            y_sb[:, 0:1024].rearrange("p (kc t) -> p kc t", t=128)]
                r_h2f_l = [r_u, r_y]

                DMA("sp", [], [], ropec[:], rc_d.rearrange("(t p) d -> p t d", p=128), pw=[r_rope])
                DMA("sp", [], [], ropes[:], rs_d.rearrange("(t p) d -> p t d", p=128), pw=[r_rope])
                DMA("pool", [], [], wbuf[:, :, 0:768],
                    w_in_d[l, :, 0:768].rearrange("(kc p) n -> p kc n", p=128), pw=[r_wbuf])
                DMA("pool", [], [], wbuf[:, :, 768:1280],
                    w_in_d[l, :, 1536:2048].rearrange("(kc p) n -> p kc n", p=128), pw=[r_wbuf])
                for h in range(8):
                    DMA("pool", [], [], qTall[64:128, h, :], mq_d, pw=r_qT)
                for g in range(2):
                    DMA("pool", [], [], kTall[64:128, g, :], mk_d, pw=r_kT)
                I("dve", "memset", [], r_va, vaug[:, :, :, 64:128], 1.0)
                DMA("sp", [], [], g64[:, 0, :], qg_d[l:l + 1, :].partition_broadcast(128), pw=[r_gv])
                DMA("sp", [], [], g64[:, 1, :], kg_d[l:l + 1, :].partition_broadcast(128), pw=[r_gv])
                DMA("sp", [], [], sgug[:], sgug_d[l:l + 1, :].partition_broadcast(128), pw=[r_gv])
                DMA("sp", [], [r_wsf], wsf, sguw_d[l].rearrange("h p q -> p h q"))
                if moe:
                    DMA("sp", [], [r_rw], rwf[:], rw_d.rearrange("(kc p) e -> p kc e", p=128))

                pb, rpb = bank("a")
                TR([r_wsf, r_id], [rpb], [(pb[:, h * 128:(h + 1) * 128], wsf[:, h, :], identf[:]) for h in range(4)])
                I("dve", "tensor_copy", [rpb], [r_wsT], out=wsT[:].rearrange("p h q -> p (h q)"), in_=pb[:])

                def ln_stats(ts, src_ap, r_src):
                    st, mv, sm = ts.st, ts.mv, ts.sm
                    P.op("dve", [r_src], [ts.r_stl], lambda e, st=st, src_ap=src_ap: [e.bn_stats(out=st[:, 0, :], in_=src_ap[:, 0:512]),
                                                                                      e.bn_stats(out=st[:, 1, :], in_=src_ap[:, 512:1024])][-1])
                    I("dve", "bn_aggr", [ts.r_stl], [ts.r_mvl], out=mv[:, 0, :], in_=st[:, 0:2, :].rearrange("p a b -> p (a b)"))
                    I("act", "activation", [ts.r_mvl, r_eps], [ts.r_sml], out=sm[:, 0:1], in_=mv[:, 0, 1:2], func=AF.Sqrt,
                      bias=epsc[:, 0:1], scale=1.0)
                    yield
                    I("dve", "reciprocal", [ts.r_sml], [ts.r_sml], out=sm[:, 0:1], in_=sm[:, 0:1])
                    I("dve", "scalar_tensor_tensor", [ts.r_mvl, ts.r_sml], [ts.r_sml], out=sm[:, 1:2], in0=mv[:, 0, 0:1],
                      scalar=-1.0, in1=sm[:, 0:1], op0=ALU.mult, op1=ALU.mult)

                def modulate_transpose(ts, t, slot_shift, slot_scale, src_ap, r_src, want_f32):
                    cnd = 0 if t < 8 else 1
                    xn, r_xn, sm = ts.xn, ts.r_xn, ts.sm
                    h2f, r_h2f = h2f_bufs[t % 2], r_h2f_l[t % 2]
                    yield from ln_stats(ts, src_ap, r_src)
                    I("act", "activation", [r_src, ts.r_sml], [r_xn], out=xn, in_=src_ap, func=AF.Identity,
                      bias=sm[:, 1:2], scale=sm[:, 0:1])
                    pa, rpa = bank("a")
                    pb2, rpb2 = bank("a")
                    TR([r_xn, r_id], [rpa, rpb2],
                       [((pa if kc < 4 else pb2)[:, (kc % 4) * 128:(kc % 4 + 1) * 128], xn[:, kc * 128:(kc + 1) * 128], identf[:])
                        for kc in range(8)])
                    yield
                    for kc in range(8):
                        src = (pa if kc < 4 else pb2)[:, (kc % 4) * 128:(kc % 4 + 1) * 128]
                        rsrc = rpa if kc < 4 else rpb2
                        sc_ap = modT[:, slot_scale * 8 + kc, cnd:cnd + 1]
                        sh_ap = modT[:, slot_shift * 8 + kc, cnd:cnd + 1]
                        if want_f32:
                            dst, rdst = h2f[:, kc, :], r_h2f
                        else:
                            dst, rdst = hT[:, kc, t * 128:(t + 1) * 128], r_h[t]
                        if kc < 4:
                            I("act", "activation", [rsrc, r_modT], [], out=dst, in_=src, func=AF.Identity,
                              bias=sh_ap, scale=sc_ap, pw=[rdst])
                        else:
                            I("dve", "tensor_scalar", [rsrc, r_modT], [], out=dst, in0=src, scalar1=sc_ap,
                              scalar2=sh_ap, op0=ALU.mult, op1=ALU.add, pw=[rdst])
                    if want_f32:
                        I("dve", "tensor_copy", [r_h2f], [r_h[t]], out=hT[:, :, t * 128:(t + 1) * 128], in_=h2f)

                def stage_env(t):
                    ts = sets[t % 2]
                    return ts, slice(t * 128, (t + 1) * 128)

                def stA(t):
                    ts, tc_ = stage_env(t)
                    for _ in modulate_transpose(ts, t, 0, 1, x_sb[:, t, :], r_x[t], False):
                        pass

                def stB(t):
                    ts, tc_ = stage_env(t)
                    bufA, bufB, bufC, kvs, sg, vn = ts.bufA, ts.bufB, ts.bufC, ts.kvs, ts.sg, ts.vn
                    r_bA, r_bB, r_bC, r_kvs, r_sg, r_vn = ts.r_bA, ts.r_bB, ts.r_bC, ts.r_kvs, ts.r_sg, ts.r_vn
                    st, mv, sm = ts.st, ts.mv, ts.sm
                    pq, rpq = bank("b")
                    MM([r_h[t], r_wbuf], [rpq], [dict(out=pq[:], lhsT=hT[:, kc, tc_], rhs=wbuf[:, kc, 0:512],
                                                      start=(kc == 0), stop=(kc == 7)) for kc in range(8)])
                    pk, rpk = bank("b")
                    MM([r_h[t], r_wbuf], [rpk], [dict(out=pk[:, 0:256], lhsT=hT[:, kc, tc_], rhs=wbuf[:, kc, 512:768],
                                                      start=(kc == 0), stop=(kc == 7)) for kc in range(8)])
                    ps_, rps = bank("b")
                    MM([r_h[t], r_wbuf], [rps], [dict(out=ps_[:], lhsT=hT[:, kc, tc_], rhs=wbuf[:, kc, 768:1280],
                                                      start=(kc == 0), stop=(kc == 7)) for kc in range(8)])
                    I("act", "copy", [rpq], [], out=bufA[:, 0:512], in_=pq[:], pw=[r_bA])
                    I("act", "copy", [rpk], [], out=bufA[:, 512:640], in_=pk[:, 0:128], pw=[r_bA])
                    I("act", "copy", [rpk], [], out=kvs[:, 0:128], in_=pk[:, 128:256], pw=[r_kvs])
                    I("act", "copy", [rps], [], out=kvs[:, 128:640], in_=ps_[:], pw=[r_kvs])
                    DMA("sp", [r_kvs], [], nv_d[l, tc_, :], kvs[:, 0:128], is_output=True)
                    I("dve", "tensor_copy", [r_kvs], [r_va[t]], out=vaug[:, t, :, 0:64],
                      in_=kvs[:, 0:128].rearrange("p (g d) -> p g d", g=2))

                def stC(t):
                    ts, tc_ = stage_env(t)
                    bufA, bufB, bufC, kvs, sg, vn = ts.bufA, ts.bufB, ts.bufC, ts.kvs, ts.sg, ts.vn
                    r_bA, r_bB, r_bC, r_kvs, r_sg, r_vn = ts.r_bA, ts.r_bB, ts.r_bC, ts.r_kvs, ts.r_sg, ts.r_vn
                    st, mv, sm = ts.st, ts.mv, ts.sm
                    smq = sm[:, 8:18]
                    I("act", "activation", [r_bA], [r_bB], out=bufB, in_=bufA, func=AF.Square)
                    I("dve", "reduce_sum", [r_bB], [ts.r_smq], out=smq,
                      in_=bufB.rearrange("p (h d) -> p h d", d=64), axis=AX.X)
                    I("dve", "tensor_scalar", [ts.r_smq], [ts.r_smq], out=smq, in0=smq,
                      scalar1=1.0 / 64, scalar2=None, op0=ALU.mult)
                    rstd_from_var(smq, smq, ts.r_smq, ts.r_smq)
                    A3 = bufA.rearrange("p (h d) -> p h d", d=64)
                    I("dve", "tensor_tensor", [r_bA, ts.r_smq], [r_bA], out=A3, in0=A3,
                      in1=smq.unsqueeze(2).to_broadcast([128, 10, 64]), op=ALU.mult)
                    I("dve", "tensor_tensor", [r_bA, r_gv], [r_bA], out=A3[:, 0:8, :], in0=A3[:, 0:8, :],
                      in1=g64[:, 0:1, :].to_broadcast([128, 8, 64]), op=ALU.mult)
                    I("dve", "tensor_tensor", [r_bA, r_gv], [r_bA], out=A3[:, 8:10, :], in0=A3[:, 8:10, :],
                      in1=g64[:, 1:2, :].to_broadcast([128, 2, 64]), op=ALU.mult)
                    B3 = bufB.rearrange("p (h d) -> p h d", d=64)
                    I("dve", "tensor_tensor", [r_bA, r_rope], [r_bB], out=B3, in0=A3,
                      in1=ropec[:, t:t + 1, :].to_broadcast([128, 10, 64]), op=ALU.mult)
                    A5 = bufA.rearrange("p (h a j f) -> p h a j f", a=2, j=2, f=16)
                    C5 = bufC.rearrange("p (h a j f) -> p h a j f", a=2, j=2, f=16)
                    S5 = ropes[:, t, :].rearrange("p (a j f) -> p a j f", a=2, j=2, f=16)
                    for a in range(2):
                        for j in range(2):
                            I("pool", "tensor_tensor", [r_bA, r_rope], [], out=C5[:, :, a, j, :], in0=A5[:, :, a, 1 - j, :],
                              in1=S5[:, a:a + 1, j, :].to_broadcast([128, 10, 16]), op=ALU.mult, pw=[r_bC])
                    I("dve", "tensor_tensor", [r_bB, r_bC], [r_bA], out=bufA, in0=bufB, in1=bufC, op=ALU.add)
                    DMA("sp", [r_bA], [], nk_d[l, tc_, :], bufA[:, 512:640], is_output=True)
                    sv3 = kvs[:, 384:640].rearrange("p (h d) -> p h d", d=64)
                    sms = sm[:, 20:24]
                    P.op("dve", [r_kvs], [ts.r_sts], lambda e, sv3=sv3, st=st: [e.bn_stats(out=st[:, 2 + h, :], in_=sv3[:, h, :]) for h in range(4)][-1])
                    P.op("dve", [ts.r_sts], [ts.r_mvs], lambda e, mv=mv, st=st: [e.bn_aggr(out=mv[:, 1 + h, :], in_=st[:, 2 + h, :]) for h in range(4)][-1])
                    rstd_from_var(mv[:, 1:5, 1], sms, ts.r_mvs, ts.r_sms)
                    sg3 = sg[:].rearrange("p (h d) -> p h d", d=64)
                    I("dve", "tensor_tensor", [r_kvs, ts.r_mvs], [r_sg], out=sg3, in0=sv3,
                      in1=mv[:, 1:5, 0:1].to_broadcast([128, 4, 64]), op=ALU.subtract)
                    I("dve", "tensor_tensor", [r_sg, ts.r_sms], [r_sg], out=sg3, in0=sg3,
                      in1=sms.unsqueeze(2).to_broadcast([128, 4, 64]), op=ALU.mult)
                    I("dve", "tensor_tensor", [r_sg, r_gv], [r_vn], out=vn[:], in0=sg[:], in1=sgug[:], op=ALU.mult)

                def stD(t):
                    ts, tc_ = stage_env(t)
                    bufA, bufB, bufC, kvs, sg, vn = ts.bufA, ts.bufB, ts.bufC, ts.kvs, ts.sg, ts.vn
                    r_bA, r_bB, r_bC, r_kvs, r_sg, r_vn = ts.r_bA, ts.r_bB, ts.r_bC, ts.r_kvs, ts.r_sg, ts.r_vn
                    pa, rpa = bank("a")
                    pb2, rpb2 = bank("a")
                    TR([r_bA, r_id], [rpa, rpb2],
                       [(pa[:, c * 128:(c + 1) * 128], bufA[:, c * 128:(c + 1) * 128], identf[:]) for c in range(4)]
                       + [(pb2[:, 0:128], bufA[:, 512:640], identf[:])])
                    pa3 = pa[:].rearrange("p (c t) -> p c t", t=128)
                    I("act", "copy", [rpa], [], out=qTall[0:64, 0:8:2, tc_], in_=pa3[0:64, :, :], pw=[r_qT[t]])
                    I("act", "copy", [rpa], [], out=qTall[0:64, 1:8:2, tc_], in_=pa3[64:128, :, :], pw=[r_qT[t]])
                    I("dve", "tensor_copy", [rpb2], [], out=kTall[0:64, 0, tc_], in_=pb2[0:64, 0:128], pw=[r_kT[t]])
                    I("dve", "tensor_copy", [rpb2], [], out=kTall[0:64, 1, tc_], in_=pb2[64:128, 0:128], pw=[r_kT[t]])
                    psg, rpsg = bank("b")
                    MM([r_vn, r_wsT], [rpsg], [dict(out=psg[:, h * 64:(h + 1) * 64], lhsT=wsT[:, h, :], rhs=vn[:, h * 64:(h + 1) * 64],
                                                    start=True, stop=True) for h in range(4)])
                    for h in range(4):
                        I("dve", "scalar_tensor_tensor", [rpsg, r_fm, r_kvs], [r_sg], out=sg[:, h * 64:(h + 1) * 64],
                          in0=psg[:, h * 64:(h + 1) * 64], scalar=fm[:, 56 + h:57 + h],
                          in1=kvs[:, 128 + h * 64:128 + (h + 1) * 64], op0=ALU.add, op1=ALU.mult)
                    pa, rpa = bank("a")
                    TR([r_sg, r_id], [rpa], [(pa[:, c * 128:(c + 1) * 128], sg[:, c * 128:(c + 1) * 128], identf[:]) for c in range(2)])
                    I("act", "copy", [rpa], [], out=mx67[:, :, tc_], in_=pa[:, 0:256].rearrange("p (c t) -> p c t", t=128), pw=[r_h[t]])

                for s_ in range(NT + 3):
                    if 0 <= s_ - 3 < NT:
                        stD(s_ - 3)
                    if 0 <= s_ - 2 < NT:
                        stC(s_ - 2)
                    if 0 <= s_ - 1 < NT:
                        stB(s_ - 1)
                    if s_ < NT:
                        stA(s_)
                DMA("sp", [], [r_ckf], ckf, ck_d[l].rearrange("(c p) d -> p c d", p=128))
                DMA("sp", [], [r_cvf], cvf, cv_d[l].rearrange("(c p) d -> p c d", p=128))
                pb, rpb = bank("a")
                TR([r_ckf, r_id], [rpb], [(pb[:, c * 128:(c + 1) * 128], ckf[:, c, :], identf[:]) for c in range(4)])
                for g in range(2):
                    I("dve", "tensor_copy", [rpb], r_kT[10:14], out=kTall[0:64, g, 1280:1792],
                      in_=pb[g * 64:(g + 1) * 64, :])
                I("dve", "tensor_copy", [r_cvf], r_va[10:14], out=vaug[:, 10:14, :, 0:64],
                  in_=cvf.rearrange("p c (g d) -> p c g d", g=2))
                P.barrier()

                for ch in range(2):
                    cols = {"cin": 768 + ch * 128, "cb": 1024 + ch * 128, "cc": 1280 + ch * 128}
                    DMA("pool", [], [r_wfb], wfb[:, :, 0:128],
                        w_fm_d[l, 0 + ch].rearrange("p (kc n) -> p kc n", kc=8))
                    DMA("pool", [], [r_wfb], wfb[:, :, 128:256],
                        w_fm_d[l, 4 + ch].rearrange("p (kc n) -> p kc n", kc=8))
                    for (g0, gn) in TG:
                        p1, rp1 = bank("b")
                        MM(r_h + [r_wfb], [rp1], [dict(out=p1[:, 0:gn], lhsT=wfb[:, kc, 0:128], rhs=hT[:, kc, g0:g0 + gn],
                                                       start=(kc == 0), stop=(kc == 7)) for kc in range(8)])
                        p2, rp2 = bank("b")
                        MM(r_h + [r_wfb], [rp2], [dict(out=p2[:, 0:gn], lhsT=wfb[:, kc, 128:256], rhs=hT[:, kc, g0:g0 + gn],
                                                       start=(kc == 0), stop=(kc == 7)) for kc in range(8)])
                        I("act", "copy", [rp1], [r_u], out=u_sb[:, g0:g0 + gn], in_=p1[:, 0:gn])
                        I("dve", "tensor_tensor", [rp2, r_u], [r_u], out=u_sb[:, g0:g0 + gn], in0=p2[:, 0:gn],
                          in1=u_sb[:, g0:g0 + gn], op=ALU.mult)
                    w0, w1, w2 = fm[:, 48 + ch:49 + ch], fm[:, 50 + ch:51 + ch], fm[:, 52 + ch:53 + ch]
                    nw0, nw2 = fm[:, 60 + ch:61 + ch], fm[:, 64 + ch:65 + ch]
                    cbias = fm[:, 54 + ch:55 + ch]
                    I("act", "activation", [r_u, r_fm], [r_y], out=y_sb[:], in_=u_sb[:], func=AF.Identity, bias=cbias, scale=w1)
                    I("dve", "scalar_tensor_tensor", [r_u, r_y, r_fm], [r_y], out=y_sb[:, 1:NTOK], in0=u_sb[:, 0:NTOK - 1],
                      scalar=w0, in1=y_sb[:, 1:NTOK], op0=ALU.mult, op1=ALU.add)
                    I("dve", "scalar_tensor_tensor", [r_u, r_y, r_fm], [r_y], out=y_sb[:, 0:NTOK - 1], in0=u_sb[:, 1:NTOK],
                      scalar=w2, in1=y_sb[:, 0:NTOK - 1], op0=ALU.mult, op1=ALU.add)
                    uv = u_sb[:].rearrange("p (a b) -> p a b", b=256)
                    yv = y_sb[:].rearrange("p (a b) -> p a b", b=256)
                    I("dve", "tensor_tensor", [r_u, r_cflag], [r_smc], out=smc[:, 0:4], in0=uv[:, 0:4, 255], in1=cflag[:], op=ALU.mult)
                    I("dve", "scalar_tensor_tensor", [r_smc, r_y, r_fm], [r_y], out=yv[:, 1:5, 0], in0=smc[:, 0:4], scalar=nw0,
                      in1=yv[:, 1:5, 0], op0=ALU.mult, op1=ALU.add)
                    I("dve", "tensor_tensor", [r_u, r_cflag], [r_smc], out=smc[:, 4:8], in0=uv[:, 1:5, 0], in1=cflag[:], op=ALU.mult)
                    I("dve", "scalar_tensor_tensor", [r_smc, r_y, r_fm], [r_y], out=yv[:, 0:4, 255], in0=smc[:, 4:8], scalar=nw2,
                      in1=yv[:, 0:4, 255], op0=ALU.mult, op1=ALU.add)
                    DMA("pool", [], [r_wfb], wfb[:, :, 0:128],
                        w_fm_d[l, 2 + ch].rearrange("p (kc n) -> p kc n", kc=8))
                    pcb = []
                    for (g0, gn) in TG:
                        p1, rp1 = bank("b")
                        MM(r_h + [r_wfb], [rp1], [dict(out=p1[:, 0:gn], lhsT=wfb[:, kc, 0:128], rhs=hT[:, kc, g0:g0 + gn],
                                                       start=(kc == 0), stop=(kc == 7)) for kc in range(8)])
                        pcb.append((p1, rp1, g0, gn))
                    for (p1, rp1, g0, gn) in pcb:
                        I("dve", "tensor_tensor", [rp1, r_y], [], out=mx45[:, ch, g0:g0 + gn], in0=p1[:, 0:gn],
                          in1=y_sb[:, g0:g0 + gn], op=ALU.mult, pw=[r_h[t_] for t_ in range(g0 // 128, (g0 + gn) // 128)])

                DMA("pool", [], [r_wbuf], wbuf[:, :, 0:1024], w_out_d[l].rearrange("(kc p) n -> p kc n", p=128))
                ecnt = [0]
                rcnt = [0]
                LOOK = 3

                def attention(q0, qn, kts):
                    r_q = [r_qT[t] for t in range(q0 // 128, (q0 + qn) // 128)]
                    r_out = [r_h[t] for t in range(q0 // 128, (q0 + qn) // 128)]
                    for h in range(8):
                        g = h // 4
                        po, rpo = bank("b")
                        pend = []
                        n = len(kts)

                        def flush(last):
                            pi, pe_i, pkt = pend.pop(0)
                            MM([r_E[pe_i], r_va[pkt]], [rpo], [dict(out=po[:, 0:qn], lhsT=vaug[:, pkt, g, :], rhs=Eb[pe_i][:, 0:qn],
                                                                    start=(pi == 0), stop=last)])
                        for i, kt in enumerate(kts):
                            psx, rps_ = bank("a")
                            MM(r_q + [r_kT[kt]], [rps_], [dict(out=psx[:, 0:qn], lhsT=kTall[:, g, kt * 128:(kt + 1) * 128],
                                                               rhs=qTall[:, h, q0:q0 + qn], start=True, stop=True)])
                            ei = ecnt[0] % 5
                            ecnt[0] += 1
                            I("act", "activation", [rps_], [r_E[ei]], out=Eb[ei][:, 0:qn], in_=psx[:, 0:qn], func=AF.Exp, scale=SCALE)
                            pend.append((i, ei, kt))
                            if len(pend) > LOOK:
                                flush(False)
                        while pend:
                            flush(len(pend) == 1)
                        ri = rcnt[0] % 2
                        rcnt[0] += 1
                        I("dve", "reciprocal", [rpo], [r_rd[ri]], out=rd[ri][64:128, 0:qn], in_=po[64:128, 0:qn])
                        ph = (h % 2) * 64
                        I("dve", "tensor_tensor", [rpo, r_rd[ri]], [],
                          out=hT[ph:ph + 64, h // 2, q0:q0 + qn], in0=po[0:64, 0:qn], in1=rd[ri][64:128, 0:qn], op=ALU.mult, pw=r_out)

                ktsA = list(range(8)) + [10, 11, 12, 13]
                attention(0, 512, ktsA)
                attention(512, 512, ktsA)
                attention(1024, 256, [8, 9])
                P.barrier()

                e1banks = {}

                def stE1a(t):
                    tc_ = slice(t * 128, (t + 1) * 128)
                    e1banks[t] = []
                    for half in range(2):
                        pw, rpw = bank("b")
                        MM([r_h[t], r_wbuf], [rpw], [dict(out=pw[:], lhsT=mixT(kc)[:, tc_], rhs=wbuf[:, kc, half * 512:(half + 1) * 512],
                                                          start=(kc == 0), stop=(kc == 7)) for kc in range(8)])
                        e1banks[t].append((pw, rpw))

                def stE1(t):
                    ts = sets[t % 2]
                    xn, r_xn, sm = ts.xn, ts.r_xn, ts.sm
                    tc_ = slice(t * 128, (t + 1) * 128)
                    cnd = 0 if t < 8 else 1
                    for half in range(2):
                        pw, rpw = e1banks[t][half]
                        hs = slice(half * 512, (half + 1) * 512)
                        I("dve", "tensor_tensor", [rpw, r_gate[cnd]], [], out=xn[:, hs], in0=pw[:], in1=gate4[:, cnd, hs], op=ALU.mult, pw=[r_xn])
                    I("dve", "scalar_tensor_tensor", [r_x[t], r_xn], [r_x[t]], out=x_sb[:, t, :], in0=x_sb[:, t, :], scalar=ALPHA,
                      in1=xn, op0=ALU.mult, op1=ALU.add)
                    yield from ln_stats(ts, x_sb[:, t, :], r_x[t])
                    I("act", "activation", [r_x[t], ts.r_sml], [r_xn], out=xn, in_=x_sb[:, t, :], func=AF.Identity,
                      bias=sm[:, 1:2], scale=sm[:, 0:1])
                    yield
                    I("dve", "tensor_tensor", [r_xn, r_lnp[0]], [r_xn], out=xn, in0=xn, in1=lnp[:, 0, :], op=ALU.mult)
                    I("pool", "tensor_tensor", [r_xn, r_lnp[1]], [r_x[t]], out=x_sb[:, t, :], in0=xn, in1=lnp[:, 1, :], op=ALU.add)

                def stE2(t):
                    ts = sets[t % 2]
                    xn, r_xn, sm = ts.xn, ts.r_xn, ts.sm
                    tc_ = slice(t * 128, (t + 1) * 128)
                    cnd = 0 if t < 8 else 1
                    yield from modulate_transpose(ts, t, 3, 4, x_sb[:, t, :], r_x[t], moe)
                    I("act", "mul", [r_x[t]], [r_x[t]], out=x_sb[:, t, :], in_=x_sb[:, t, :], mul=ALPHA)
                    if moe:
                        pr_, rpr = bank("a")
                        h2f, r_h2f = h2f_bufs[t % 2], r_h2f_l[t % 2]
                        MM([r_h2f, r_rw], [rpr], [dict(out=pr_[:, 0:8], lhsT=h2f[:, kc, :], rhs=rwf[:, kc, :],
                                                       start=(kc == 0), stop=(kc == 7)) for kc in range(8)])
                        r_rt = ts.r_rt
                        lg = sm[:, 24:32]
                        m1, m2, dd, e1 = sm[:, 2:3], sm[:, 3:4], sm[:, 4:5], sm[:, 5:6]
                        eq1, eq2, l2 = sm[:, 32:40], sm[:, 40:48], ts.sg[:, 0:8]
                        yield
                        I("dve", "tensor_copy", [rpr], [r_rt], out=lg, in_=pr_[:, 0:8])
                        I("dve", "reduce_max", [r_rt], [r_rt], out=m1, in_=lg, axis=AX.X)
                        I("dve", "tensor_scalar", [r_rt], [r_G[t]], out=G[:, t, :], in0=lg, scalar1=m1, scalar2=None, op0=ALU.is_equal)
                        I("dve", "scalar_tensor_tensor", [r_rt, r_G[t]], [ts.r_sg], out=l2, in0=G[:, t, :], scalar=-1e30, in1=lg,
                          op0=ALU.mult, op1=ALU.add)
                        I("dve", "reduce_max", [ts.r_sg], [r_rt], out=m2, in_=l2, axis=AX.X)
                        I("dve", "tensor_scalar", [r_rt, ts.r_sg], [r_G[t]], out=GQ2[:, t, :], in0=l2, scalar1=m2, scalar2=None, op0=ALU.is_equal)
                        I("dve", "tensor_tensor", [r_rt], [], out=DDt[:, 0, t:t + 1], in0=m2, in1=m1, op=ALU.subtract, pw=[r_DD])

                for s_ in range(NT + 1):
                    if s_ < NT:
                        stE1a(s_)
                    gens = []
                    if s_ < NT:
                        gens.append(stE1(s_))
                    if 0 <= s_ - 1 < NT:
                        gens.append(stE2(s_ - 1))
                    while gens:
                        for g_ in list(gens):
                            try:
                                next(g_)
                            except StopIteration:
                                gens.remove(g_)
                if moe:
                    I("act", "activation", [r_DD], [r_DD], out=DDt[:, 1, :], in_=DDt[:, 0, :], func=AF.Exp)
                    I("dve", "tensor_scalar", [r_DD], [r_DD], out=DDt[:, 2, :], in0=DDt[:, 1, :], scalar1=1.0, scalar2=None, op0=ALU.add)
                    I("dve", "reciprocal", [r_DD], [r_DD], out=DDt[:, 2, :], in_=DDt[:, 2, :])
                    I("dve", "tensor_tensor", [r_DD], [r_DD], out=DDt[:, 3, :], in0=DDt[:, 1, :], in1=DDt[:, 2, :], op=ALU.mult)
                    I("dve", "tensor_tensor", r_G + [r_DD], r_G, out=G[:], in0=G[:],
                      in1=DDt[:, 2, :].unsqueeze(2).to_broadcast([128, NT, 8]), op=ALU.mult)
                    I("dve", "tensor_tensor", r_G + [r_DD], r_G, out=GQ2[:], in0=GQ2[:],
                      in1=DDt[:, 3, :].unsqueeze(2).to_broadcast([128, NT, 8]), op=ALU.mult)
                    I("dve", "tensor_tensor", r_G, r_G, out=G[:], in0=G[:], in1=GQ2[:], op=ALU.add)
            P.barrier()

            with ExitStack() as es:
                w1c = [S(es, f"w1c{i}", [128, 8, 384], BF16) for i in range(3)]
                w3c = [S(es, f"w3c{i}", [128, 8, 384], BF16) for i in range(3)]
                r_w1c = [Res(f"w1c{i}") for i in range(3)]
                r_w3c = [Res(f"w3c{i}") for i in range(3)]
                w2b = S(es, "w2b", [128, 11, D], BF16)
                r_w2b = Res("w2b")
                gT = S(es, "gT", [128, 11, NTOK], BF16)
                r_gT = [Res(f"gT{g}") for g in range(3)]
                sa = [S(es, f"sa{i}", [128, 512]) for i in range(2)]
                r_sa = [Res(f"sa{i}") for i in range(2)]
                tmp = [S(es, f"tmp{i}", [128, 512]) for i in range(2)]
                r_tmp = [Res(f"tmp{i}") for i in range(2)]
                xn = S(es, "xn2", [128, D])
                r_xn = Res("xn2")
                st = S(es, "st2", [128, 2, 6])
                mv = S(es, "mv2", [128, 2])
                sm = S(es, "sm2", [128, 4])
                r_st, r_mv, r_sm = Res("st2"), Res("mv2"), Res("sm2")

                if moe:
                    experts = [(mw1_d[e], mw3_d[e], mw2_d[e], e) for e in range(8)]
                else:
                    experts = [(fw1_d[e], fw3_d[e], fw2_d[e * 1408:(e + 1) * 1408, :], None) for e in range(2)]
                chunks = [(0, 384), (384, 384), (768, 384), (1152, 256)]
                cidx = 0
                sidx = 0
                tidx = 0
                for (w1_ap, w3_ap, w2_ap, ge) in experts:
                    DMA("pool", [], [r_w2b], w2b[:], w2_ap.rearrange("(kc p) n -> p kc n", p=128))
                    for (c0, cw) in chunks:
                        bi = cidx % 3
                        cidx += 1
                        DMA("pool", [], [r_w1c[bi]], w1c[bi][:, :, 0:cw], w1_ap[:, 8 * c0:8 * (c0 + cw)].rearrange("p (kc n) -> p kc n", kc=8))
                        DMA("pool", [], [r_w3c[bi]], w3c[bi][:, :, 0:cw], w3_ap[:, 8 * c0:8 * (c0 + cw)].rearrange("p (kc n) -> p kc n", kc=8))
                        for sub in range(cw // 128):
                            fc = c0 // 128 + sub
                            for gi_, (g0, gn) in enumerate(TG):
                                rh = [r_h[t] for t in range(g0 // 128, (g0 + gn) // 128)]
                                pa_, rpa_ = bank("a")
                                MM(rh + [r_w1c[bi]], [rpa_], [dict(out=pa_[:, 0:gn], lhsT=w1c[bi][:, kc, sub * 128:(sub + 1) * 128],
                                                                   rhs=hT[:, kc, g0:g0 + gn], start=(kc == 0), stop=(kc == 7)) for kc in range(8)])
                                pb_, rpb_ = bank("b")
                                MM(rh + [r_w3c[bi]], [rpb_], [dict(out=pb_[:, 0:gn], lhsT=w3c[bi][:, kc, sub * 128:(sub + 1) * 128],
                                                                   rhs=hT[:, kc, g0:g0 + gn], start=(kc == 0), stop=(kc == 7)) for kc in range(8)])
                                si = sidx % 2
                                sidx += 1
                                I("act", "activation", [rpa_], [r_sa[si]], out=sa[si][:, 0:gn], in_=pa_[:, 0:gn], func=AF.Silu)
                                I("dve", "tensor_tensor", [r_sa[si], rpb_], [], out=gT[:, fc, g0:g0 + gn], in0=sa[si][:, 0:gn],
                                  in1=pb_[:, 0:gn], op=ALU.mult, pw=[r_gT[gi_]])
                    for t in range(NT):
                        cnd = 0 if t < 8 else 1
                        tc_ = slice(t * 128, (t + 1) * 128)
                        for half in range(2):
                            hs = slice(half * 512, (half + 1) * 512)
                            pd, rpd = bank("a" if (t * 2 + half) % 2 == 0 else "b")
                            MM([r_gT[min(t // 4, 2)], r_w2b], [rpd], [dict(out=pd[:], lhsT=gT[:, kc, tc_], rhs=w2b[:, kc, hs],
                                                                          start=(kc == 0), stop=(kc == 10)) for kc in range(11)])
                            ti = tidx % 2
                            tidx += 1
                            if ge is None:
                                I("dve", "tensor_tensor", [rpd, r_gate[2 + cnd]], [r_tmp[ti]], out=tmp[ti][:], in0=pd[:],
                                  in1=gate4[:, 2 + cnd, hs], op=ALU.mult)
                            else:
                                I("act", "activation", [rpd, r_G[t]], [r_tmp[ti]], out=tmp[ti][:], in_=pd[:], func=AF.Identity,
                                  scale=G[:, t, ge:ge + 1])
                                I("dve", "tensor_tensor", [r_tmp[ti], r_gate[2 + cnd]], [r_tmp[ti]], out=tmp[ti][:], in0=tmp[ti][:],
                                  in1=gate4[:, 2 + cnd, hs], op=ALU.mult)
                            I("dve", "tensor_tensor", [r_tmp[ti], r_x[t]], [r_x[t]], out=x_sb[:, t, hs], in0=x_sb[:, t, hs],
                              in1=tmp[ti][:], op=ALU.add)
                for t in range(NT):
                    src = x_sb[:, t, :]
                    P.op("dve", [r_x[t]], [r_st], lambda e, src=src, st=st: [e.bn_stats(out=st[:, 0, :], in_=src[:, 0:512]),
                                                                       e.bn_stats(out=st[:, 1, :], in_=src[:, 512:1024])][-1])
                    I("dve", "bn_aggr", [r_st], [r_mv], out=mv[:], in_=st[:].rearrange("p a b -> p (a b)"))
                    rstd_from_var(mv[:, 1:2], sm[:, 0:1], r_mv, r_sm)
                    I("dve", "scalar_tensor_tensor", [r_mv, r_sm], [r_sm], out=sm[:, 1:2], in0=mv[:, 0:1],
                      scalar=-1.0, in1=sm[:, 0:1], op0=ALU.mult, op1=ALU.mult)
                    I("act", "activation", [r_x[t], r_sm], [r_xn], out=xn[:], in_=src, func=AF.Identity, bias=sm[:, 1:2], scale=sm[:, 0:1])
                    I("dve", "tensor_tensor", [r_xn, r_lnp[2]], [r_xn], out=xn[:], in0=xn[:], in1=lnp[:, 2, :], op=ALU.mult)
                    I("pool", "tensor_tensor", [r_xn, r_lnp[3]], [r_x[t]], out=src, in0=xn[:], in1=lnp[:, 3, :], op=ALU.add)
                    if last:
                        DMA("sp", [r_x[t]], [], y_d[t * 128:(t + 1) * 128, :], src, is_output=True)
            P.barrier()

        P.emit()
    return nc


def _rope_tables():
    n = 1024
    rows = n // 64
    row = np.repeat(np.arange(rows, dtype=np.float32), 64)
    col = np.tile(np.arange(64, dtype=np.float32), rows)
    inv_freq = (np.float32(10000.0) ** (-np.arange(0, 32, 2, dtype=np.float32) / np.float32(32))).astype(np.float32)
    ang = np.stack([row[:, None] * inv_freq, col[:, None] * inv_freq], axis=1).astype(np.float32)
    cos, sin = np.cos(ang).astype(np.float32), np.sin(ang).astype(np.float32)
    cosE = np.stack([cos, cos], axis=2).reshape(n, 64)
    sinE = np.stack([-sin, sin], axis=2).reshape(n, 64)
    return cosE.astype(np.float32), sinE.astype(np.float32)


def _core_inputs(c, inp, shared):
    xs, xp = inp["x_sample"], inp["x_prompt"]
    cosE, sinE = shared["rope"]
    rc = np.ones((NTOK, 64), np.float32)
    rs = np.zeros((NTOK, 64), np.float32)
    ids_q = np.zeros(NTOK, np.int64)
    ids_k = np.zeros(1792, np.int64)
    if c < 2:
        x = np.concatenate([xs[c], xp[c]], axis=0)
        cond = np.stack([inp["c"][c], inp["c_ctx"]])
        ck = inp["cache_k"][c].reshape(DEPTH, 512, 128)
        cv = inp["cache_v"][c].reshape(DEPTH, 512, 128)
        rc[:1024], rs[:1024] = cosE, sinE
        ids_q[1024:] = 1
        ids_k[:NTOK] = ids_q
        ids_k[NTOK:] = 0
        cflag = np.tile(np.array([0, 0, 0, 1], np.float32), (128, 1))
    else:
        p0 = 2 + 5 * (c - 2)
        x = xp[p0:p0 + 5].reshape(NTOK, D)
        cond = np.stack([inp["c_ctx"], inp["c_ctx"]])
        ck = np.zeros((DEPTH, 512, 128), np.float32)
        cv = np.zeros((DEPTH, 512, 128), np.float32)
        ids_q = np.arange(NTOK) // 256
        ids_k[:NTOK] = ids_q
        ids_k[NTOK:] = 63
        cflag = np.ones((128, 4), np.float32)
    mk = (np.arange(64)[:, None] == ids_k[None, :]).astype(np.float32)
    mq = np.where(np.arange(64)[:, None] == ids_q[None, :], 0.0, NEG).astype(np.float32)
    m = dict(shared["weights"])
    m.update({
        "x": np.ascontiguousarray(x, dtype=np.float32),
        "cond": np.ascontiguousarray(cond.reshape(16, 128), dtype=np.float32),
        "ck": np.ascontiguousarray(ck, dtype=np.float32), "cv": np.ascontiguousarray(cv, dtype=np.float32),
        "mq": mq, "mk": mk, "ropec": rc, "ropes": rs, "cflag": cflag,
        "ident": np.eye(128, dtype=np.float32),
    })
    return m


_NC_CACHE = {}


def kernel(**inp):
    inp = {k: np.asarray(v) for k, v in inp.items()}
    f = lambda a: np.ascontiguousarray(a, dtype=np.float32)
    def tile_cols(w):
        E = w.shape[0]
        parts = []
        for c0, cw in ((0, 384), (384, 384), (768, 384), (1152, 256)):
            blk = w[:, :, c0:c0 + cw].reshape(E, 8, 128, cw).transpose(0, 2, 1, 3).reshape(E, 128, 8 * cw)
            parts.append(blk)
        return f(np.concatenate(parts, axis=2))

    ada_t = inp["ada_w"].reshape(DEPTH, 8, 128, 12, 512).transpose(0, 3, 2, 1, 4).reshape(DEPTH, 12, 128, 4096)
    wfm_t = inp["w_in"][:, :, 768:1536].reshape(DEPTH, 8, 128, 6, 128).transpose(0, 3, 2, 1, 4).reshape(DEPTH, 6, 128, 1024)
    fw1 = inp["ffn_w1"][0].reshape(D, 2, 1408).transpose(1, 0, 2)
    fw3 = inp["ffn_w3"][0].reshape(D, 2, 1408).transpose(1, 0, 2)
    weights = {
        "ada_w": f(ada_t), "ada_b": f(inp["ada_b"].reshape(DEPTH, 48, 128)),
        "w_in": f(inp["w_in"]), "w_in_fm": f(wfm_t), "q_g": f(inp["q_norm_g"]), "k_g": f(inp["k_norm_g"]),
        "conv_w": f(inp["conv_w"].reshape(DEPTH, 6, 128)), "conv_b": f(inp["conv_b"].reshape(DEPTH, 2, 128)),
        "sgu_g": f(inp["sgu_norm_g"]), "sgu_w": f(inp["sgu_w"]), "sgu_b": f(inp["sgu_b"]),
        "w_out": f(inp["w_out"]), "ln1_g": f(inp["ln1_g"]), "ln1_b": f(inp["ln1_b"]),
        "ln2_g": f(inp["ln2_g"]), "ln2_b": f(inp["ln2_b"]),
        "ffn_w1": tile_cols(fw1), "ffn_w3": tile_cols(fw3), "ffn_w2": f(inp["ffn_w2"][0]),
        "router_w": f(inp["router_w"][0]), "moe_w1": tile_cols(inp["moe_w1"][0]), "moe_w3": tile_cols(inp["moe_w3"][0]),
        "moe_w2": f(inp["moe_w2"][0]),
    }
    shared = {"weights": weights, "rope": _rope_tables()}
    in_maps = [_core_inputs(c, inp, shared) for c in range(8)]
    if "nc" not in _NC_CACHE:
        _NC_CACHE["nc"] = build_program()
    res = run_bass_kernel_spmd(_NC_CACHE["nc"], in_maps, core_ids=list(range(8)))
    R = res.results
    y_p = np.zeros((32, 256, D), np.float32)
    y_s = np.zeros((2, 1024, D), np.float32)
    new_k = np.zeros((32, DEPTH, 256, 2, 64), np.float32)
    new_v = np.zeros((32, DEPTH, 256, 2, 64), np.float32)
    for c in range(8):
        y, nk, nv = R[c]["y"], R[c]["nk"], R[c]["nv"]
        if c < 2:
            y_s[c] = y[:1024]
            y_p[c] = y[1024:]
            new_k[c] = nk[:, 1024:].reshape(DEPTH, 256, 2, 64)
            new_v[c] = nv[:, 1024:].reshape(DEPTH, 256, 2, 64)
        else:
            p0 = 2 + 5 * (c - 2)
            y_p[p0:p0 + 5] = y.reshape(5, 256, D)
            new_k[p0:p0 + 5] = nk.reshape(DEPTH, 5, 256, 2, 64).transpose(1, 0, 2, 3, 4)
            new_v[p0:p0 + 5] = nv.reshape(DEPTH, 5, 256, 2, 64).transpose(1, 0, 2, 3, 4)
    return (y_p, y_s, new_k, new_v)
```

```python
from contextlib import ExitStack
import numpy as np
import concourse.bass as bass
import concourse.mybir as mybir
from concourse.bass_utils import run_bass_kernel_spmd

F32 = mybir.dt.float32
BF16 = mybir.dt.bfloat16
AF = mybir.ActivationFunctionType
ALU = mybir.AluOpType
AX = mybir.AxisListType

D = 1024
NT = 10
NTOK = 1280
DEPTH = 2
EPS = 1e-6
ALPHA = (2 * DEPTH) ** 0.25
SCALE = 64 ** -0.5
NEG = -30000.0
TG = [(0, 512), (512, 512), (1024, 256)]


class Res:
    __slots__ = ("name", "last_w", "readers", "par_w", "psum")

    def __init__(self, name="", psum=False):
        self.name = name
        self.psum = psum
        self.last_w = None
        self.readers = []
        self.par_w = []


class Prog:
    COMPUTE = ("pe", "act", "dve", "pool")
    NQ = 8

    def __init__(self, nc):
        self.nc = nc
        self.eng = {"pe": nc.tensor, "act": nc.scalar, "dve": nc.vector,
                    "pool": nc.gpsimd, "sp": nc.sync}
        self.count = {e: 0 for e in self.COMPUTE}
        self.ops = {e: [] for e in self.eng}
        self.seen = {e: {} for e in self.eng}
        self.semobj = {}
        self.dsem = {}
        self.dcount = {}
        self.dnext = {}
        for e in self.COMPUTE:
            self.semobj[f"prog_{e}"] = nc.alloc_semaphore(name=f"prog_{e}")
        for q in ("sp", "pool"):
            self.dsem[q] = []
            for i in range(self.NQ):
                nm = f"dma_{q}_{i}"
                self.semobj[nm] = nc.alloc_semaphore(name=nm)
                self.dsem[q].append(nm)
                self.dcount[nm] = 0
            self.dnext[q] = 0
        self.final = []
        self.extra = {e: [] for e in self.eng}

    def _need(self, waits, tok):
        if tok is None:
            return
        s, v = tok
        if waits.get(s, 0) < v:
            waits[s] = v

    def _deps(self, e, reads, writes, pwrites=()):
        waits = {}
        for s, v in self.extra[e]:
            self._need(waits, (s, v))
        self.extra[e] = []
        for r in reads:
            self._need(waits, r.last_w)
            for t in r.par_w:
                self._need(waits, t)
        for w in writes:
            self._need(waits, w.last_w)
            for t in w.par_w:
                self._need(waits, t)
            for t in w.readers:
                self._need(waits, t)
        for w in pwrites:
            self._need(waits, w.last_w)
            for t in w.readers:
                self._need(waits, t)
        out = []
        for s, v in waits.items():
            if e == "pe" and s == "prog_pe":
                continue
            if self.seen[e].get(s, 0) >= v:
                continue
            self.seen[e][s] = v
            out.append((s, v))
        return out

    def _commit(self, tok, reads, writes, pwrites=()):
        for r in reads:
            r.readers.append(tok)
        for w in writes:
            w.last_w = tok
            w.readers = []
            w.par_w = []
        for w in pwrites:
            w.par_w.append(tok)

    PAR = True

    def op(self, e, reads, writes, fn, pwrites=()):
        if not self.PAR:
            writes, pwrites = list(writes) + list(pwrites), ()
        if e != "pe":
            extra_w = [r for r in reads if r.psum and r not in writes]
            if extra_w:
                writes = list(writes) + extra_w
        waits = self._deps(e, reads, writes, pwrites)
        self.count[e] += 1
        tok = (f"prog_{e}", self.count[e])
        self.ops[e].append((waits, fn, tok))
        self._commit(tok, reads, writes, pwrites)
        return tok

    def dma(self, q, reads, writes, fn, is_output=False, pwrites=()):
        i = self.dnext[q]
        self.dnext[q] = (i + 1) % self.NQ
        nm = self.dsem[q][i]
        waits = self._deps(q, reads, writes, pwrites)
        prev = self.dcount[nm]
        if prev > 0 and self.seen[q].get(nm, 0) < prev:
            self.seen[q][nm] = prev
            waits.append((nm, prev))
        self.dcount[nm] = prev + 16
        tok = (nm, prev + 16)
        self.ops[q].append((waits, fn, tok))
        self._commit(tok, reads, writes, pwrites)
        if is_output:
            self.final.append(tok)
        return tok

    def barrier(self):
        toks = [(f"prog_{e}", self.count[e]) for e in self.COMPUTE if self.count[e] > 0]
        toks += [(nm, v) for nm, v in self.dcount.items() if v > 0]
        for e in self.eng:
            self.extra[e] = list(toks)

    def emit(self):
        nc = self.nc
        fin = {}
        for s, v in self.final:
            fin[s] = max(fin.get(s, 0), v)

        def run(e, engine):
            for waits, fn, tok in self.ops[e]:
                for s, v in waits:
                    engine.wait_ge(self.semobj[s], v)
                ins = fn(engine)
                s, v = tok
                ins.then_inc(self.semobj[s], 1 if s.startswith("prog_") else 16)
            if e == "sp":
                for s, v in fin.items():
                    engine.wait_ge(self.semobj[s], v)

        with nc.Block() as block:
            @block.tensor
            def _(eng):
                run("pe", eng)

            @block.scalar
            def _(eng):
                run("act", eng)

            @block.vector
            def _(eng):
                run("dve", eng)

            @block.gpsimd
            def _(eng):
                run("pool", eng)

            @block.sync
            def _(eng):
                run("sp", eng)


def build_program(n_layers=DEPTH):
    nc = bass.Bass("TRN2", target_bir_lowering=False)

    def din(name, shape):
        return nc.dram_tensor(name, list(shape), F32, kind="ExternalInput").ap()

    def dout(name, shape):
        return nc.dram_tensor(name, list(shape), F32, kind="ExternalOutput").ap()

    x_d = din("x", [NTOK, D])
    cond_d = din("cond", [16, 128])
    ck_d = din("ck", [DEPTH, 512, 128])
    cv_d = din("cv", [DEPTH, 512, 128])
    mq_d = din("mq", [64, NTOK])
    mk_d = din("mk", [64, 1792])
    rc_d = din("ropec", [NTOK, 64])
    rs_d = din("ropes", [NTOK, 64])
    cflag_d = din("cflag", [128, 4])
    ident_d = din("ident", [128, 128])
    ada_w_d = din("ada_w", [DEPTH, 12, 128, 4096])
    ada_b_d = din("ada_b", [DEPTH, 48, 128])
    w_in_d = din("w_in", [DEPTH, D, 2048])
    w_fm_d = din("w_in_fm", [DEPTH, 6, 128, 1024])
    qg_d = din("q_g", [DEPTH, 64])
    kg_d = din("k_g", [DEPTH, 64])
    convw_d = din("conv_w", [DEPTH, 6, 128])
    convb_d = din("conv_b", [DEPTH, 2, 128])
    sgug_d = din("sgu_g", [DEPTH, 256])
    sguw_d = din("sgu_w", [DEPTH, 4, 128, 128])
    sgub_d = din("sgu_b", [DEPTH, 4, 128])
    w_out_d = din("w_out", [DEPTH, D, D])
    ln1g_d = din("ln1_g", [DEPTH, D])
    ln1b_d = din("ln1_b", [DEPTH, D])
    ln2g_d = din("ln2_g", [DEPTH, D])
    ln2b_d = din("ln2_b", [DEPTH, D])
    fw1_d = din("ffn_w1", [2, 128, 8 * 1408])
    fw3_d = din("ffn_w3", [2, 128, 8 * 1408])
    fw2_d = din("ffn_w2", [2816, D])
    rw_d = din("router_w", [D, 8])
    mw1_d = din("moe_w1", [8, 128, 8 * 1408])
    mw3_d = din("moe_w3", [8, 128, 8 * 1408])
    mw2_d = din("moe_w2", [8, 1408, D])

    y_d = dout("y", [NTOK, D])
    nk_d = dout("nk", [DEPTH, NTOK, 128])
    nv_d = dout("nv", [DEPTH, NTOK, 128])

    P = Prog(nc)

    def I(eng, method, reads, writes, *a, pw=(), **kw):
        return P.op(eng, reads, writes, lambda e: getattr(e, method)(*a, **kw), pwrites=pw)

    def MM(reads, writes, lst):
        return P.op("pe", reads, writes, lambda e: [e.matmul(**kw) for kw in lst][-1])

    def TR(reads, writes, lst, pw=()):
        return P.op("pe", reads, writes, lambda e: [e.transpose(*a) for a in lst][-1], pwrites=pw)

    def DMA(q, reads, writes, out, in_, is_output=False, pw=()):
        return P.dma(q, reads, writes, lambda e: e.dma_start(out=out, in_=in_), is_output=is_output, pwrites=pw)

    with ExitStack() as top:
        uid = [0]

        def S(es, name, shape, dt=F32):
            uid[0] += 1
            return es.enter_context(nc.sbuf_tensor(f"s{uid[0]}_{name}", list(shape), dt))

        banks = [top.enter_context(nc.psum_tensor(f"bank{i}", [128, 512], F32)) for i in range(8)]
        rbank = [Res(f"bank{i}", psum=True) for i in range(8)]
        pool_ptr = {"a": 0, "b": 0}

        def bank(pool):
            i = pool_ptr[pool]
            pool_ptr[pool] = (i + 1) % 4
            j = i if pool == "a" else 4 + i
            return banks[j], rbank[j]

        x_sb = S(top, "x_sb", [128, NT, D])
        r_x = [Res(f"x{t}") for t in range(NT)]
        hT = S(top, "hT", [128, 8, NTOK], BF16)
        r_h = [Res(f"hT{t}") for t in range(NT)]
        mx67 = S(top, "mx67", [128, 2, NTOK], BF16)
        mx45 = S(top, "mx45", [128, 2, NTOK], BF16)
        gate4 = S(top, "gate4", [128, 4, D])
        r_gate = [Res(f"gate{i}") for i in range(4)]
        lnp = S(top, "lnp", [128, 4, D])
        r_lnp = [Res(f"lnp{i}") for i in range(4)]
        modT = S(top, "modT", [128, 48, 2])
        r_modT = Res("modT")
        fm = S(top, "fm", [128, 72])
        r_fm = Res("fm")
        identf = S(top, "identf", [128, 128])
        r_id = Res("ident")
        cflag = S(top, "cflag", [128, 4])
        r_cflag = Res("cflag")
        condT = S(top, "condT", [128, 16])
        scTb = S(top, "scTb", [128, 16], BF16)
        scAB = S(top, "scAB", [128, 8, 2], BF16)
        r_sc = Res("sc")
        G = S(top, "G", [128, NT, 8])
        r_G = [Res(f"G{t}") for t in range(NT)]
        GQ2 = S(top, "GQ2", [128, NT, 8])
        DDt = S(top, "DDt", [128, 4, NT])
        r_DD = Res("DDt")
        epsc = S(top, "epsc", [128, 1])
        r_eps = Res("eps")

        def mixT(kc):
            if kc < 4:
                return hT[:, kc, :]
            return mx45[:, kc - 4, :] if kc < 6 else mx67[:, kc - 6, :]

        DMA("sp", [], [r_id], identf[:], ident_d)
        DMA("sp", [], [r_cflag], cflag[:], cflag_d)
        I("dve", "memset", [], [r_eps], epsc[:], EPS)

        def rstd_from_var(var_ap, out_ap, r_in, r_out):
            I("act", "activation", [r_in, r_eps], [r_out], out=out_ap, in_=var_ap, func=AF.Sqrt,
              bias=epsc[:, 0:1], scale=1.0)
            I("dve", "reciprocal", [r_out], [r_out], out=out_ap, in_=out_ap)

        with ExitStack() as es0:
            stg = S(es0, "stg0", [16, 128])
            r_stg = Res("stg0")
            DMA("sp", [], [r_stg], stg[:], cond_d)
            I("act", "activation", [r_stg], [r_stg], out=stg[:], in_=stg[:], func=AF.Silu)
            pb, rpb = bank("a")
            TR([r_stg, r_id], [rpb], [(pb[:, 0:16], stg[:], identf[0:16, 0:16])])
            I("dve", "tensor_copy", [rpb], [r_sc], out=condT[:], in_=pb[:, 0:16])
            I("dve", "tensor_copy", [r_sc], [r_sc], out=scTb[:], in_=condT[:])
            I("dve", "tensor_copy", [r_sc], [r_sc], out=scAB[:].rearrange("p k c -> p c k"),
              in_=condT[:].rearrange("p (c k) -> p c k", k=8))
            for t in range(NT):
                DMA("sp", [], [r_x[t]], x_sb[:, t, :], x_d[t * 128:(t + 1) * 128, :])
        P.barrier()

        for l in range(n_layers):
            moe = (l % 2 == 1)
            last = (l == n_layers - 1)

            with ExitStack() as es:
                awb = [S(es, f"awb{i}", [128, 8, 512], BF16) for i in range(3)]
                r_awb = [Res(f"awb{i}") for i in range(3)]
                rowsb = [S(es, f"rowsb{i}", [2, 512]) for i in range(2)]
                r_rows = [Res(f"rows{i}") for i in range(2)]
                gbias = [S(es, f"gbias{i}", [128, 512]) for i in range(2)]
                r_gb = [Res(f"gb{i}") for i in range(2)]
                scRep = S(es, "scRep", [128, 16, 128], BF16)
                r_scRep = Res("scRep")
                stg = S(es, "stgl", [64, 128])
                r_stg = Res("stgl")
                I("dve", "tensor_copy", [r_sc], [r_scRep], out=scRep[:],
                  in_=scTb[:].unsqueeze(2).to_broadcast([128, 16, 128]))
                DMA("sp", [], [], stg[0:48, :], ada_b_d[l], pw=[r_stg])
                DMA("sp", [], [], stg[48:54, :], convw_d[l], pw=[r_stg])
                DMA("sp", [], [], stg[54:56, :], convb_d[l], pw=[r_stg])
                DMA("sp", [], [], stg[56:60, :], sgub_d[l], pw=[r_stg])
                pb, rpb = bank("a")
                TR([r_stg, r_id], [rpb], [(pb[:, 0:60], stg[0:60, :], identf[0:60, 0:60])])
                I("dve", "tensor_copy", [rpb], [r_fm], out=fm[:, 0:60], in_=pb[:, 0:60])
                I("dve", "tensor_scalar", [r_fm], [r_fm], out=fm[:, 60:66], in0=fm[:, 48:54],
                  scalar1=-1.0, scalar2=None, op0=ALU.mult)
                for i, dd in enumerate((ln1g_d, ln1b_d, ln2g_d, ln2b_d)):
                    DMA("sp", [], [r_lnp[i]], lnp[:, i, :], dd[l:l + 1, :].partition_broadcast(128))

                pfm, rpfm = bank("b")
                gi = 0
                for cc in range(12):
                    slot, half = cc // 2, cc % 2
                    bi = cc % 3
                    DMA("pool", [], [r_awb[bi]], awb[bi][:],
                        ada_w_d[l, cc].rearrange("p (kc n) -> p kc n", kc=8))
                    prow, rprow = bank("a")
                    MM([r_awb[bi], r_sc], [rprow], [dict(out=prow[0:2, :], lhsT=scAB[:, kc, :], rhs=awb[bi][:, kc, :],
                                                        start=(kc == 0), stop=(kc == 7)) for kc in range(8)])
                    I("act", "copy", [rprow], [r_rows[cc % 2]], out=rowsb[cc % 2][0:2, :], in_=prow[0:2, :])
                    TR([r_rows[cc % 2], r_id], [], [(pfm[:, 2 * (cc * 4 + oc):2 * (cc * 4 + oc) + 2],
                                                    rowsb[cc % 2][0:2, oc * 128:(oc + 1) * 128], identf[0:2, 0:2]) for oc in range(4)], pw=[rpfm])
                    if slot in (2, 5):
                        gslot = 0 if slot == 2 else 2
                        DMA("sp", [], [r_gb[gi % 2]], gbias[gi % 2][:],
                            ada_b_d[l, cc * 4:(cc + 1) * 4, :].rearrange("(o a) b -> o (a b)", o=1).partition_broadcast(128))
                        for cnd in range(2):
                            pr_, rpr = bank("a")
                            MM([r_awb[bi], r_scRep], [rpr],
                               [dict(out=pr_[:], lhsT=scRep[:, cnd * 8 + kc, :], rhs=awb[bi][:, kc, :],
                                     start=(kc == 0), stop=(kc == 7)) for kc in range(8)])
                            I("dve", "tensor_tensor", [rpr, r_gb[gi % 2]], [r_gate[gslot + cnd]],
                              out=gate4[:, gslot + cnd, half * 512:(half + 1) * 512], in0=pr_[:],
                              in1=gbias[gi % 2][:], op=ALU.add)
                        gi += 1
                I("dve", "tensor_tensor", [rpfm, r_fm], [r_modT], out=modT[:],
                  in0=pfm[:, 0:96].rearrange("p (c k) -> p c k", k=2),
                  in1=fm[:, 0:48].unsqueeze(2).to_broadcast([128, 48, 2]), op=ALU.add)
                for s_ in (1, 4):
                    I("dve", "tensor_scalar", [r_modT], [r_modT], out=modT[:, s_ * 8:(s_ + 1) * 8, :],
                      in0=modT[:, s_ * 8:(s_ + 1) * 8, :], scalar1=1.0, scalar2=None, op0=ALU.add)
            P.barrier()

            with ExitStack() as es:
                ropec = S(es, "ropec", [128, NT, 64])
                ropes = S(es, "ropes", [128, NT, 64])
                r_rope = Res("rope")
                qTall = S(es, "qTall", [128, 8, NTOK], BF16)
                r_qT = [Res(f"qT{t}") for t in range(NT)]
                kTall = S(es, "kTall", [128, 2, 1792], BF16)
                r_kT = [Res(f"kT{t}") for t in range(14)]
                vaug = S(es, "vaug", [128, 14, 2, 128], BF16)
                r_va = [Res(f"va{t}") for t in range(14)]
                wbuf = S(es, "wbuf", [128, 8, 1280], BF16)
                r_wbuf = Res("wbuf")
                wfb = S(es, "wfb", [128, 8, 256], BF16)
                r_wfb = Res("wfb")
                u_sb = S(es, "u_sb", [128, NTOK])
                y_sb = S(es, "y_sb", [128, NTOK])
                r_u, r_y = Res("u"), Res("y")
                xn0 = S(es, "xn", [128, D])
                rdbuf = S(es, "rdbuf", [128, D])
                bufA0 = S(es, "bufA", [128, 640])
                bufB0 = S(es, "bufB", [128, 640])
                bufC0 = S(es, "bufC", [128, 640])
                kvs0 = S(es, "kvs", [128, 640])
                Eb = [S(es, f"E{i}", [128, 512], BF16) for i in range(5)]
                r_E = [Res(f"E{i}") for i in range(5)]
                rd = [rdbuf[:, 0:512], rdbuf[:, 512:1024]]
                r_rd = [Res(f"rd{i}") for i in range(2)]
                g64 = S(es, "g64", [128, 2, 64])
                sgug = S(es, "sgug", [128, 256])
                r_gv = Res("gv")
                wsT = S(es, "wsT", [128, 4, 128], BF16)
                r_wsT = Res("wsT")
                rwf = S(es, "rwf", [128, 8, 8])
                r_rw = Res("rw")
                smc = S(es, "smc", [128, 8])
                r_smc = Res("smc")

                class TS:
                    pass
                sets = []
                for i in range(2):
                    ts = TS()
                    if i == 0:
                        ts.xn, ts.bufA, ts.bufB, ts.bufC, ts.kvs = xn0[:], bufA0[:], bufB0[:], bufC0[:], kvs0[:]
                    else:
                        ts.xn, ts.bufA, ts.bufB = rdbuf[:], u_sb[:, 0:640], u_sb[:, 640:1280]
                        ts.bufC, ts.kvs = y_sb[:, 0:640], y_sb[:, 640:1280]
                    ts.sg = S(es, f"sg{i}", [128, 256])
                    ts.vn = S(es, f"vn{i}", [128, 256], BF16)
                    ts.st = S(es, f"st{i}", [128, 6, 6])
                    ts.mv = S(es, f"mv{i}", [128, 5, 2])
                    ts.sm = S(es, f"sm{i}", [128, 48])
                    for nm in ("xn", "bA", "bB", "bC", "kvs", "sg", "vn", "stl", "mvl", "sml", "smq", "sts", "mvs", "sms", "rt"):
                        setattr(ts, "r_" + nm, Res(f"{nm}{i}"))
                    sets.append(ts)
                s0 = sets[0]
                wsf = s0.xn[:, 0:512].rearrange("p (h q) -> p h q", q=128)
                r_wsf = s0.r_xn
                ckf = s0.bufB[:, 0:512].rearrange("p (c d) -> p c d", d=128)
                cvf = s0.bufC[:, 0:512].rearrange("p (c d) -> p c d", d=128)
                r_ckf, r_cvf = s0.r_bB, s0.r_bC
                h2f_bufs = [u_sb[:, 0:1024].rearrange("p (kc t) -> p kc t", t=128),
                            y_sb[:, 0:1024].rearrange("p (kc t) -> p kc t", t=128)]
                r_h2f_l = [r_u, r_y]

                DMA("sp", [], [], ropec[:], rc_d.rearrange("(t p) d -> p t d", p=128), pw=[r_rope])
                DMA("sp", [], [], ropes[:], rs_d.rearrange("(t p) d -> p t d", p=128), pw=[r_rope])
                DMA("pool", [], [], wbuf[:, :, 0:768],
                    w_in_d[l, :, 0:768].rearrange("(kc p) n -> p kc n", p=128), pw=[r_wbuf])
                DMA("pool", [], [], wbuf[:, :, 768:1280],
                    w_in_d[l, :, 1536:2048].rearrange("(kc p) n -> p kc n", p=128), pw=[r_wbuf])
                for h in range(8):
                    DMA("pool", [], [], qTall[64:128, h, :], mq_d, pw=r_qT)
                for g in range(2):
                    DMA("pool", [], [], kTall[64:128, g, :], mk_d, pw=r_kT)
                I("dve", "memset", [], r_va, vaug[:, :, :, 64:128], 1.0)
                DMA("sp", [], [], g64[:, 0, :], qg_d[l:l + 1, :].partition_broadcast(128), pw=[r_gv])
                DMA("sp", [], [], g64[:, 1, :], kg_d[l:l + 1, :].partition_broadcast(128), pw=[r_gv])
                DMA("sp", [], [], sgug[:], sgug_d[l:l + 1, :].partition_broadcast(128), pw=[r_gv])
                DMA("sp", [], [r_wsf], wsf, sguw_d[l].rearrange("h p q -> p h q"))
                if moe:
                    DMA("sp", [], [r_rw], rwf[:], rw_d.rearrange("(kc p) e -> p kc e", p=128))

                pb, rpb = bank("a")
                TR([r_wsf, r_id], [rpb], [(pb[:, h * 128:(h + 1) * 128], wsf[:, h, :], identf[:]) for h in range(4)])
                I("dve", "tensor_copy", [rpb], [r_wsT], out=wsT[:].rearrange("p h q -> p (h q)"), in_=pb[:])

                def ln_stats(ts, src_ap, r_src):
                    st, mv, sm = ts.st, ts.mv, ts.sm
                    P.op("dve", [r_src], [ts.r_stl], lambda e, st=st, src_ap=src_ap: [e.bn_stats(out=st[:, 0, :], in_=src_ap[:, 0:512]),
                                                                                      e.bn_stats(out=st[:, 1, :], in_=src_ap[:, 512:1024])][-1])
                    I("dve", "bn_aggr", [ts.r_stl], [ts.r_mvl], out=mv[:, 0, :], in_=st[:, 0:2, :].rearrange("p a b -> p (a b)"))
                    I("act", "activation", [ts.r_mvl, r_eps], [ts.r_sml], out=sm[:, 0:1], in_=mv[:, 0, 1:2], func=AF.Sqrt,
                      bias=epsc[:, 0:1], scale=1.0)
                    yield
                    I("dve", "reciprocal", [ts.r_sml], [ts.r_sml], out=sm[:, 0:1], in_=sm[:, 0:1])
                    I("dve", "scalar_tensor_tensor", [ts.r_mvl, ts.r_sml], [ts.r_sml], out=sm[:, 1:2], in0=mv[:, 0, 0:1],
                      scalar=-1.0, in1=sm[:, 0:1], op0=ALU.mult, op1=ALU.mult)

                def modulate_transpose(ts, t, slot_shift, slot_scale, src_ap, r_src, want_f32):
                    cnd = 0 if t < 8 else 1
                    xn, r_xn, sm = ts.xn, ts.r_xn, ts.sm
                    h2f, r_h2f = h2f_bufs[t % 2], r_h2f_l[t % 2]
                    yield from ln_stats(ts, src_ap, r_src)
                    I("act", "activation", [r_src, ts.r_sml], [r_xn], out=xn, in_=src_ap, func=AF.Identity,
                      bias=sm[:, 1:2], scale=sm[:, 0:1])
                    pa, rpa = bank("a")
                    pb2, rpb2 = bank("a")
                    TR([r_xn, r_id], [rpa, rpb2],
                       [((pa if kc < 4 else pb2)[:, (kc % 4) * 128:(kc % 4 + 1) * 128], xn[:, kc * 128:(kc + 1) * 128], identf[:])
                        for kc in range(8)])
                    yield
                    for kc in range(8):
                        src = (pa if kc < 4 else pb2)[:, (kc % 4) * 128:(kc % 4 + 1) * 128]
                        rsrc = rpa if kc < 4 else rpb2
                        sc_ap = modT[:, slot_scale * 8 + kc, cnd:cnd + 1]
                        sh_ap = modT[:, slot_shift * 8 + kc, cnd:cnd + 1]
                        if want_f32:
                            dst, rdst = h2f[:, kc, :], r_h2f
                        else:
                            dst, rdst = hT[:, kc, t * 128:(t + 1) * 128], r_h[t]
                        if kc < 4:
                            I("act", "activation", [rsrc, r_modT], [], out=dst, in_=src, func=AF.Identity,
                              bias=sh_ap, scale=sc_ap, pw=[rdst])
                        else:
                            I("dve", "tensor_scalar", [rsrc, r_modT], [], out=dst, in0=src, scalar1=sc_ap,
                              scalar2=sh_ap, op0=ALU.mult, op1=ALU.add, pw=[rdst])
                    if want_f32:
                        I("dve", "tensor_copy", [r_h2f], [r_h[t]], out=hT[:, :, t * 128:(t + 1) * 128], in_=h2f)

                def stage_env(t):
                    ts = sets[t % 2]
                    return ts, slice(t * 128, (t + 1) * 128)

                def stA(t):
                    ts, tc_ = stage_env(t)
                    for _ in modulate_transpose(ts, t, 0, 1, x_sb[:, t, :], r_x[t], False):
                        pass

                def stB(t):
                    ts, tc_ = stage_env(t)
                    bufA, bufB, bufC, kvs, sg, vn = ts.bufA, ts.bufB, ts.bufC, ts.kvs, ts.sg, ts.vn
                    r_bA, r_bB, r_bC, r_kvs, r_sg, r_vn = ts.r_bA, ts.r_bB, ts.r_bC, ts.r_kvs, ts.r_sg, ts.r_vn
                    st, mv, sm = ts.st, ts.mv, ts.sm
                    pq, rpq = bank("b")
                    MM([r_h[t], r_wbuf], [rpq], [dict(out=pq[:], lhsT=hT[:, kc, tc_], rhs=wbuf[:, kc, 0:512],
                                                      start=(kc == 0), stop=(kc == 7)) for kc in range(8)])
                    pk, rpk = bank("b")
                    MM([r_h[t], r_wbuf], [rpk], [dict(out=pk[:, 0:256], lhsT=hT[:, kc, tc_], rhs=wbuf[:, kc, 512:768],
                                                      start=(kc == 0), stop=(kc == 7)) for kc in range(8)])
                    ps_, rps = bank("b")
                    MM([r_h[t], r_wbuf], [rps], [dict(out=ps_[:], lhsT=hT[:, kc, tc_], rhs=wbuf[:, kc, 768:1280],
                                                      start=(kc == 0), stop=(kc == 7)) for kc in range(8)])
                    I("act", "copy", [rpq], [], out=bufA[:, 0:512], in_=pq[:], pw=[r_bA])
                    I("act", "copy", [rpk], [], out=bufA[:, 512:640], in_=pk[:, 0:128], pw=[r_bA])
                    I("act", "copy", [rpk], [], out=kvs[:, 0:128], in_=pk[:, 128:256], pw=[r_kvs])
                    I("act", "copy", [rps], [], out=kvs[:, 128:640], in_=ps_[:], pw=[r_kvs])
                    DMA("sp", [r_kvs], [], nv_d[l, tc_, :], kvs[:, 0:128], is_output=True)
                    I("dve", "tensor_copy", [r_kvs], [r_va[t]], out=vaug[:, t, :, 0:64],
                      in_=kvs[:, 0:128].rearrange("p (g d) -> p g d", g=2))

                def stC(t):
                    ts, tc_ = stage_env(t)
                    bufA, bufB, bufC, kvs, sg, vn = ts.bufA, ts.bufB, ts.bufC, ts.kvs, ts.sg, ts.vn
                    r_bA, r_bB, r_bC, r_kvs, r_sg, r_vn = ts.r_bA, ts.r_bB, ts.r_bC, ts.r_kvs, ts.r_sg, ts.r_vn
                    st, mv, sm = ts.st, ts.mv, ts.sm
                    smq = sm[:, 8:18]
                    I("act", "activation", [r_bA], [r_bB], out=bufB, in_=bufA, func=AF.Square)
                    I("dve", "reduce_sum", [r_bB], [ts.r_smq], out=smq,
                      in_=bufB.rearrange("p (h d) -> p h d", d=64), axis=AX.X)
                    I("dve", "tensor_scalar", [ts.r_smq], [ts.r_smq], out=smq, in0=smq,
                      scalar1=1.0 / 64, scalar2=None, op0=ALU.mult)
                    rstd_from_var(smq, smq, ts.r_smq, ts.r_smq)
                    A3 = bufA.rearrange("p (h d) -> p h d", d=64)
                    I("dve", "tensor_tensor", [r_bA, ts.r_smq], [r_bA], out=A3, in0=A3,
                      in1=smq.unsqueeze(2).to_broadcast([128, 10, 64]), op=ALU.mult)
                    I("dve", "tensor_tensor", [r_bA, r_gv], [r_bA], out=A3[:, 0:8, :], in0=A3[:, 0:8, :],
                      in1=g64[:, 0:1, :].to_broadcast([128, 8, 64]), op=ALU.mult)
                    I("dve", "tensor_tensor", [r_bA, r_gv], [r_bA], out=A3[:, 8:10, :], in0=A3[:, 8:10, :],
                      in1=g64[:, 1:2, :].to_broadcast([128, 2, 64]), op=ALU.mult)
                    B3 = bufB.rearrange("p (h d) -> p h d", d=64)
                    I("dve", "tensor_tensor", [r_bA, r_rope], [r_bB], out=B3, in0=A3,
                      in1=ropec[:, t:t + 1, :].to_broadcast([128, 10, 64]), op=ALU.mult)
                    A5 = bufA.rearrange("p (h a j f) -> p h a j f", a=2, j=2, f=16)
                    C5 = bufC.rearrange("p (h a j f) -> p h a j f", a=2, j=2, f=16)
                    S5 = ropes[:, t, :].rearrange("p (a j f) -> p a j f", a=2, j=2, f=16)
                    for a in range(2):
                        for j in range(2):
                            I("pool", "tensor_tensor", [r_bA, r_rope], [], out=C5[:, :, a, j, :], in0=A5[:, :, a, 1 - j, :],
                              in1=S5[:, a:a + 1, j, :].to_broadcast([128, 10, 16]), op=ALU.mult, pw=[r_bC])
                    I("dve", "tensor_tensor", [r_bB, r_bC], [r_bA], out=bufA, in0=bufB, in1=bufC, op=ALU.add)
                    DMA("sp", [r_bA], [], nk_d[l, tc_, :], bufA[:, 512:640], is_output=True)
                    sv3 = kvs[:, 384:640].rearrange("p (h d) -> p h d", d=64)
                    sms = sm[:, 20:24]
                    P.op("dve", [r_kvs], [ts.r_sts], lambda e, sv3=sv3, st=st: [e.bn_stats(out=st[:, 2 + h, :], in_=sv3[:, h, :]) for h in range(4)][-1])
                    P.op("dve", [ts.r_sts], [ts.r_mvs], lambda e, mv=mv, st=st: [e.bn_aggr(out=mv[:, 1 + h, :], in_=st[:, 2 + h, :]) for h in range(4)][-1])
                    rstd_from_var(mv[:, 1:5, 1], sms, ts.r_mvs, ts.r_sms)
                    sg3 = sg[:].rearrange("p (h d) -> p h d", d=64)
                    I("dve", "tensor_tensor", [r_kvs, ts.r_mvs], [r_sg], out=sg3, in0=sv3,
                      in1=mv[:, 1:5, 0:1].to_broadcast([128, 4, 64]), op=ALU.subtract)
                    I("dve", "tensor_tensor", [r_sg, ts.r_sms], [r_sg], out=sg3, in0=sg3,
                      in1=sms.unsqueeze(2).to_broadcast([128, 4, 64]), op=ALU.mult)
                    I("dve", "tensor_tensor", [r_sg, r_gv], [r_vn], out=vn[:], in0=sg[:], in1=sgug[:], op=ALU.mult)

                def stD(t):
                    ts, tc_ = stage_env(t)
                    bufA, bufB, bufC, kvs, sg, vn = ts.bufA, ts.bufB, ts.bufC, ts.kvs, ts.sg, ts.vn
                    r_bA, r_bB, r_bC, r_kvs, r_sg, r_vn = ts.r_bA, ts.r_bB, ts.r_bC, ts.r_kvs, ts.r_sg, ts.r_vn
                    pa, rpa = bank("a")
                    pb2, rpb2 = bank("a")
                    TR([r_bA, r_id], [rpa, rpb2],
                       [(pa[:, c * 128:(c + 1) * 128], bufA[:, c * 128:(c + 1) * 128], identf[:]) for c in range(4)]
                       + [(pb2[:, 0:128], bufA[:, 512:640], identf[:])])
                    pa3 = pa[:].rearrange("p (c t) -> p c t", t=128)
                    I("act", "copy", [rpa], [], out=qTall[0:64, 0:8:2, tc_], in_=pa3[0:64, :, :], pw=[r_qT[t]])
                    I("act", "copy", [rpa], [], out=qTall[0:64, 1:8:2, tc_], in_=pa3[64:128, :, :], pw=[r_qT[t]])
                    I("dve", "tensor_copy", [rpb2], [], out=kTall[0:64, 0, tc_], in_=pb2[0:64, 0:128], pw=[r_kT[t]])
                    I("dve", "tensor_copy", [rpb2], [], out=kTall[0:64, 1, tc_], in_=pb2[64:128, 0:128], pw=[r_kT[t]])
                    psg, rpsg = bank("b")
                    MM([r_vn, r_wsT], [rpsg], [dict(out=psg[:, h * 64:(h + 1) * 64], lhsT=wsT[:, h, :], rhs=vn[:, h * 64:(h + 1) * 64],
                                                    start=True, stop=True) for h in range(4)])
                    for h in range(4):
                        I("dve", "scalar_tensor_tensor", [rpsg, r_fm, r_kvs], [r_sg], out=sg[:, h * 64:(h + 1) * 64],
                          in0=psg[:, h * 64:(h + 1) * 64], scalar=fm[:, 56 + h:57 + h],
                          in1=kvs[:, 128 + h * 64:128 + (h + 1) * 64], op0=ALU.add, op1=ALU.mult)
                    pa, rpa = bank("a")
                    TR([r_sg, r_id], [rpa], [(pa[:, c * 128:(c + 1) * 128], sg[:, c * 128:(c + 1) * 128], identf[:]) for c in range(2)])
                    I("act", "copy", [rpa], [], out=mx67[:, :, tc_], in_=pa[:, 0:256].rearrange("p (c t) -> p c t", t=128), pw=[r_h[t]])

                for s_ in range(NT + 3):
                    if 0 <= s_ - 3 < NT:
                        stD(s_ - 3)
                    gA = None
                    if s_ < NT:
                        ts_a, _ = stage_env(s_)
                        gA = modulate_transpose(ts_a, s_, 0, 1, x_sb[:, s_, :], r_x[s_], False)
                        next(gA)
                        next(gA)
                    if 0 <= s_ - 2 < NT:
                        stC(s_ - 2)
                    if 0 <= s_ - 1 < NT:
                        stB(s_ - 1)
                    if gA is not None:
                        for _ in gA:
                            pass
                DMA("sp", [], [r_ckf], ckf, ck_d[l].rearrange("(c p) d -> p c d", p=128))
                DMA("sp", [], [r_cvf], cvf, cv_d[l].rearrange("(c p) d -> p c d", p=128))
                pb, rpb = bank("a")
                TR([r_ckf, r_id], [rpb], [(pb[:, c * 128:(c + 1) * 128], ckf[:, c, :], identf[:]) for c in range(4)])
                for g in range(2):
                    I("dve", "tensor_copy", [rpb], r_kT[10:14], out=kTall[0:64, g, 1280:1792],
                      in_=pb[g * 64:(g + 1) * 64, :])
                I("dve", "tensor_copy", [r_cvf], r_va[10:14], out=vaug[:, 10:14, :, 0:64],
                  in_=cvf.rearrange("p c (g d) -> p c g d", g=2))
                P.barrier()

                for ch in range(2):
                    cols = {"cin": 768 + ch * 128, "cb": 1024 + ch * 128, "cc": 1280 + ch * 128}
                    DMA("pool", [], [r_wfb], wfb[:, :, 0:128],
                        w_fm_d[l, 0 + ch].rearrange("p (kc n) -> p kc n", kc=8))
                    DMA("pool", [], [r_wfb], wfb[:, :, 128:256],
                        w_fm_d[l, 4 + ch].rearrange("p (kc n) -> p kc n", kc=8))
                    for (g0, gn) in TG:
                        p1, rp1 = bank("b")
                        MM(r_h + [r_wfb], [rp1], [dict(out=p1[:, 0:gn], lhsT=wfb[:, kc, 0:128], rhs=hT[:, kc, g0:g0 + gn],
                                                       start=(kc == 0), stop=(kc == 7)) for kc in range(8)])
                        p2, rp2 = bank("b")
                        MM(r_h + [r_wfb], [rp2], [dict(out=p2[:, 0:gn], lhsT=wfb[:, kc, 128:256], rhs=hT[:, kc, g0:g0 + gn],
                                                       start=(kc == 0), stop=(kc == 7)) for kc in range(8)])
                        I("act", "copy", [rp1], [r_u], out=u_sb[:, g0:g0 + gn], in_=p1[:, 0:gn])
                        I("dve", "tensor_tensor", [rp2, r_u], [r_u], out=u_sb[:, g0:g0 + gn], in0=p2[:, 0:gn],
                          in1=u_sb[:, g0:g0 + gn], op=ALU.mult)
                    w0, w1, w2 = fm[:, 48 + ch:49 + ch], fm[:, 50 + ch:51 + ch], fm[:, 52 + ch:53 + ch]
                    nw0, nw2 = fm[:, 60 + ch:61 + ch], fm[:, 64 + ch:65 + ch]
                    cbias = fm[:, 54 + ch:55 + ch]
                    I("act", "activation", [r_u, r_fm], [r_y], out=y_sb[:], in_=u_sb[:], func=AF.Identity, bias=cbias, scale=w1)
                    I("dve", "scalar_tensor_tensor", [r_u, r_y, r_fm], [r_y], out=y_sb[:, 1:NTOK], in0=u_sb[:, 0:NTOK - 1],
                      scalar=w0, in1=y_sb[:, 1:NTOK], op0=ALU.mult, op1=ALU.add)
                    I("dve", "scalar_tensor_tensor", [r_u, r_y, r_fm], [r_y], out=y_sb[:, 0:NTOK - 1], in0=u_sb[:, 1:NTOK],
                      scalar=w2, in1=y_sb[:, 0:NTOK - 1], op0=ALU.mult, op1=ALU.add)
                    uv = u_sb[:].rearrange("p (a b) -> p a b", b=256)
                    yv = y_sb[:].rearrange("p (a b) -> p a b", b=256)
                    I("dve", "tensor_tensor", [r_u, r_cflag], [r_smc], out=smc[:, 0:4], in0=uv[:, 0:4, 255], in1=cflag[:], op=ALU.mult)
                    I("dve", "scalar_tensor_tensor", [r_smc, r_y, r_fm], [r_y], out=yv[:, 1:5, 0], in0=smc[:, 0:4], scalar=nw0,
                      in1=yv[:, 1:5, 0], op0=ALU.mult, op1=ALU.add)
                    I("dve", "tensor_tensor", [r_u, r_cflag], [r_smc], out=smc[:, 4:8], in0=uv[:, 1:5, 0], in1=cflag[:], op=ALU.mult)
                    I("dve", "scalar_tensor_tensor", [r_smc, r_y, r_fm], [r_y], out=yv[:, 0:4, 255], in0=smc[:, 4:8], scalar=nw2,
                      in1=yv[:, 0:4, 255], op0=ALU.mult, op1=ALU.add)
                    DMA("pool", [], [r_wfb], wfb[:, :, 0:128],
                        w_fm_d[l, 2 + ch].rearrange("p (kc n) -> p kc n", kc=8))
                    pcb = []
                    for (g0, gn) in TG:
                        p1, rp1 = bank("b")
                        MM(r_h + [r_wfb], [rp1], [dict(out=p1[:, 0:gn], lhsT=wfb[:, kc, 0:128], rhs=hT[:, kc, g0:g0 + gn],
                                                       start=(kc == 0), stop=(kc == 7)) for kc in range(8)])
                        pcb.append((p1, rp1, g0, gn))
                    for (p1, rp1, g0, gn) in pcb:
                        I("dve", "tensor_tensor", [rp1, r_y], [], out=mx45[:, ch, g0:g0 + gn], in0=p1[:, 0:gn],
                          in1=y_sb[:, g0:g0 + gn], op=ALU.mult, pw=[r_h[t_] for t_ in range(g0 // 128, (g0 + gn) // 128)])

                DMA("pool", [], [r_wbuf], wbuf[:, :, 0:1024], w_out_d[l].rearrange("(kc p) n -> p kc n", p=128))
                ecnt = [0]
                rcnt = [0]
                LOOK = 3

                def attention(q0, qn, kts):
                    r_q = [r_qT[t] for t in range(q0 // 128, (q0 + qn) // 128)]
                    r_out = [r_h[t] for t in range(q0 // 128, (q0 + qn) // 128)]
                    for h in range(8):
                        g = h // 4
                        po, rpo = bank("b")
                        pend = []
                        n = len(kts)

                        def flush(last):
                            pi, pe_i, pkt = pend.pop(0)
                            MM([r_E[pe_i], r_va[pkt]], [rpo], [dict(out=po[:, 0:qn], lhsT=vaug[:, pkt, g, :], rhs=Eb[pe_i][:, 0:qn],
                                                                    start=(pi == 0), stop=last)])
                        for i, kt in enumerate(kts):
                            psx, rps_ = bank("a")
                            MM(r_q + [r_kT[kt]], [rps_], [dict(out=psx[:, 0:qn], lhsT=kTall[:, g, kt * 128:(kt + 1) * 128],
                                                               rhs=qTall[:, h, q0:q0 + qn], start=True, stop=True)])
                            ei = ecnt[0] % 5
                            ecnt[0] += 1
                            I("act", "activation", [rps_], [r_E[ei]], out=Eb[ei][:, 0:qn], in_=psx[:, 0:qn], func=AF.Exp, scale=SCALE)
                            pend.append((i, ei, kt))
                            if len(pend) > LOOK:
                                flush(False)
                        while pend:
                            flush(len(pend) == 1)
                        ri = rcnt[0] % 2
                        rcnt[0] += 1
                        I("dve", "reciprocal", [rpo], [r_rd[ri]], out=rd[ri][64:128, 0:qn], in_=po[64:128, 0:qn])
                        ph = (h % 2) * 64
                        I("dve", "tensor_tensor", [rpo, r_rd[ri]], [],
                          out=hT[ph:ph + 64, h // 2, q0:q0 + qn], in0=po[0:64, 0:qn], in1=rd[ri][64:128, 0:qn], op=ALU.mult, pw=r_out)

                ktsA = list(range(8)) + [10, 11, 12, 13]
                attention(0, 512, ktsA)
                attention(512, 512, ktsA)
                attention(1024, 256, [8, 9])
                P.barrier()

                e1banks = {}

                def stE1a(t):
                    tc_ = slice(t * 128, (t + 1) * 128)
                    e1banks[t] = []
                    for half in range(2):
                        pw, rpw = bank("b")
                        MM([r_h[t], r_wbuf], [rpw], [dict(out=pw[:], lhsT=mixT(kc)[:, tc_], rhs=wbuf[:, kc, half * 512:(half + 1) * 512],
                                                          start=(kc == 0), stop=(kc == 7)) for kc in range(8)])
                        e1banks[t].append((pw, rpw))

                def stE1(t):
                    ts = sets[t % 2]
                    xn, r_xn, sm = ts.xn, ts.r_xn, ts.sm
                    tc_ = slice(t * 128, (t + 1) * 128)
                    cnd = 0 if t < 8 else 1
                    for half in range(2):
                        pw, rpw = e1banks[t][half]
                        hs = slice(half * 512, (half + 1) * 512)
                        I("dve", "tensor_tensor", [rpw, r_gate[cnd]], [], out=xn[:, hs], in0=pw[:], in1=gate4[:, cnd, hs], op=ALU.mult, pw=[r_xn])
                    I("dve", "scalar_tensor_tensor", [r_x[t], r_xn], [r_x[t]], out=x_sb[:, t, :], in0=x_sb[:, t, :], scalar=ALPHA,
                      in1=xn, op0=ALU.mult, op1=ALU.add)
                    yield from ln_stats(ts, x_sb[:, t, :], r_x[t])
                    I("act", "activation", [r_x[t], ts.r_sml], [r_xn], out=xn, in_=x_sb[:, t, :], func=AF.Identity,
                      bias=sm[:, 1:2], scale=sm[:, 0:1])
                    yield
                    I("dve", "tensor_tensor", [r_xn, r_lnp[0]], [r_xn], out=xn, in0=xn, in1=lnp[:, 0, :], op=ALU.mult)
                    I("pool", "tensor_tensor", [r_xn, r_lnp[1]], [r_x[t]], out=x_sb[:, t, :], in0=xn, in1=lnp[:, 1, :], op=ALU.add)

                def stE2(t):
                    ts = sets[t % 2]
                    xn, r_xn, sm = ts.xn, ts.r_xn, ts.sm
                    tc_ = slice(t * 128, (t + 1) * 128)
                    cnd = 0 if t < 8 else 1
                    yield from modulate_transpose(ts, t, 3, 4, x_sb[:, t, :], r_x[t], moe)
                    I("act", "mul", [r_x[t]], [r_x[t]], out=x_sb[:, t, :], in_=x_sb[:, t, :], mul=ALPHA)
                    if moe:
                        pr_, rpr = bank("a")
                        h2f, r_h2f = h2f_bufs[t % 2], r_h2f_l[t % 2]
                        MM([r_h2f, r_rw], [rpr], [dict(out=pr_[:, 0:8], lhsT=h2f[:, kc, :], rhs=rwf[:, kc, :],
                                                       start=(kc == 0), stop=(kc == 7)) for kc in range(8)])
                        r_rt = ts.r_rt
                        lg = sm[:, 24:32]
                        m1, m2, dd, e1 = sm[:, 2:3], sm[:, 3:4], sm[:, 4:5], sm[:, 5:6]
                        eq1, eq2, l2 = sm[:, 32:40], sm[:, 40:48], ts.sg[:, 0:8]
                        yield
                        I("dve", "tensor_copy", [rpr], [r_rt], out=lg, in_=pr_[:, 0:8])
                        I("dve", "reduce_max", [r_rt], [r_rt], out=m1, in_=lg, axis=AX.X)
                        I("dve", "tensor_scalar", [r_rt], [r_G[t]], out=G[:, t, :], in0=lg, scalar1=m1, scalar2=None, op0=ALU.is_equal)
                        I("dve", "scalar_tensor_tensor", [r_rt, r_G[t]], [ts.r_sg], out=l2, in0=G[:, t, :], scalar=-1e30, in1=lg,
                          op0=ALU.mult, op1=ALU.add)
                        I("dve", "reduce_max", [ts.r_sg], [r_rt], out=m2, in_=l2, axis=AX.X)
                        I("dve", "tensor_scalar", [r_rt, ts.r_sg], [r_G[t]], out=GQ2[:, t, :], in0=l2, scalar1=m2, scalar2=None, op0=ALU.is_equal)
                        I("dve", "tensor_tensor", [r_rt], [], out=DDt[:, 0, t:t + 1], in0=m2, in1=m1, op=ALU.subtract, pw=[r_DD])

                for s_ in range(NT + 1):
                    if s_ < NT:
                        stE1a(s_)
                    gens = []
                    if s_ < NT:
                        gens.append(stE1(s_))
                    if 0 <= s_ - 1 < NT:
                        gens.append(stE2(s_ - 1))
                    while gens:
                        for g_ in list(gens):
                            try:
                                next(g_)
                            except StopIteration:
                                gens.remove(g_)
                if moe:
                    I("act", "activation", [r_DD], [r_DD], out=DDt[:, 1, :], in_=DDt[:, 0, :], func=AF.Exp)
                    I("dve", "tensor_scalar", [r_DD], [r_DD], out=DDt[:, 2, :], in0=DDt[:, 1, :], scalar1=1.0, scalar2=None, op0=ALU.add)
                    I("dve", "reciprocal", [r_DD], [r_DD], out=DDt[:, 2, :], in_=DDt[:, 2, :])
                    I("dve", "tensor_tensor", [r_DD], [r_DD], out=DDt[:, 3, :], in0=DDt[:, 1, :], in1=DDt[:, 2, :], op=ALU.mult)
                    I("dve", "tensor_tensor", r_G + [r_DD], r_G, out=G[:], in0=G[:],
                      in1=DDt[:, 2, :].unsqueeze(2).to_broadcast([128, NT, 8]), op=ALU.mult)
                    I("dve", "tensor_tensor", r_G + [r_DD], r_G, out=GQ2[:], in0=GQ2[:],
                      in1=DDt[:, 3, :].unsqueeze(2).to_broadcast([128, NT, 8]), op=ALU.mult)
                    I("dve", "tensor_tensor", r_G, r_G, out=G[:], in0=G[:], in1=GQ2[:], op=ALU.add)
            P.barrier()

            with ExitStack() as es:
                w1c = [S(es, f"w1c{i}", [128, 8, 384], BF16) for i in range(3)]
                w3c = [S(es, f"w3c{i}", [128, 8, 384], BF16) for i in range(3)]
                r_w1c = [Res(f"w1c{i}") for i in range(3)]
                r_w3c = [Res(f"w3c{i}") for i in range(3)]
                w2b = S(es, "w2b", [128, 11, D], BF16)
                r_w2b = Res("w2b")
                gT = S(es, "gT", [128, 11, NTOK], BF16)
                r_gT = [Res(f"gT{g}") for g in range(3)]
                sa = [S(es, f"sa{i}", [128, 512]) for i in range(2)]
                r_sa = [Res(f"sa{i}") for i in range(2)]
                tmp = [S(es, f"tmp{i}", [128, 512]) for i in range(2)]
                r_tmp = [Res(f"tmp{i}") for i in range(2)]
                xn = S(es, "xn2", [128, D])
                r_xn = Res("xn2")
                st = S(es, "st2", [128, 2, 6])
                mv = S(es, "mv2", [128, 2])
                sm = S(es, "sm2", [128, 4])
                r_st, r_mv, r_sm = Res("st2"), Res("mv2"), Res("sm2")

                if moe:
                    experts = [(mw1_d[e], mw3_d[e], mw2_d[e], e) for e in range(8)]
                else:
                    experts = [(fw1_d[e], fw3_d[e], fw2_d[e * 1408:(e + 1) * 1408, :], None) for e in range(2)]
                chunks = [(0, 384), (384, 384), (768, 384), (1152, 256)]
                cidx = 0
                sidx = 0
                tidx = 0
                for (w1_ap, w3_ap, w2_ap, ge) in experts:
                    DMA("pool", [], [r_w2b], w2b[:], w2_ap.rearrange("(kc p) n -> p kc n", p=128))
                    for (c0, cw) in chunks:
                        bi = cidx % 3
                        cidx += 1
                        DMA("pool", [], [r_w1c[bi]], w1c[bi][:, :, 0:cw], w1_ap[:, 8 * c0:8 * (c0 + cw)].rearrange("p (kc n) -> p kc n", kc=8))
                        DMA("pool", [], [r_w3c[bi]], w3c[bi][:, :, 0:cw], w3_ap[:, 8 * c0:8 * (c0 + cw)].rearrange("p (kc n) -> p kc n", kc=8))
                        for sub in range(cw // 128):
                            fc = c0 // 128 + sub
                            for gi_, (g0, gn) in enumerate(TG):
                                rh = [r_h[t] for t in range(g0 // 128, (g0 + gn) // 128)]
                                pa_, rpa_ = bank("a")
                                MM(rh + [r_w1c[bi]], [rpa_], [dict(out=pa_[:, 0:gn], lhsT=w1c[bi][:, kc, sub * 128:(sub + 1) * 128],
                                                                   rhs=hT[:, kc, g0:g0 + gn], start=(kc == 0), stop=(kc == 7)) for kc in range(8)])
                                pb_, rpb_ = bank("b")
                                MM(rh + [r_w3c[bi]], [rpb_], [dict(out=pb_[:, 0:gn], lhsT=w3c[bi][:, kc, sub * 128:(sub + 1) * 128],
                                                                   rhs=hT[:, kc, g0:g0 + gn], start=(kc == 0), stop=(kc == 7)) for kc in range(8)])
                                si = sidx % 2
                                sidx += 1
                                I("act", "activation", [rpa_], [r_sa[si]], out=sa[si][:, 0:gn], in_=pa_[:, 0:gn], func=AF.Silu)
                                I("dve", "tensor_tensor", [r_sa[si], rpb_], [], out=gT[:, fc, g0:g0 + gn], in0=sa[si][:, 0:gn],
                                  in1=pb_[:, 0:gn], op=ALU.mult, pw=[r_gT[gi_]])
                    for t in range(NT):
                        cnd = 0 if t < 8 else 1
                        tc_ = slice(t * 128, (t + 1) * 128)
                        for half in range(2):
                            hs = slice(half * 512, (half + 1) * 512)
                            pd, rpd = bank("a" if (t * 2 + half) % 2 == 0 else "b")
                            MM([r_gT[min(t // 4, 2)], r_w2b], [rpd], [dict(out=pd[:], lhsT=gT[:, kc, tc_], rhs=w2b[:, kc, hs],
                                                                          start=(kc == 0), stop=(kc == 10)) for kc in range(11)])
                            ti = tidx % 2
                            tidx += 1
                            if ge is None:
                                I("dve", "tensor_tensor", [rpd, r_gate[2 + cnd]], [r_tmp[ti]], out=tmp[ti][:], in0=pd[:],
                                  in1=gate4[:, 2 + cnd, hs], op=ALU.mult)
                            else:
                                I("act", "activation", [rpd, r_G[t]], [r_tmp[ti]], out=tmp[ti][:], in_=pd[:], func=AF.Identity,
                                  scale=G[:, t, ge:ge + 1])
                                I("dve", "tensor_tensor", [r_tmp[ti], r_gate[2 + cnd]], [r_tmp[ti]], out=tmp[ti][:], in0=tmp[ti][:],
                                  in1=gate4[:, 2 + cnd, hs], op=ALU.mult)
                            I("dve", "tensor_tensor", [r_tmp[ti], r_x[t]], [r_x[t]], out=x_sb[:, t, hs], in0=x_sb[:, t, hs],
                              in1=tmp[ti][:], op=ALU.add)
                for t in range(NT):
                    src = x_sb[:, t, :]
                    P.op("dve", [r_x[t]], [r_st], lambda e, src=src, st=st: [e.bn_stats(out=st[:, 0, :], in_=src[:, 0:512]),
                                                                       e.bn_stats(out=st[:, 1, :], in_=src[:, 512:1024])][-1])
                    I("dve", "bn_aggr", [r_st], [r_mv], out=mv[:], in_=st[:].rearrange("p a b -> p (a b)"))
                    rstd_from_var(mv[:, 1:2], sm[:, 0:1], r_mv, r_sm)
                    I("dve", "scalar_tensor_tensor", [r_mv, r_sm], [r_sm], out=sm[:, 1:2], in0=mv[:, 0:1],
                      scalar=-1.0, in1=sm[:, 0:1], op0=ALU.mult, op1=ALU.mult)
                    I("act", "activation", [r_x[t], r_sm], [r_xn], out=xn[:], in_=src, func=AF.Identity, bias=sm[:, 1:2], scale=sm[:, 0:1])
                    I("dve", "tensor_tensor", [r_xn, r_lnp[2]], [r_xn], out=xn[:], in0=xn[:], in1=lnp[:, 2, :], op=ALU.mult)
                    I("pool", "tensor_tensor", [r_xn, r_lnp[3]], [r_x[t]], out=src, in0=xn[:], in1=lnp[:, 3, :], op=ALU.add)
                    if last:
                        DMA("sp", [r_x[t]], [], y_d[t * 128:(t + 1) * 128, :], src, is_output=True)
            P.barrier()

        P.emit()
    return nc


def _rope_tables():
    n = 1024
    rows = n // 64
    row = np.repeat(np.arange(rows, dtype=np.float32), 64)
    col = np.tile(np.arange(64, dtype=np.float32), rows)
    inv_freq = (np.float32(10000.0) ** (-np.arange(0, 32, 2, dtype=np.float32) / np.float32(32))).astype(np.float32)
    ang = np.stack([row[:, None] * inv_freq, col[:, None] * inv_freq], axis=1).astype(np.float32)
    cos, sin = np.cos(ang).astype(np.float32), np.sin(ang).astype(np.float32)
    cosE = np.stack([cos, cos], axis=2).reshape(n, 64)
    sinE = np.stack([-sin, sin], axis=2).reshape(n, 64)
    return cosE.astype(np.float32), sinE.astype(np.float32)


def _core_inputs(c, inp, shared):
    xs, xp = inp["x_sample"], inp["x_prompt"]
    cosE, sinE = shared["rope"]
    rc = np.ones((NTOK, 64), np.float32)
    rs = np.zeros((NTOK, 64), np.float32)
    ids_q = np.zeros(NTOK, np.int64)
    ids_k = np.zeros(1792, np.int64)
    if c < 2:
        x = np.concatenate([xs[c], xp[c]], axis=0)
        cond = np.stack([inp["c"][c], inp["c_ctx"]])
        ck = inp["cache_k"][c].reshape(DEPTH, 512, 128)
        cv = inp["cache_v"][c].reshape(DEPTH, 512, 128)
        rc[:1024], rs[:1024] = cosE, sinE
        ids_q[1024:] = 1
        ids_k[:NTOK] = ids_q
        ids_k[NTOK:] = 0
        cflag = np.tile(np.array([0, 0, 0, 1], np.float32), (128, 1))
    else:
        p0 = 2 + 5 * (c - 2)
        x = xp[p0:p0 + 5].reshape(NTOK, D)
        cond = np.stack([inp["c_ctx"], inp["c_ctx"]])
        ck = np.zeros((DEPTH, 512, 128), np.float32)
        cv = np.zeros((DEPTH, 512, 128), np.float32)
        ids_q = np.arange(NTOK) // 256
        ids_k[:NTOK] = ids_q
        ids_k[NTOK:] = 63
        cflag = np.ones((128, 4), np.float32)
    mk = (np.arange(64)[:, None] == ids_k[None, :]).astype(np.float32)
    mq = np.where(np.arange(64)[:, None] == ids_q[None, :], 0.0, NEG).astype(np.float32)
    m = dict(shared["weights"])
    m.update({
        "x": np.ascontiguousarray(x, dtype=np.float32),
        "cond": np.ascontiguousarray(cond.reshape(16, 128), dtype=np.float32),
        "ck": np.ascontiguousarray(ck, dtype=np.float32), "cv": np.ascontiguousarray(cv, dtype=np.float32),
        "mq": mq, "mk": mk, "ropec": rc, "ropes": rs, "cflag": cflag,
        "ident": np.eye(128, dtype=np.float32),
    })
    return m


_NC_CACHE = {}


def kernel(**inp):
    inp = {k: np.asarray(v) for k, v in inp.items()}
    f = lambda a: np.ascontiguousarray(a, dtype=np.float32)
    def tile_cols(w):
        E = w.shape[0]
        parts = []
        for c0, cw in ((0, 384), (384, 384), (768, 384), (1152, 256)):
            blk = w[:, :, c0:c0 + cw].reshape(E, 8, 128, cw).transpose(0, 2, 1, 3).reshape(E, 128, 8 * cw)
            parts.append(blk)
        return f(np.concatenate(parts, axis=2))

    ada_t = inp["ada_w"].reshape(DEPTH, 8, 128, 12, 512).transpose(0, 3, 2, 1, 4).reshape(DEPTH, 12, 128, 4096)
    wfm_t = inp["w_in"][:, :, 768:1536].reshape(DEPTH, 8, 128, 6, 128).transpose(0, 3, 2, 1, 4).reshape(DEPTH, 6, 128, 1024)
    fw1 = inp["ffn_w1"][0].reshape(D, 2, 1408).transpose(1, 0, 2)
    fw3 = inp["ffn_w3"][0].reshape(D, 2, 1408).transpose(1, 0, 2)
    weights = {
        "ada_w": f(ada_t), "ada_b": f(inp["ada_b"].reshape(DEPTH, 48, 128)),
        "w_in": f(inp["w_in"]), "w_in_fm": f(wfm_t), "q_g": f(inp["q_norm_g"]), "k_g": f(inp["k_norm_g"]),
        "conv_w": f(inp["conv_w"].reshape(DEPTH, 6, 128)), "conv_b": f(inp["conv_b"].reshape(DEPTH, 2, 128)),
        "sgu_g": f(inp["sgu_norm_g"]), "sgu_w": f(inp["sgu_w"]), "sgu_b": f(inp["sgu_b"]),
        "w_out": f(inp["w_out"]), "ln1_g": f(inp["ln1_g"]), "ln1_b": f(inp["ln1_b"]),
        "ln2_g": f(inp["ln2_g"]), "ln2_b": f(inp["ln2_b"]),
        "ffn_w1": tile_cols(fw1), "ffn_w3": tile_cols(fw3), "ffn_w2": f(inp["ffn_w2"][0]),
        "router_w": f(inp["router_w"][0]), "moe_w1": tile_cols(inp["moe_w1"][0]), "moe_w3": tile_cols(inp["moe_w3"][0]),
        "moe_w2": f(inp["moe_w2"][0]),
    }
    shared = {"weights": weights, "rope": _rope_tables()}
    in_maps = [_core_inputs(c, inp, shared) for c in range(8)]
    if "nc" not in _NC_CACHE:
        _NC_CACHE["nc"] = build_program()
    res = run_bass_kernel_spmd(_NC_CACHE["nc"], in_maps, core_ids=list(range(8)))
    R = res.results
    y_p = np.zeros((32, 256, D), np.float32)
    y_s = np.zeros((2, 1024, D), np.float32)
    new_k = np.zeros((32, DEPTH, 256, 2, 64), np.float32)
    new_v = np.zeros((32, DEPTH, 256, 2, 64), np.float32)
    for c in range(8):
        y, nk, nv = R[c]["y"], R[c]["nk"], R[c]["nv"]
        if c < 2:
            y_s[c] = y[:1024]
            y_p[c] = y[1024:]
            new_k[c] = nk[:, 1024:].reshape(DEPTH, 256, 2, 64)
            new_v[c] = nv[:, 1024:].reshape(DEPTH, 256, 2, 64)
        else:
            p0 = 2 + 5 * (c - 2)
            y_p[p0:p0 + 5] = y.reshape(5, 256, D)
            new_k[p0:p0 + 5] = nk.reshape(DEPTH, 5, 256, 2, 64).transpose(1, 0, 2, 3, 4)
            new_v[p0:p0 + 5] = nv.reshape(DEPTH, 5, 256, 2, 64).transpose(1, 0, 2, 3, 4)
    return (y_p, y_s, new_k, new_v)
```
